# Optimizing a Trainium2 kernel written in Bass

```python
import jax, jax.numpy as jnp
from jax import lax
import numpy as np

D_MODEL = 4096
BATCH = 1
SEQ = 16384
DEPTH = 1

RET_HEADS = 8
RET_DV = D_MODEL // (2 * RET_HEADS)
RET_DK = RET_DV // 2
RET_QK = RET_HEADS * RET_DK
RET_WIDTH = RET_HEADS * RET_DV
RET_CHUNK = 128
FOX_HEADS = 16
FOX_DH = D_MODEL // (2 * FOX_HEADS)
FOX_WIDTH = FOX_HEADS * FOX_DH
FOX_BLOCK = 128
MIX_WIDTH = RET_WIDTH + FOX_WIDTH
IN_WIDTH = 2 * RET_QK + 2 * RET_WIDTH + 3 * FOX_WIDTH + FOX_HEADS
ROPE_BASE = 10000.0
PEER_HEADS = 8
PEER_NKEYS = 128
PEER_NEXPERTS = PEER_NKEYS * PEER_NKEYS
PEER_DQ = 256
PEER_TOPK = 16
PEER_CHUNK = 64
EPS = 1e-6

kernel_name = 'hybrid_retention_fox_peer_adaln'


def _in_splits():
    sizes = (RET_QK, RET_QK, RET_WIDTH, RET_WIDTH, FOX_WIDTH, FOX_WIDTH, FOX_WIDTH)
    out, acc = [], 0
    for s in sizes:
        acc += s
        out.append(acc)
    return out


def rmsnorm(x, g):
    xf = x.astype(jnp.float32)
    y = xf * lax.rsqrt(jnp.mean(xf * xf, axis=-1, keepdims=True) + EPS)
    return (y * g.astype(jnp.float32)).astype(x.dtype)


def modulate(h, shift, scale):
    return h * (1.0 + scale[:, None, :]) + shift[:, None, :]


def rotary(x, positions):
    half = x.shape[-1] // 2
    inv_freq = ROPE_BASE ** (-jnp.arange(half, dtype=jnp.float32) / half)
    ang = positions.astype(jnp.float32)[..., None] * inv_freq
    cos = jnp.cos(ang)[:, :, None, :]
    sin = jnp.sin(ang)[:, :, None, :]
    xf = x.astype(jnp.float32)
    x1, x2 = xf[..., :half], xf[..., half:]
    return jnp.concatenate([x1 * cos - x2 * sin, x1 * sin + x2 * cos], axis=-1)


def retention_chunkwise(q, k, v, positions):
    B, S, H, dk = q.shape
    dv = v.shape[-1]
    C = RET_CHUNK
    nc = S // C
    q = rotary(q, positions)
    k = rotary(k, positions) * (dk ** -0.5)
    v = v.astype(jnp.float32)
    log_g = jnp.log1p(-jnp.exp2(-5.0 - jnp.arange(H, dtype=jnp.float32)))

    def chunks(t):
        return t.reshape(B, nc, C, H, t.shape[-1]).transpose(0, 3, 1, 2, 4)

    qc, kc, vc = chunks(q), chunks(k), chunks(v)
    i = jnp.arange(C, dtype=jnp.float32)
    diff = i[:, None] - i[None, :]
    decay_in = jnp.where(diff >= 0, jnp.exp(jnp.maximum(diff, 0.0)[None] * log_g[:, None, None]), 0.0)
    scores = jnp.einsum('bhncd,bhnsd->bhncs', qc, kc) * decay_in[None, :, None]
    intra = jnp.einsum('bhncs,bhnse->bhnce', scores, vc)
    k_dec = kc * jnp.exp((C - 1.0 - i)[None, :] * log_g[:, None])[None, :, None, :, None]
    kv = jnp.einsum('bhncd,bhnce->nbhde', k_dec, vc)
    chunk_decay = jnp.exp(C * log_g)[None, :, None, None]

    def step(state, kv_n):
        return state * chunk_decay + kv_n, state

    _, state_prev = lax.scan(step, jnp.zeros((B, H, dk, dv), jnp.float32), kv)
    q_dec = qc * jnp.exp((i + 1.0)[None, :] * log_g[:, None])[None, :, None, :, None]
    cross = jnp.einsum('bhncd,nbhde->bhnce', q_dec, state_prev)
    return (intra + cross).transpose(0, 2, 3, 1, 4).reshape(B, S, H, dv)


def forgetting_attention(q, k, v, f_logit, b_forget):
    B, S, H, dh = q.shape
    nq = S // FOX_BLOCK
    scale = dh ** -0.5
    log_f = jax.nn.log_sigmoid(f_logit.astype(jnp.float32) + b_forget.astype(jnp.float32))
    cum = jnp.cumsum(log_f, axis=1).transpose(0, 2, 1)
    pos = jnp.arange(S)
    q_blocks = q.reshape(B, nq, FOX_BLOCK, H, dh).transpose(1, 0, 2, 3, 4)
    c_blocks = cum.reshape(B, H, nq, FOX_BLOCK).transpose(2, 0, 1, 3)
    p_blocks = pos.reshape(nq, FOX_BLOCK)

    def block(args):
        q_i, c_i, p_i = args
        logits = jnp.einsum('bqhd,bkhd->bhqk', q_i, k, preferred_element_type=jnp.float32) * scale
        logits = logits + (c_i[..., :, None] - cum[..., None, :])
        causal = p_i[:, None] >= pos[None, :]
        probs = jax.nn.softmax(jnp.where(causal, logits, -jnp.inf), axis=-1)
        return jnp.einsum('bhqk,bkhd->bqhd', probs.astype(v.dtype), v)

    out = lax.map(block, (q_blocks, c_blocks, p_blocks))
    return out.transpose(1, 0, 2, 3, 4).reshape(B, S, H * dh)


def peer_ffn(h, w_q, sub_keys, u, v):
    B, S, D = h.shape
    T = B * S
    ht = h.reshape(T, D)
    q = (ht @ w_q).reshape(T, PEER_HEADS, 2, PEER_DQ // 2)
    s = jnp.einsum('thpd,hpkd->thpk', q, sub_keys, preferred_element_type=jnp.float32)
    s_top, i_top = lax.top_k(s, PEER_TOPK)
    cand = (s_top[:, :, 0, :, None] + s_top[:, :, 1, None, :]).reshape(T, PEER_HEADS, PEER_TOPK * PEER_TOPK)
    c_top, c_idx = lax.top_k(cand, PEER_TOPK)
    i1 = jnp.take_along_axis(i_top[:, :, 0], c_idx // PEER_TOPK, axis=-1)
    i2 = jnp.take_along_axis(i_top[:, :, 1], c_idx % PEER_TOPK, axis=-1)
    expert = (i1 * PEER_NKEYS + i2).reshape(T, PEER_HEADS * PEER_TOPK)
    gates = jax.nn.softmax(c_top, axis=-1).reshape(T, PEER_HEADS * PEER_TOPK)
    nch = T // PEER_CHUNK

    def block(args):
        h_c, e_c, g_c = args
        u_sel = u[e_c]
        a = jnp.einsum('td,ted->te', h_c, u_sel, preferred_element_type=jnp.float32)
        a = jax.nn.gelu(a) * g_c
        v_sel = v[e_c]
        return jnp.einsum('te,ted->td', a.astype(v_sel.dtype), v_sel)

    out = lax.map(block, (ht.reshape(nch, PEER_CHUNK, D),
                          expert.reshape(nch, PEER_CHUNK, PEER_HEADS * PEER_TOPK),
                          gates.reshape(nch, PEER_CHUNK, PEER_HEADS * PEER_TOPK)))
    return out.reshape(B, S, D).astype(h.dtype)


def setup_inputs(seed: int = 0) -> dict:
    key = jax.random.key(seed)
    ks = jax.random.split(key, 16)
    nrm = jax.random.normal
    x = nrm(ks[0], (BATCH, SEQ, D_MODEL), jnp.float32)
    c = nrm(ks[1], (BATCH, D_MODEL), jnp.float32)
    positions = jnp.broadcast_to(jnp.arange(SEQ, dtype=jnp.int32)[None, :], (BATCH, SEQ))
    w_ada = nrm(ks[2], (DEPTH, D_MODEL, 6 * D_MODEL), jnp.float32) * (0.3 * D_MODEL ** -0.5)
    b_ada = 0.01 * nrm(ks[3], (DEPTH, 6 * D_MODEL), jnp.float32)
    norm1_g = 1.0 + 0.02 * nrm(ks[4], (DEPTH, D_MODEL), jnp.float32)
    w_in = nrm(ks[5], (DEPTH, D_MODEL, IN_WIDTH), jnp.float32) * D_MODEL ** -0.5
    b_forget = jnp.linspace(1.0, 5.0, FOX_HEADS, dtype=jnp.float32)[None, :] + 0.1 * nrm(ks[6], (DEPTH, FOX_HEADS), jnp.float32)
    ret_gn_g = 1.0 + 0.02 * nrm(ks[7], (DEPTH, RET_WIDTH), jnp.float32)
    fox_norm_g = 1.0 + 0.02 * nrm(ks[8], (DEPTH, FOX_WIDTH), jnp.float32)
    w_out = nrm(ks[9], (DEPTH, MIX_WIDTH, D_MODEL), jnp.float32) * MIX_WIDTH ** -0.5
    norm2_g = 1.0 + 0.02 * nrm(ks[10], (DEPTH, D_MODEL), jnp.float32)
    w_peer_q = nrm(ks[11], (DEPTH, D_MODEL, PEER_HEADS * PEER_DQ), jnp.float32) * D_MODEL ** -0.5
    peer_sub_keys = nrm(ks[12], (DEPTH, PEER_HEADS, 2, PEER_NKEYS, PEER_DQ // 2), jnp.float32) * (PEER_DQ // 2) ** -0.5
    peer_u = nrm(ks[13], (DEPTH, PEER_NEXPERTS, D_MODEL), jnp.float32) * D_MODEL ** -0.5
    peer_v = nrm(ks[14], (DEPTH, PEER_NEXPERTS, D_MODEL), jnp.float32) * 0.5
    final_g = 1.0 + 0.02 * nrm(ks[15], (D_MODEL,), jnp.float32)
    return {'x': x, 'c': c, 'positions': positions, 'w_ada': w_ada, 'b_ada': b_ada,
            'norm1_g': norm1_g, 'w_in': w_in, 'b_forget': b_forget, 'ret_gn_g': ret_gn_g,
            'fox_norm_g': fox_norm_g, 'w_out': w_out, 'norm2_g': norm2_g, 'w_peer_q': w_peer_q,
            'peer_sub_keys': peer_sub_keys, 'peer_u': peer_u, 'peer_v': peer_v, 'final_g': final_g}


def reference(x, c, positions, w_ada, b_ada, norm1_g, w_in, b_forget, ret_gn_g, fox_norm_g,
              w_out, norm2_g, w_peer_q, peer_sub_keys, peer_u, peer_v, final_g):
    B, S, D = x.shape
    c_act = jax.nn.silu(c)
    splits = _in_splits()
    for l in range(DEPTH):
        mod = c_act @ w_ada[l] + b_ada[l]
        shift1, scale1, gate1, shift2, scale2, gate2 = jnp.split(mod, 6, axis=-1)
        h = modulate(rmsnorm(x, norm1_g[l]), shift1, scale1)
        proj = h @ w_in[l]
        rq, rk, rv, rg, fq, fk, fv, ff = jnp.split(proj, splits, axis=-1)
        y_ret = retention_chunkwise(rq.reshape(B, S, RET_HEADS, RET_DK), rk.reshape(B, S, RET_HEADS, RET_DK),
                                    rv.reshape(B, S, RET_HEADS, RET_DV), positions)
        mu = jnp.mean(y_ret, axis=-1, keepdims=True)
        var = jnp.mean(jnp.square(y_ret - mu), axis=-1, keepdims=True)
        y_ret = ((y_ret - mu) * lax.rsqrt(var + EPS)).reshape(B, S, RET_WIDTH)
        y_ret = y_ret * ret_gn_g[l].astype(jnp.float32) * jax.nn.silu(rg.astype(jnp.float32))
        y_fox = forgetting_attention(fq.reshape(B, S, FOX_HEADS, FOX_DH), fk.reshape(B, S, FOX_HEADS, FOX_DH),
                                     fv.reshape(B, S, FOX_HEADS, FOX_DH), ff, b_forget[l])
        y_fox = rmsnorm(y_fox, fox_norm_g[l])
        mixed = jnp.concatenate([y_ret.astype(x.dtype), y_fox.astype(x.dtype)], axis=-1) @ w_out[l]
        x = x + gate1[:, None, :] * mixed
        h = modulate(rmsnorm(x, norm2_g[l]), shift2, scale2)
        x = x + gate2[:, None, :] * peer_ffn(h, w_peer_q[l], peer_sub_keys[l], peer_u[l], peer_v[l])
    return rmsnorm(x, final_g)
```

```python
import numpy as np
from contextlib import ExitStack
import concourse.bass as bass
import concourse.mybir as mybir
from concourse.bass_utils import run_bass_kernel_spmd

F32 = mybir.dt.float32
BF16 = mybir.dt.bfloat16
I32 = mybir.dt.int32
U32 = mybir.dt.uint32
ALU = mybir.AluOpType
AF = mybir.ActivationFunctionType
AX = mybir.AxisListType


class Tok:
    __slots__ = ("sem", "val", "key")
    def __init__(self, sem, val, key):
        self.sem = sem; self.val = val; self.key = key


class Buf:
    def __init__(self, t, name=""):
        self.t = t
        self.name = name
        self.writers = {}
        self.readers = {}
    def __getitem__(self, idx):
        return self.t[idx]


class Slot:
    def __init__(self, sem, key):
        self.sem = sem; self.total = 0; self.key = key


class Eng:
    def __init__(self, h, sem, key, is_pe=False):
        self.h = h; self.sem = sem; self.count = 0; self.key = key
        self.seen = {}
        self.is_pe = is_pe
    def wait(self, tok, same_ok=False):
        if tok is None:
            return
        if tok.key == self.key and (self.is_pe or same_ok):
            return
        if self.seen.get(tok.key, 0) >= tok.val:
            return
        self.h.wait_ge(tok.sem, tok.val)
        self.seen[tok.key] = tok.val
    def _deps(self, reads, writes):
        for b in reads:
            for t in b.writers.values():
                self.wait(t)
        for b in writes:
            for t in b.writers.values():
                self.wait(t, same_ok=True)
            for t in b.readers.values():
                self.wait(t, same_ok=True)
    def _commit(self, tok, reads, writes):
        for b in reads:
            b.readers[tok.key] = tok
        for b in writes:
            b.writers[tok.key] = tok
            b.readers = {}
    def op(self, fn, reads=(), writes=()):
        self._deps(reads, writes)
        inst = fn()
        self.count += 1
        inst.then_inc(self.sem, 1)
        tok = Tok(self.sem, self.count, self.key)
        self._commit(tok, reads, writes)
        return tok
    def dma(self, out, in_, slot, reads=(), writes=(), cont=False, **kw):
        self._deps(reads, writes)
        if not cont:
            self.wait(Tok(slot.sem, slot.total, slot.key))
        inst = self.h.dma_start(out=out, in_=in_, **kw)
        slot.total += 16
        inst.then_inc(slot.sem, 16)
        tok = Tok(slot.sem, slot.total, slot.key)
        self._commit(tok, reads, writes)
        return tok


class Ctx:
    def __init__(self, nc, es):
        self.nc = nc; self.es = es
        self._n = 0
        def mk(h, name, is_pe=False):
            sem = es.enter_context(nc.semaphore("e_" + name))
            return Eng(h, sem, "e_" + name, is_pe)
        self.pe = mk(nc.tensor, "pe", True)
        self.act = mk(nc.scalar, "act")
        self.dve = mk(nc.vector, "dve")
        self.pool = mk(nc.gpsimd, "pool")
        self.sp = mk(nc.sync, "sp")
        self.engs = [self.pe, self.act, self.dve, self.pool, self.sp]
        self.slots = []
    def new_epoch(self):
        self._site_cnt = {}
    def slot(self, name=None):
        import sys as _sys
        ln = _sys._getframe(1).f_lineno
        if not hasattr(self, "_site_cnt"):
            self._site_cnt = {}; self._site_cache = {}
        k = self._site_cnt.get(ln, 0); self._site_cnt[ln] = k + 1
        if (ln, k) in self._site_cache:
            return self._site_cache[(ln, k)]
        sl = self._slot_new(name)
        self._site_cache[(ln, k)] = sl
        return sl
    def _slot_new(self, name=None):
        self._n += 1
        name = name or f"s{self._n}"
        sem = self.es.enter_context(self.nc.semaphore("sl_" + name + f"_{self._n}"))
        sl = Slot(sem, "sl_" + name + f"_{self._n}")
        self.slots.append(sl)
        return sl
    def sbuf(self, es, name, shape, dt):
        self._n += 1
        t = es.enter_context(self.nc.sbuf_tensor(f"{name}_{self._n}", list(shape), dt))
        return Buf(t, name)
    def psum(self, es, name, shape, dt=F32):
        self._n += 1
        t = es.enter_context(self.nc.psum_tensor(f"{name}_{self._n}", list(shape), dt))
        return Buf(t, name)
    def dram(self, name, shape, dt, kind="Internal"):
        t = self.nc.dram_tensor(name, list(shape), dt, kind=kind)
        return Buf(t.ap(), name)
    def barrier(self):
        toks = [Tok(e.sem, e.count, e.key) for e in self.engs]
        toks += [Tok(s.sem, s.total, s.key) for s in self.slots]
        for e in self.engs:
            for t in toks:
                if t.key != e.key and t.val > 0:
                    e.wait(t)
    def finish(self, bufs):
        for b in bufs:
            for t in b.writers.values():
                self.sp.wait(t)

import math

def build_l0(NCOL=3072):
    nc = bass.Bass("TRN2", target_bir_lowering=False)
    cT = nc.dram_tensor("cT", [128, 32], F32, kind="ExternalInput").ap()
    w = nc.dram_tensor("w", [4096, NCOL], F32, kind="ExternalInput").ap()
    b = nc.dram_tensor("b", [1, NCOL], F32, kind="ExternalInput").ap()
    o = nc.dram_tensor("mod", [1, NCOL], F32, kind="ExternalOutput").ap()
    NB = NCOL // 512
    with ExitStack() as es:
        cx = Ctx(nc, es)
        c_sb = cx.sbuf(es, "c", [128, 32], F32)
        ca = cx.sbuf(es, "ca", [128, 32], F32)
        bs = cx.sbuf(es, "b", [1, NCOL], F32)
        res = cx.sbuf(es, "res", [1, NCOL], F32)
        wb = [cx.sbuf(es, f"w{i}", [128, 32, 512], F32) for i in range(2)]
        ws = [cx.slot() for _ in range(2)]
        ps = [cx.psum(es, f"ps{i}", [1, 512]) for i in range(2)]
        s0 = cx.slot()
        cx.sp.dma(c_sb[:], cT, s0, writes=[c_sb])
        cx.sp.dma(bs[:], b, s0, writes=[bs], cont=True)
        cx.act.op(lambda: nc.scalar.activation(out=ca[:], in_=c_sb[:], func=AF.Silu), reads=[c_sb], writes=[ca])
        wv = w.rearrange("(k p) n -> p k n", p=128)
        for j in range(NB):
            wt = wb[j % 2]
            cx.sp.dma(wt[:], wv[:, :, j * 512:(j + 1) * 512], ws[j % 2], writes=[wt])
            p = ps[j % 2]
            for k in range(32):
                cx.pe.op(lambda: nc.tensor.matmul(p[:], lhsT=ca[:, k:k + 1], rhs=wt[:, k, :], start=(k == 0), stop=(k == 31)),
                         reads=[ca, wt], writes=[p])
            cx.dve.op(lambda: nc.vector.tensor_tensor(out=res[:, j * 512:(j + 1) * 512], in0=p[:], in1=bs[:, j * 512:(j + 1) * 512], op=ALU.add),
                      reads=[p, bs], writes=[res])
        so = cx.slot()
        ob = Buf(o, 'o')
        cx.sp.dma(o, res[:], so, reads=[res], writes=[ob])
        cx.finish([ob])
    return nc


NW = 1538
EPS = 1e-6

def core_cols(i):
    r = lambda a, n: list(range(a, a + n))
    fm = r(6144 + 256 * i, 256) + r(8192 + 256 * i, 256)
    tm = r(128 * i, 128) + r(1024 + 128 * i, 128) + r(2048 + 256 * i, 256) + r(4096 + 256 * i, 256) + r(10240 + 256 * i, 256) + r(12288 + 2 * i, 2)
    return np.array(fm + tm)


def emit_inproj(cx, nc, S, x, wc, modT_d, n1T_d, ident, d_fqk, d_rqkv, d_rgfv, d_ff, d_dbg=None):
    NT = S // 128
    NBLK = S // 512
    with ExitStack() as es:
        Wb = cx.sbuf(es, "Wb", [128, 32, NW], BF16)
        idt = cx.sbuf(es, "ident", [128, 128], F32)
        gp = cx.sbuf(es, "gp", [128, 32], F32)
        sh = cx.sbuf(es, "sh", [128, 32], F32)
        sW = cx.sbuf(es, "sW", [1, NW], F32)
        ones_r = cx.sbuf(es, "ones", [1, 128], F32)
        ss = cx.sbuf(es, "ss", [128, NT], F32)
        rs = cx.sbuf(es, "rs", [128, NT], F32)
        irs = cx.sbuf(es, "irs", [128, NT], F32)
        banks = [cx.psum(es, f"bank{i}", [128, 512]) for i in range(8)]
        s0 = cx.slot()
        cx.sp.dma(idt[:], ident, s0, writes=[idt])
        modT = cx.sbuf(es, "modT1", [128, 192], F32)
        n1T = cx.sbuf(es, "n1T", [128, 32], F32)
        cx.sp.dma(modT[:], modT_d, s0, writes=[modT], cont=True)
        cx.sp.dma(n1T[:], n1T_d, s0, writes=[n1T], cont=True)
        ft = Tok(s0.sem, s0.total, s0.key)
        for dst in (idt, modT, n1T):
            dst.writers[s0.key] = ft
        cx.dve.op(lambda: nc.vector.scalar_tensor_tensor(out=gp[:], in0=modT[:, 32:64], scalar=1.0, in1=n1T[:], op0=ALU.add, op1=ALU.mult), reads=[modT, n1T], writes=[gp])
        cx.dve.op(lambda: nc.vector.tensor_copy(out=sh[:], in_=modT[:, 0:32]), reads=[modT], writes=[sh])
        cx.dve.op(lambda: nc.vector.memset(ones_r[:], 1.0), writes=[ones_r])
        cx.dve.op(lambda: nc.vector.memset(ss[:], 0.0), writes=[ss])
        epsc = cx.sbuf(es, "epsc", [128, 1], F32)
        cx.dve.op(lambda: nc.vector.memset(epsc[:], EPS), writes=[epsc])
        with ExitStack() as es2:
            stg = [cx.sbuf(es2, f"wst{i}", [128, 4, NW], F32) for i in range(2)]
            sl = [cx.slot() for _ in range(2)]
            wv = wc.rearrange("(k p) n -> p k n", p=128)
            nsl = [(0, 512), (512, 512), (1024, 512), (1536, 2)]
            for g in range(8):
                st = stg[g % 2]
                cx.sp.dma(st[:], wv[:, 4 * g:4 * g + 4, :], sl[g % 2], writes=[st])
                for kk in range(4):
                    kc = 4 * g + kk
                    for bi, (a, n) in enumerate(nsl):
                        cx.pe.op(lambda: nc.tensor.matmul(banks[bi][0:1, 0:n], lhsT=sh[:, kc:kc + 1], rhs=st[:, kk, a:a + n],
                                                          start=(kc == 0), stop=(kc == 31)), reads=[sh, st], writes=[banks[bi]])
                    if kk % 2 == 0:
                        cx.dve.op(lambda: nc.vector.tensor_scalar(out=Wb[:, kc, :], in0=st[:, kk, :], scalar1=gp[:, kc:kc + 1], scalar2=None, op0=ALU.mult),
                                  reads=[st, gp], writes=[Wb])
                    else:
                        cx.act.op(lambda: nc.scalar.activation(out=Wb[:, kc, :], in_=st[:, kk, :], func=AF.Copy, scale=gp[:, kc:kc + 1]),
                                  reads=[st, gp], writes=[Wb])
            for bi, (a, n) in enumerate(nsl):
                cx.act.op(lambda: nc.scalar.copy(out=sW[0:1, a:a + n], in_=banks[bi][0:1, 0:n]), reads=[banks[bi]], writes=[sW])
        cx.barrier()
        if d_dbg is not None:
            cx.sp.dma(d_dbg[:, :], sW[:], cx.slot(), reads=[sW], writes=[d_dbg])
        with ExitStack() as es2:
            xt = [cx.sbuf(es2, f"xt{i}", [128, 4096], F32) for i in range(2)]
            xsl = [cx.slot() for _ in range(2)]
            junk = cx.sbuf(es2, "junk", [128, 4096], BF16)
            xT = [cx.sbuf(es2, f"xT{i}", [128, 32, 512], BF16) for i in range(1)]
            rrow = [cx.sbuf(es2, f"rrow{i}", [1, 512], F32) for i in range(2)]
            rbc = [cx.sbuf(es2, f"rbc{i}", [128, 512], F32) for i in range(2)]
            rrow2 = [cx.sbuf(es2, f"rrowb{i}", [1, 512], F32) for i in range(2)]
            ofm = [cx.sbuf(es2, f"ofm{i}", [128, 4, 512], BF16) for i in range(2)]
            otm = [cx.sbuf(es2, f"otm{i}", [128, 1026], F32) for i in range(2)]
            osl = [cx.slot() for _ in range(4)]
            pT = banks[0:2]; pFM = banks[2:4]; pTM = banks[4:7]; pM = banks[7]
            xv = x.rearrange("(n p) d -> n p d", p=128)
            for blk in range(NBLK):
                xTb = xT[0]
                for tt in range(4):
                    ti = blk * 4 + tt
                    xb = xt[ti % 2]
                    cx.sp.dma(xb[:], xv[ti], xsl[ti % 2], writes=[xb])
                    cx.act.op(lambda: nc.scalar.activation(out=junk[:], in_=xb[:], func=AF.Square, accum_out=ss[:, ti:ti + 1]),
                              reads=[xb], writes=[junk, ss])
                    cx.act.op(lambda: nc.scalar.activation(out=irs[:, ti:ti + 1], in_=ss[:, ti:ti + 1], func=AF.Sqrt, bias=epsc[:, 0:1], scale=1.0 / 4096),
                              reads=[ss, epsc], writes=[irs])
                    cx.dve.op(lambda: nc.vector.reciprocal(out=rs[:, ti:ti + 1], in_=irs[:, ti:ti + 1]), reads=[irs], writes=[rs])
                    for g in range(8):
                        pb = pT[g % 2]
                        for kk in range(4):
                            kc = 4 * g + kk
                            cx.pe.op(lambda: nc.tensor.transpose(out=pb[:, kk * 128:(kk + 1) * 128], in_=xb[:, kc * 128:(kc + 1) * 128], identity=idt[:]),
                                     reads=[xb, idt], writes=[pb])
                        cx.dve.op(lambda: nc.vector.tensor_copy(out=xTb[:, 4 * g:4 * g + 4, tt * 128:(tt + 1) * 128],
                                                                in_=pb[:].rearrange("p (a b) -> p a b", a=4)), reads=[pb], writes=[xTb])
                    for (col, row) in ((irs, rrow[blk % 2]), (rs, rrow2[blk % 2])):
                        cx.pe.op(lambda: nc.tensor.transpose(out=pM[0:1, 0:128], in_=col[:, ti:ti + 1], identity=idt[:]), reads=[col, idt], writes=[pM])
                        cx.act.op(lambda: nc.scalar.copy(out=row[0:1, tt * 128:(tt + 1) * 128], in_=pM[0:1, 0:128]), reads=[pM], writes=[row])
                cx.pe.op(lambda: nc.tensor.matmul(pM[:, :], lhsT=ones_r[0:1, :], rhs=rrow2[blk % 2][0:1, :], start=True, stop=True),
                         reads=[ones_r, rrow2[blk % 2]], writes=[pM])
                cx.act.op(lambda: nc.scalar.copy(out=rbc[blk % 2][:], in_=pM[:, :]), reads=[pM], writes=[rbc[blk % 2]])
                of = ofm[blk % 2]
                for j in range(4):
                    pf = pFM[j % 2]
                    for kc in range(32):
                        cx.pe.op(lambda: nc.tensor.matmul(pf[:, :], lhsT=Wb[:, kc, j * 128:(j + 1) * 128], rhs=xTb[:, kc, :], start=(kc == 0), stop=False),
                                 reads=[Wb, xTb], writes=[pf])
                    cx.pe.op(lambda: nc.tensor.matmul(pf[:, :], lhsT=sW[0:1, j * 128:(j + 1) * 128], rhs=rrow[blk % 2][0:1, :], start=False, stop=True),
                             reads=[sW, rrow[blk % 2]], writes=[pf])
                    cx.dve.op(lambda: nc.vector.tensor_tensor(out=of[:, j, :], in0=pf[:, :], in1=rbc[blk % 2][:], op=ALU.mult),
                              reads=[pf, rbc[blk % 2]], writes=[of])
                cx.sp.dma(d_fqk[:, :, blk * 512:(blk + 1) * 512].rearrange("j p t -> p j t"), of[:], osl[blk % 2], reads=[of], writes=[d_fqk])
                for tt in range(4):
                    ti = blk * 4 + tt
                    ot = otm[ti % 2]
                    for bi, (a, n) in enumerate(((512, 512), (1024, 512), (1536, 2))):
                        pt = pTM[bi]
                        for kc in range(32):
                            cx.pe.op(lambda: nc.tensor.matmul(pt[:, 0:n], lhsT=xTb[:, kc, tt * 128:(tt + 1) * 128], rhs=Wb[:, kc, a:a + n], start=(kc == 0), stop=False),
                                     reads=[Wb, xTb], writes=[pt])
                        cx.pe.op(lambda: nc.tensor.matmul(pt[:, 0:n], lhsT=rrow[blk % 2][0:1, tt * 128:(tt + 1) * 128], rhs=sW[0:1, a:a + n], start=False, stop=True),
                                 reads=[sW, rrow[blk % 2]], writes=[pt])
                        cx.act.op(lambda: nc.scalar.activation(out=ot[:, a - 512:a - 512 + n], in_=pt[:, 0:n], func=AF.Copy, scale=rs[:, ti:ti + 1]),
                                  reads=[pt, rs], writes=[ot])
                    sl_ = osl[2 + ti % 2]
                    cx.sp.dma(d_rqkv[ti * 128:(ti + 1) * 128, :], ot[:, 0:512], sl_, reads=[ot], writes=[d_rqkv])
                    cx.sp.dma(d_rgfv[ti * 128:(ti + 1) * 128, :], ot[:, 512:1024], sl_, reads=[ot], writes=[d_rgfv], cont=True)
                    cx.sp.dma(d_ff[ti * 128:(ti + 1) * 128, :], ot[:, 1024:1026], sl_, reads=[ot], writes=[d_ff], cont=True)
        cx.barrier()


def ret_consts(h):
    lg = math.log1p(-2.0 ** (-5.0 - h))
    c = np.arange(128, dtype=np.float64)
    t = np.zeros((128, 8), np.float32)
    t[:, 0] = np.exp((c + 1) * lg)
    t[:, 1] = -np.exp((c + 1) * lg)
    t[:, 2] = 128 ** -0.5
    t[:, 3] = -(128 ** -0.5)
    t[:, 4] = np.exp((127 - c) * lg) * 128 ** -0.5
    t[:, 5] = -np.exp((127 - c) * lg) * 128 ** -0.5
    t[:, 6] = np.exp(128 * lg)
    t[:, 7] = -math.pi
    s = c[:, None]; cc = c[None, :]
    maskT = np.where(cc >= s, np.exp((-s - 1) * lg) * np.ones_like(cc), 0.0).astype(np.float32)
    return t, maskT

def rot_consts():
    half = 64
    inv_freq = (10000.0 ** (-np.arange(half, dtype=np.float32) / half)).astype(np.float32)
    invf2 = np.tile(np.concatenate([inv_freq, inv_freq])[None, :], (128, 1)).astype(np.float32)
    offs = np.tile(np.concatenate([np.zeros(64), np.full(64, math.pi / 2)])[None, :], (128, 1)).astype(np.float32)
    return invf2, offs


def emit_ret(cx, nc, S, d_rqkv, d_rgfv, posT, rc, maskT_d, invf2_d, offs_d, gn_d, ident, d_yret, dbg=None):
    NT = S // 128
    TWO_PI = 2 * math.pi
    with ExitStack() as es:
        idt = cx.sbuf(es, "ident", [128, 128], F32)
        rcs = cx.sbuf(es, "rc", [128, 8], F32)
        mk = cx.sbuf(es, "maskT", [128, 128], F32)
        invf2 = cx.sbuf(es, "invf2", [128, 128], F32)
        offs = cx.sbuf(es, "offs", [128, 128], F32)
        gn = cx.sbuf(es, "gn", [128, 256], F32)
        posi = cx.sbuf(es, "posi", [128, NT], I32)
        posf = cx.sbuf(es, "posf", [128, NT], F32)
        epsc = cx.sbuf(es, "epsc", [128, 1], F32)
        state = cx.sbuf(es, "state", [128, 256], F32)
        state_bf = cx.sbuf(es, "state_bf", [128, 256], BF16)
        banks = [cx.psum(es, f"rbank{i}", [128, 512]) for i in range(8)]
        s0 = cx.slot()
        for i, (dst, src) in enumerate(((idt, ident), (rcs, rc), (mk, maskT_d), (invf2, invf2_d), (offs, offs_d), (gn, gn_d), (posi, posT))):
            cx.sp.dma(dst[:], src, s0, writes=[dst], cont=(i > 0))
        ft = Tok(s0.sem, s0.total, s0.key)
        for dst in (idt, rcs, mk, invf2, offs, gn, posi):
            dst.writers[s0.key] = ft
        cx.dve.op(lambda: nc.vector.tensor_copy(out=posf[:], in_=posi[:]), reads=[posi], writes=[posf])
        cx.dve.op(lambda: nc.vector.memset(epsc[:], 1e-6), writes=[epsc])
        NB = 2
        qkv = [cx.sbuf(es, f"qkv{i}", [128, 512], F32) for i in range(NB)]
        rg = [cx.sbuf(es, f"rg{i}", [128, 256], F32) for i in range(NB)]
        lsl = [cx.slot() for _ in range(NB)]
        ang = cx.sbuf(es, "ang", [128, 128], F32)
        sc = cx.sbuf(es, "sc", [128, 128], F32)
        tq = cx.sbuf(es, "tq", [128, 128], F32)
        ki = cx.sbuf(es, "ki", [128, 128], I32)
        tmp = [cx.sbuf(es, f"rt{i}", [128, 64], F32) for i in range(4)]
        qd = cx.sbuf(es, "qd", [128, 128], F32)
        kr = cx.sbuf(es, "kr", [128, 128], F32)
        kd = cx.sbuf(es, "kd", [128, 128], BF16)
        vb = cx.sbuf(es, "vb", [128, 256], BF16)
        qdT = cx.sbuf(es, "qdT", [128, 128], BF16)
        krT = cx.sbuf(es, "krT", [128, 128], BF16)
        scT = cx.sbuf(es, "scT", [128, 128], BF16)
        o = cx.sbuf(es, "o", [128, 256], F32)
        junk = cx.sbuf(es, "rjunk", [128, 256], F32)
        st = cx.sbuf(es, "st", [128, 8], F32)
        sg = cx.sbuf(es, "sg", [128, 256], F32)
        yo = [cx.sbuf(es, f"yo{i}", [128, 256], BF16) for i in range(2)]
        osl = [cx.slot() for _ in range(2)]
        pqT, pkT, pS, pKV, pO = banks[0], banks[1], banks[2], banks[3], banks[4]
        V = nc.vector
        for n in range(NT):
            t_ = qkv[n % NB]; g_ = rg[n % NB]
            cx.sp.dma(t_[:], d_rqkv[n * 128:(n + 1) * 128, :], lsl[n % NB], reads=[d_rqkv], writes=[t_])
            cx.sp.dma(g_[:], d_rgfv[n * 128:(n + 1) * 128, 0:256], lsl[n % NB], reads=[d_rgfv], writes=[g_], cont=True)
            ft = Tok(lsl[n % NB].sem, lsl[n % NB].total, lsl[n % NB].key)
            t_.writers[ft.key] = ft
            cx.dve.op(lambda: V.scalar_tensor_tensor(out=ang[:], in0=invf2[:], scalar=posf[:, n:n + 1], in1=offs[:], op0=ALU.mult, op1=ALU.add),
                      reads=[invf2, posf, offs], writes=[ang])
            cx.dve.op(lambda: V.tensor_scalar(out=tq[:], in0=ang[:], scalar1=1.0 / TWO_PI, scalar2=None, op0=ALU.mult), reads=[ang], writes=[tq])
            cx.dve.op(lambda: V.tensor_copy(out=ki[:], in_=tq[:]), reads=[tq], writes=[ki])
            cx.dve.op(lambda: V.tensor_copy(out=tq[:], in_=ki[:]), reads=[ki], writes=[tq])
            cx.dve.op(lambda: V.scalar_tensor_tensor(out=ang[:], in0=tq[:], scalar=-6.28125, in1=ang[:], op0=ALU.mult, op1=ALU.add), reads=[tq, ang], writes=[ang])
            cx.dve.op(lambda: V.scalar_tensor_tensor(out=ang[:], in0=tq[:], scalar=-0.0019353071795864769, in1=ang[:], op0=ALU.mult, op1=ALU.add), reads=[tq, ang], writes=[ang])
            cx.dve.op(lambda: V.tensor_scalar(out=ang[:], in0=ang[:], scalar1=3.1415925, scalar2=-3.1415925, op0=ALU.min, op1=ALU.max), reads=[ang], writes=[ang])
            cx.act.op(lambda: nc.scalar.activation(out=sc[:], in_=ang[:], func=AF.Sin), reads=[ang], writes=[sc])
            sinp = sc[:, 0:64]; cosp = sc[:, 64:128]
            for (base, outs) in ((0, ((qd, 0, 1),)), (128, ((kr, 2, 3), (kd, 4, 5)))):
                x1 = t_[:, base:base + 64]; x2 = t_[:, base + 64:base + 128]
                cx.dve.op(lambda: V.tensor_tensor(out=tmp[0][:], in0=x1, in1=cosp, op=ALU.mult), reads=[t_, sc], writes=[tmp[0]])
                cx.dve.op(lambda: V.tensor_tensor(out=tmp[1][:], in0=x2, in1=sinp, op=ALU.mult), reads=[t_, sc], writes=[tmp[1]])
                cx.dve.op(lambda: V.tensor_tensor(out=tmp[1][:], in0=tmp[0][:], in1=tmp[1][:], op=ALU.subtract), reads=[tmp[0], tmp[1]], writes=[tmp[1]])
                cx.dve.op(lambda: V.tensor_tensor(out=tmp[2][:], in0=x1, in1=sinp, op=ALU.mult), reads=[t_, sc], writes=[tmp[2]])
                cx.dve.op(lambda: V.tensor_tensor(out=tmp[3][:], in0=x2, in1=cosp, op=ALU.mult), reads=[t_, sc], writes=[tmp[3]])
                cx.dve.op(lambda: V.tensor_tensor(out=tmp[3][:], in0=tmp[3][:], in1=tmp[2][:], op=ALU.add), reads=[tmp[2], tmp[3]], writes=[tmp[3]])
                for (dst, cp, cn) in outs:
                    cx.dve.op(lambda: V.tensor_scalar(out=dst[:, 0:64], in0=tmp[1][:], scalar1=rcs[:, cp:cp + 1], scalar2=None, op0=ALU.mult), reads=[tmp[1], rcs], writes=[dst])
                    cx.dve.op(lambda: V.tensor_scalar(out=dst[:, 64:128], in0=tmp[3][:], scalar1=rcs[:, cp:cp + 1], scalar2=None, op0=ALU.mult), reads=[tmp[3], rcs], writes=[dst])
            cx.act.op(lambda: nc.scalar.copy(out=vb[:], in_=t_[:, 256:512]), reads=[t_], writes=[vb])
            cx.pe.op(lambda: nc.tensor.transpose(out=pqT[:, 0:128], in_=qd[:], identity=idt[:]), reads=[qd, idt], writes=[pqT])
            cx.act.op(lambda: nc.scalar.copy(out=qdT[:], in_=pqT[:, 0:128]), reads=[pqT], writes=[qdT])
            cx.pe.op(lambda: nc.tensor.transpose(out=pkT[:, 0:128], in_=kr[:], identity=idt[:]), reads=[kr, idt], writes=[pkT])
            cx.act.op(lambda: nc.scalar.copy(out=krT[:], in_=pkT[:, 0:128]), reads=[pkT], writes=[krT])
            cx.pe.op(lambda: nc.tensor.matmul(pS[:, 0:128], lhsT=krT[:], rhs=qdT[:], start=True, stop=True), reads=[krT, qdT], writes=[pS])
            cx.dve.op(lambda: V.tensor_tensor(out=scT[:], in0=pS[:, 0:128], in1=mk[:], op=ALU.mult), reads=[pS, mk], writes=[scT])
            cx.pe.op(lambda: nc.tensor.matmul(pKV[:, 0:256], lhsT=kd[:], rhs=vb[:], start=True, stop=True), reads=[kd, vb], writes=[pKV])
            if n > 0:
                cx.pe.op(lambda: nc.tensor.matmul(pO[:, 0:256], lhsT=qdT[:], rhs=state_bf[:], start=True, stop=False), reads=[qdT, state_bf], writes=[pO])
            cx.pe.op(lambda: nc.tensor.matmul(pO[:, 0:256], lhsT=scT[:], rhs=vb[:], start=(n == 0), stop=True), reads=[scT, vb], writes=[pO])
            if n == 0:
                cx.dve.op(lambda: V.tensor_copy(out=state[:], in_=pKV[:, 0:256]), reads=[pKV], writes=[state])
            else:
                cx.dve.op(lambda: V.scalar_tensor_tensor(out=state[:], in0=state[:], scalar=rcs[:, 6:7], in1=pKV[:, 0:256], op0=ALU.mult, op1=ALU.add),
                          reads=[state, rcs, pKV], writes=[state])
            cx.act.op(lambda: nc.scalar.copy(out=state_bf[:], in_=state[:]), reads=[state], writes=[state_bf])
            cx.dve.op(lambda: V.memset(st[:], 0.0), writes=[st])
            cx.act.op(lambda: nc.scalar.activation(out=o[:], in_=pO[:, 0:256], func=AF.Identity, accum_out=st[:, 0:1]), reads=[pO, st], writes=[o, st])
            cx.act.op(lambda: nc.scalar.activation(out=junk[:], in_=pO[:, 0:256], func=AF.Square, accum_out=st[:, 1:2]), reads=[pO, st], writes=[junk, st])
            cx.dve.op(lambda: V.tensor_scalar(out=st[:, 2:3], in0=st[:, 0:1], scalar1=1.0 / 256, scalar2=None, op0=ALU.mult), reads=[st], writes=[st])
            cx.dve.op(lambda: V.tensor_tensor(out=st[:, 3:4], in0=st[:, 2:3], in1=st[:, 2:3], op=ALU.mult), reads=[st], writes=[st])
            cx.dve.op(lambda: V.scalar_tensor_tensor(out=st[:, 4:5], in0=st[:, 1:2], scalar=1.0 / 256, in1=st[:, 3:4], op0=ALU.mult, op1=ALU.subtract),
                      reads=[st], writes=[st])
            cx.act.op(lambda: nc.scalar.activation(out=st[:, 5:6], in_=st[:, 4:5], func=AF.Sqrt, bias=epsc[:, 0:1], scale=1.0), reads=[st, epsc], writes=[st])
            cx.dve.op(lambda: V.reciprocal(out=st[:, 6:7], in_=st[:, 5:6]), reads=[st], writes=[st])
            cx.dve.op(lambda: V.tensor_scalar(out=o[:], in0=o[:], scalar1=st[:, 2:3], scalar2=st[:, 6:7], op0=ALU.subtract, op1=ALU.mult), reads=[o, st], writes=[o])
            cx.act.op(lambda: nc.scalar.activation(out=sg[:], in_=g_[:], func=AF.Silu), reads=[g_], writes=[sg])
            cx.dve.op(lambda: V.tensor_tensor(out=o[:], in0=o[:], in1=gn[:], op=ALU.mult), reads=[o, gn], writes=[o])
            y_ = yo[n % 2]
            cx.dve.op(lambda: V.tensor_tensor(out=y_[:], in0=o[:], in1=sg[:], op=ALU.mult), reads=[o, sg], writes=[y_])
            cx.sp.dma(d_yret[n * 128:(n + 1) * 128, :], y_[:], osl[n % 2], reads=[y_], writes=[d_yret])
        cx.barrier()

def fox_consts():
    s = np.arange(128)[:, None]; c = np.arange(128)[None, :]
    triU = (s <= c).astype(np.float32)
    ones = np.ones((128, 128), np.float32)
    sel = np.zeros((128, 128), np.float32); sel[64, :] = 1.0
    return triU, ones, sel


def emit_fox(cx, nc, S, d_fqk, d_rgfv, d_ff, bf_d, triU_d, ones_d, sel_d, d_yfox):
    NT = S // 128
    SCALE = 128 ** -0.5
    V = nc.vector
    with ExitStack() as es:
        triU = cx.sbuf(es, "triU", [128, 128], F32)
        triUb = cx.sbuf(es, "triUb", [128, 128], BF16)
        ones = cx.sbuf(es, "ones", [128, 128], F32)
        sel = cx.sbuf(es, "sel", [128, 128], F32)
        bfc = cx.sbuf(es, "bfc", [128, 2], F32)
        nbf = cx.sbuf(es, "nbf", [128, 2], F32)
        one1 = cx.sbuf(es, "one1", [128, 1], F32)
        ff = cx.sbuf(es, "ff", [128, NT, 2], F32)
        lf = cx.sbuf(es, "lf", [128, NT, 2], F32)
        cum = cx.sbuf(es, "cum", [128, NT, 2], F32)
        negc = cx.sbuf(es, "negc", [128, 2, NT], F32)
        tot = cx.sbuf(es, "tot", [128, NT, 2], F32)
        off = cx.sbuf(es, "off", [128, NT, 2], F32)
        rmid = cx.sbuf(es, "rmid", [128, 2, NT], F32)
        banks = [cx.psum(es, f"fbank{i}", [128, 512]) for i in range(8)]
        s0 = cx.slot()
        for i, (dst, src) in enumerate(((triU, triU_d), (ones, ones_d), (sel, sel_d), (bfc, bf_d), (ff, d_ff.t.rearrange("(n p) h -> p n h", p=128)))):
            cx.sp.dma(dst[:], src, s0, reads=([d_ff] if dst is ff else []), writes=[dst], cont=(i > 0))
        ft = Tok(s0.sem, s0.total, s0.key)
        for dst in (triU, ones, sel, bfc, ff):
            dst.writers[s0.key] = ft
        cx.dve.op(lambda: V.memset(one1[:], 1.0), writes=[one1])
        cx.dve.op(lambda: V.tensor_copy(out=triUb[:], in_=triU[:]), reads=[triU], writes=[triUb])
        cx.dve.op(lambda: V.tensor_scalar(out=nbf[:], in0=bfc[:], scalar1=-1.0, scalar2=None, op0=ALU.mult), reads=[bfc], writes=[nbf])
        for h in range(2):
            cx.act.op(lambda: nc.scalar.activation(out=lf[:, :, h], in_=ff[:, :, h], func=AF.Exp, bias=nbf[:, h:h + 1], scale=-1.0), reads=[ff, nbf], writes=[lf])
        cx.act.op(lambda: nc.scalar.activation(out=lf[:], in_=lf[:], func=AF.Ln, bias=one1[:, 0:1], scale=1.0), reads=[lf, one1], writes=[lf])
        cx.dve.op(lambda: V.tensor_scalar(out=lf[:], in0=lf[:], scalar1=-1.0, scalar2=None, op0=ALU.mult), reads=[lf], writes=[lf])
        lf2 = lf[:].rearrange("p n h -> p (n h)"); cum2 = cum[:].rearrange("p n h -> p (n h)"); tot2 = tot[:].rearrange("p n h -> p (n h)")
        W2 = NT * 2
        for a in range(0, W2, 512):
            n_ = min(512, W2 - a)
            cx.pe.op(lambda: nc.tensor.matmul(banks[0][:, 0:n_], lhsT=triU[:], rhs=lf2[:, a:a + n_], start=True, stop=True), reads=[triU, lf], writes=[banks[0]])
            cx.dve.op(lambda: V.tensor_copy(out=cum2[:, a:a + n_], in_=banks[0][:, 0:n_]), reads=[banks[0]], writes=[cum])
            cx.pe.op(lambda: nc.tensor.matmul(banks[1][:, 0:n_], lhsT=ones[:], rhs=lf2[:, a:a + n_], start=True, stop=True), reads=[ones, lf], writes=[banks[1]])
            cx.dve.op(lambda: V.tensor_copy(out=tot2[:, a:a + n_], in_=banks[1][:, 0:n_]), reads=[banks[1]], writes=[tot])
        cx.dve.op(lambda: V.memset(off[:, 0, :], 0.0), writes=[off])
        for n in range(1, NT):
            cx.dve.op(lambda: V.tensor_tensor(out=off[:, n, :], in0=off[:, n - 1, :], in1=tot[:, n - 1, :], op=ALU.add), reads=[off, tot], writes=[off])
        cx.dve.op(lambda: V.tensor_tensor(out=cum[:], in0=cum[:], in1=off[:], op=ALU.add), reads=[cum, off], writes=[cum])
        for h in range(2):
            cx.dve.op(lambda: V.tensor_scalar(out=negc[:, h, :], in0=cum[:, :, h], scalar1=-1.0, scalar2=None, op0=ALU.mult), reads=[cum], writes=[negc])
        for a in range(0, W2, 512):
            n_ = min(512, W2 - a)
            cx.pe.op(lambda: nc.tensor.matmul(banks[0][:, 0:n_], lhsT=sel[:], rhs=cum2[:, a:a + n_], start=True, stop=True), reads=[sel, cum], writes=[banks[0]])
            cx.dve.op(lambda: V.tensor_copy(out=tot2[:, a:a + n_], in_=banks[0][:, 0:n_]), reads=[banks[0]], writes=[tot])
        for h in range(2):
            cx.dve.op(lambda: V.tensor_copy(out=rmid[:, h, :], in_=tot[:, :, h]), reads=[tot], writes=[rmid])
        KT = cx.sbuf(es, "KT", [128, S], BF16)
        QT = cx.sbuf(es, "QT", [128, S], BF16)
        Va = cx.sbuf(es, "Va", [128, NT, 130], BF16)
        vst = [cx.sbuf(es, f"vst{i}", [128, 8, 128], F32) for i in range(2)]
        vsl = [cx.slot() for _ in range(2)]
        BQ = [cx.sbuf(es, f"BQ{i}", [128, NT], F32) for i in range(2)]
        PT = [cx.sbuf(es, f"PT{i}", [128, 4, 128], BF16) for i in range(4)]
        rl = cx.sbuf(es, "rl", [128, 2], F32)
        yo = [cx.sbuf(es, f"fyo{i}", [128, 128], BF16) for i in range(2)]
        ysl = [cx.slot() for _ in range(2)]
        ksl = cx.slot()
        pS = banks[0:4]; pO = banks[4:6]
        cx.dve.op(lambda: V.memset(Va[:, :, 128:130], 1.0), writes=[Va])
        fv = d_rgfv.t.rearrange("(n p) c -> p n c", p=128)
        grp = 0
        for h in range(2):
            cx.sp.dma(QT[:], d_fqk[h], ksl, reads=[d_fqk], writes=[QT])
            cx.sp.dma(KT[:], d_fqk[2 + h], ksl, reads=[d_fqk], writes=[KT], cont=True)
            ft = Tok(ksl.sem, ksl.total, ksl.key); QT.writers[ksl.key] = ft
            for g in range(0, NT, 8):
                ng = min(8, NT - g); i_ = (g // 8) % 2
                cx.sp.dma(vst[i_][:, 0:ng, :], fv[:, g:g + ng, 256 + h * 128:256 + (h + 1) * 128], vsl[i_], reads=[d_rgfv], writes=[vst[i_]])
                cx.dve.op(lambda: V.tensor_copy(out=Va[:, g:g + ng, 0:128], in_=vst[i_][:, 0:ng, :]), reads=[vst[i_]], writes=[Va])
            for Q in range(NT):
                bq = BQ[Q % 2]
                cx.dve.op(lambda: V.tensor_scalar(out=bq[:, 0:Q + 1], in0=negc[:, h, 0:Q + 1], scalar1=rmid[:, h, Q:Q + 1], scalar2=None, op0=ALU.add),
                          reads=[negc, rmid], writes=[bq])
                po = pO[Q % 2]
                for k0 in range(0, Q + 1, 4):
                    nk = min(4, Q + 1 - k0)
                    ps = pS[grp % 4]; pt = PT[grp % 4]; grp += 1
                    for j in range(nk):
                        kap = k0 + j
                        cx.pe.op(lambda: nc.tensor.matmul(ps[:, j * 128:(j + 1) * 128], lhsT=KT[:, kap * 128:(kap + 1) * 128], rhs=QT[:, Q * 128:(Q + 1) * 128],
                                                          start=True, stop=True), reads=[KT, QT], writes=[ps])
                    for j in range(nk):
                        kap = k0 + j
                        cx.act.op(lambda: nc.scalar.activation(out=pt[:, j, :], in_=ps[:, j * 128:(j + 1) * 128], func=AF.Exp, bias=bq[:, kap:kap + 1], scale=SCALE),
                                  reads=[ps, bq], writes=[pt])
                        if kap == Q:
                            cx.dve.op(lambda: V.tensor_tensor(out=pt[:, j, :], in0=pt[:, j, :], in1=triUb[:], op=ALU.mult), reads=[pt, triUb], writes=[pt])
                    for j in range(nk):
                        kap = k0 + j
                        cx.pe.op(lambda: nc.tensor.matmul(po[:, 0:129], lhsT=pt[:, j, :], rhs=Va[:, kap, 0:129], start=(kap == 0), stop=(kap == Q)),
                                 reads=[pt, Va], writes=[po])
                cx.dve.op(lambda: V.reciprocal(out=rl[:, Q % 2:Q % 2 + 1], in_=po[:, 128:129]), reads=[po], writes=[rl])
                y_ = yo[Q % 2]
                cx.dve.op(lambda: V.tensor_scalar(out=y_[:], in0=po[:, 0:128], scalar1=rl[:, Q % 2:Q % 2 + 1], scalar2=None, op0=ALU.mult), reads=[po, rl], writes=[y_])
                cx.sp.dma(d_yfox[Q * 128:(Q + 1) * 128, h * 128:(h + 1) * 128], y_[:], ysl[Q % 2], reads=[y_], writes=[d_yfox])
        cx.barrier()

class TopkScratch:
    def __init__(self, cx, es, nc):
        self.s2 = cx.sbuf(es, "tk_s2", [128, 128], F32)
        self.t1 = None
        self.i1u = None
        self.i1 = cx.sbuf(es, "tk_i1", [128, 16, 16], F32)
        self.cand = cx.sbuf(es, "tk_cand", [128, 256], F32)
        self.cand2 = cx.sbuf(es, "tk_cand2", [128, 256], F32)
        self.ct = cx.sbuf(es, "tk_ct", [128, 16], F32)
        self.pu = cx.sbuf(es, "tk_pu", [128, 16], U32)
        self.au = cx.sbuf(es, "tk_au", [128, 16], U32)
        self.bu = cx.sbuf(es, "tk_bu", [128, 16], U32)
        self.af = cx.sbuf(es, "tk_af", [128, 16], F32)
        self.bf = cx.sbuf(es, "tk_bf", [128, 16], F32)
        self.eq = cx.sbuf(es, "tk_eq", [128, 16, 16], F32)
        self.io16 = cx.sbuf(es, "tk_io16", [128, 16], F32)
        self.ncm = cx.sbuf(es, "tk_ncm", [128, 1], F32)
        self.z = cx.sbuf(es, "tk_z", [128, 2], F32)
        self.ex = cx.sbuf(es, "tk_ex", [128, 16], F32)
        cx.pool.op(lambda: nc.gpsimd.iota(self.io16[:], pattern=[[1, 16]], base=0, channel_multiplier=0, allow_small_or_imprecise_dtypes=True), writes=[self.io16])


def emit_level1(cx, nc, sc, s_sb, chunk):
    V = nc.vector
    cx.dve.op(lambda: V.max(out=sc.t1[:, chunk, 0:8], in_=s_sb[:]), reads=[s_sb], writes=[sc.t1])
    cx.dve.op(lambda: V.max_index(out=sc.i1u[:, chunk, 0:8], in_max=sc.t1[:, chunk, 0:8], in_values=s_sb[:]), reads=[s_sb, sc.t1], writes=[sc.i1u])
    cx.dve.op(lambda: V.match_replace(out=sc.s2[:], in_to_replace=sc.t1[:, chunk, 0:8], in_values=s_sb[:], imm_value=-1e30), reads=[s_sb, sc.t1], writes=[sc.s2])
    cx.dve.op(lambda: V.max(out=sc.t1[:, chunk, 8:16], in_=sc.s2[:]), reads=[sc.s2], writes=[sc.t1])
    cx.dve.op(lambda: V.max_index(out=sc.i1u[:, chunk, 8:16], in_max=sc.t1[:, chunk, 8:16], in_values=sc.s2[:]), reads=[sc.s2, sc.t1], writes=[sc.i1u])


def emit_level2(cx, nc, sc, TI1, TI2, TG):
    V = nc.vector
    cx.dve.op(lambda: V.tensor_copy(out=sc.i1[:], in_=sc.i1u[:]), reads=[sc.i1u], writes=[sc.i1])
    for h in range(8):
        ta = sc.t1[:, 2 * h, :]; tb = sc.t1[:, 2 * h + 1, :]
        cand3 = sc.cand[:].rearrange("p (a b) -> p a b", a=16)
        cx.dve.op(lambda: V.tensor_tensor(out=cand3, in0=ta.unsqueeze(2).to_broadcast([128, 16, 16]), in1=tb.unsqueeze(1).to_broadcast([128, 16, 16]), op=ALU.add),
                  reads=[sc.t1], writes=[sc.cand])
        cx.dve.op(lambda: V.max(out=sc.ct[:, 0:8], in_=sc.cand[:]), reads=[sc.cand], writes=[sc.ct])
        cx.dve.op(lambda: V.max_index(out=sc.pu[:, 0:8], in_max=sc.ct[:, 0:8], in_values=sc.cand[:]), reads=[sc.cand, sc.ct], writes=[sc.pu])
        cx.dve.op(lambda: V.match_replace(out=sc.cand2[:], in_to_replace=sc.ct[:, 0:8], in_values=sc.cand[:], imm_value=-1e30), reads=[sc.cand, sc.ct], writes=[sc.cand2])
        cx.dve.op(lambda: V.max(out=sc.ct[:, 8:16], in_=sc.cand2[:]), reads=[sc.cand2], writes=[sc.ct])
        cx.dve.op(lambda: V.max_index(out=sc.pu[:, 8:16], in_max=sc.ct[:, 8:16], in_values=sc.cand2[:]), reads=[sc.cand2, sc.ct], writes=[sc.pu])
        cx.dve.op(lambda: V.tensor_single_scalar(out=sc.au[:], in_=sc.pu[:], scalar=4, op=ALU.logical_shift_right), reads=[sc.pu], writes=[sc.au])
        cx.dve.op(lambda: V.tensor_single_scalar(out=sc.bu[:], in_=sc.pu[:], scalar=15, op=ALU.bitwise_and), reads=[sc.pu], writes=[sc.bu])
        cx.dve.op(lambda: V.tensor_copy(out=sc.af[:], in_=sc.au[:]), reads=[sc.au], writes=[sc.af])
        cx.dve.op(lambda: V.tensor_copy(out=sc.bf[:], in_=sc.bu[:]), reads=[sc.bu], writes=[sc.bf])
        for (sel, chunk, TI) in ((sc.af, 2 * h, TI1), (sc.bf, 2 * h + 1, TI2)):
            cx.dve.op(lambda: V.tensor_tensor(out=sc.eq[:], in0=sel[:].unsqueeze(2).to_broadcast([128, 16, 16]), in1=sc.io16[:].unsqueeze(1).to_broadcast([128, 16, 16]), op=ALU.is_equal),
                      reads=[sel, sc.io16], writes=[sc.eq])
            cx.dve.op(lambda: V.tensor_tensor(out=sc.eq[:], in0=sc.eq[:], in1=sc.i1[:, chunk, :].unsqueeze(1).to_broadcast([128, 16, 16]), op=ALU.mult),
                      reads=[sc.eq, sc.i1], writes=[sc.eq])
            cx.dve.op(lambda: V.tensor_reduce(out=TI[:, h * 16:(h + 1) * 16], in_=sc.eq[:], axis=AX.X, op=ALU.add), reads=[sc.eq], writes=[TI])
        cx.dve.op(lambda: V.tensor_scalar(out=sc.ncm[:], in0=sc.ct[:, 0:1], scalar1=-1.0, scalar2=None, op0=ALU.mult), reads=[sc.ct], writes=[sc.ncm])
        cx.dve.op(lambda: V.memset(sc.z[:], 0.0), writes=[sc.z])
        cx.act.op(lambda: nc.scalar.activation(out=sc.ex[:], in_=sc.ct[:], func=AF.Exp, bias=sc.ncm[:, 0:1], scale=1.0, accum_out=sc.z[:, 0:1]), reads=[sc.ct, sc.ncm, sc.z], writes=[sc.ex, sc.z])
        cx.dve.op(lambda: V.reciprocal(out=sc.z[:, 1:2], in_=sc.z[:, 0:1]), reads=[sc.z], writes=[sc.z])
        cx.dve.op(lambda: V.tensor_scalar(out=TG[:, h * 16:(h + 1) * 16], in0=sc.ex[:], scalar1=sc.z[:, 1:2], scalar2=None, op0=ALU.mult), reads=[sc.ex, sc.z], writes=[TG])

EPS = 1e-6

def emit_l2(cx, nc, TPC, x_d, yret_d, yfox_d, mod_row, modT_d, n2T_d, gfT_d, fg_row, Wo_d, Wq_d, skT_d, uT_d, v_d, ident_d, d_x2, d_x3, out_d, stage=9, NCH=128):
    V = nc.vector
    NTT = TPC // 128
    TGA = min(TPC, 1024)
    T = min(TPC, 512)
    NTG = T // 128
    with ExitStack() as es:
        banks = [cx.psum(es, f"l2bank{i}", [128, 512]) for i in range(8)]
        idt = cx.sbuf(es, "ident", [128, 128], F32)
        idtb = cx.sbuf(es, "identb", [128, 128], BF16)
        modT = cx.sbuf(es, "modT", [128, 192], F32)
        n2T = cx.sbuf(es, "n2T", [128, 32], F32)
        gfT = cx.sbuf(es, "gfT", [128, 16], F32)
        g2p = cx.sbuf(es, "g2p", [128, 32], F32)
        epsc = cx.sbuf(es, "epsc", [128, 1], F32)
        ssf = cx.sbuf(es, "ssf", [128, NTT], F32)
        rsf = cx.sbuf(es, "rsf", [128, NTT], F32)
        ss2 = cx.sbuf(es, "ss2", [128, NTT, 8], F32)
        rs2 = cx.sbuf(es, "rs2", [128, NTT], F32)
        ss3 = cx.sbuf(es, "ss3", [128, NTT, 8], F32)
        rs3 = cx.sbuf(es, "rs3", [128, NTT], F32)
        iota = cx.sbuf(es, "iota", [128, 128], F32)
        s0 = cx.slot()
        for i, (dst, src) in enumerate(((idt, ident_d), (modT, modT_d), (n2T, n2T_d), (gfT, gfT_d))):
            cx.sp.dma(dst[:], src, s0, writes=[dst], cont=(i > 0))
        ft = Tok(s0.sem, s0.total, s0.key)
        for dst in (idt, modT, n2T, gfT):
            dst.writers[s0.key] = ft
        cx.dve.op(lambda: V.tensor_copy(out=idtb[:], in_=idt[:]), reads=[idt], writes=[idtb])
        cx.dve.op(lambda: V.memset(epsc[:], EPS), writes=[epsc])
        for b_ in (ssf, ss2, ss3):
            cx.dve.op(lambda: V.memset(b_[:], 0.0), writes=[b_])
        cx.pool.op(lambda: nc.gpsimd.iota(iota[:], pattern=[[1, 128]], base=0, channel_multiplier=0, allow_small_or_imprecise_dtypes=True), writes=[iota])
        cx.dve.op(lambda: V.scalar_tensor_tensor(out=g2p[:], in0=modT[:, 128:160], scalar=1.0, in1=n2T[:], op0=ALU.add, op1=ALU.mult), reads=[modT, n2T], writes=[g2p])
        sh2 = modT[:, 96:128]

        with ExitStack() as esA:
            gate1 = cx.sbuf(esA, "gate1", [128, 4096], F32)
            cx.sp.dma(gate1[:], mod_row[0:1, 8192:12288].partition_broadcast(128), cx.slot(), writes=[gate1])
            yT = cx.sbuf(esA, "yT", [128, 32, TGA], BF16)
            yt = [cx.sbuf(esA, f"yt{i}", [128, 4096], BF16) for i in range(2)]
            ysl = [cx.slot() for _ in range(2)]
            junkb = cx.sbuf(esA, "junkb", [128, 2048], BF16)
            wst = [cx.sbuf(esA, f"wost{i}", [128, 8, 512], F32) for i in range(2)]
            wsl = [cx.slot() for _ in range(2)]
            wb = [cx.sbuf(esA, f"wob{i}", [128, 32, 512], BF16) for i in range(2)]
            xp = [cx.sbuf(esA, f"xp{i}", [128, 512], F32) for i in range(3)]
            xsl = [cx.slot() for _ in range(3)]
            osl = [cx.slot() for _ in range(3)]
            junk = cx.sbuf(esA, "junkA", [128, 512], F32)
            pTb = [banks[i][:].bitcast(BF16) for i in (0, 1)]
            wov = Wo_d.rearrange("(k p) n -> p k n", p=128)
            pc = 0
            for ga in range(TPC // TGA):
                for tt in range(TGA // 128):
                    ti = ga * (TGA // 128) + tt
                    yb = yt[ti % 2]
                    cx.sp.dma(yb[:, 0:2048], yret_d[ti * 128:(ti + 1) * 128, :], ysl[ti % 2], reads=[yret_d], writes=[yb])
                    cx.sp.dma(yb[:, 2048:4096], yfox_d[ti * 128:(ti + 1) * 128, :], ysl[ti % 2], reads=[yfox_d], writes=[yb], cont=True)
                    ft = Tok(ysl[ti % 2].sem, ysl[ti % 2].total, ysl[ti % 2].key); yb.writers[ft.key] = ft
                    cx.act.op(lambda: nc.scalar.activation(out=junkb[:], in_=yb[:, 2048:4096], func=AF.Square, accum_out=ssf[:, ti:ti + 1]), reads=[yb, ssf], writes=[junkb, ssf])
                    cx.act.op(lambda: nc.scalar.activation(out=rsf[:, ti:ti + 1], in_=ssf[:, ti:ti + 1], func=AF.Sqrt, bias=epsc[:, 0:1], scale=1.0 / 2048), reads=[ssf, epsc], writes=[rsf])
                    cx.dve.op(lambda: V.reciprocal(out=rsf[:, ti:ti + 1], in_=rsf[:, ti:ti + 1]), reads=[rsf], writes=[rsf])
                    for g in range(4):
                        pb = banks[g % 2]; pv = pTb[g % 2]
                        for kk in range(8):
                            kc = 8 * g + kk
                            cx.pe.op(lambda: nc.tensor.transpose(out=pv[:, kk * 128:(kk + 1) * 128], in_=yb[:, kc * 128:(kc + 1) * 128], identity=idtb[:]), reads=[yb, idtb], writes=[pb])
                        cx.dve.op(lambda: V.tensor_copy(out=yT[:, 8 * g:8 * g + 8, tt * 128:(tt + 1) * 128], in_=pv[:, :].rearrange("p (a b) -> p a b", a=8)), reads=[pb], writes=[yT])
                for nb in range(8):
                    wbb = wb[nb % 2]
                    for q4 in range(4):
                        st = wst[(nb * 4 + q4) % 2]
                        cx.sp.dma(st[:], wov[:, 8 * q4:8 * q4 + 8, nb * 512:(nb + 1) * 512], wsl[(nb * 4 + q4) % 2], writes=[st])
                        if q4 < 2:
                            cx.dve.op(lambda: V.tensor_tensor(out=wbb[:, 8 * q4:8 * q4 + 8, :], in0=st[:], in1=gate1[:, nb * 512:(nb + 1) * 512].unsqueeze(1).to_broadcast([128, 8, 512]), op=ALU.mult),
                                      reads=[st, gate1], writes=[wbb])
                        else:
                            for kk in range(8):
                                kc = 8 * q4 + kk
                                cx.dve.op(lambda: V.scalar_tensor_tensor(out=wbb[:, kc, :], in0=st[:, kk, :], scalar=gfT[:, kc - 16:kc - 15], in1=gate1[:, nb * 512:(nb + 1) * 512], op0=ALU.mult, op1=ALU.mult),
                                          reads=[st, gfT, gate1], writes=[wbb])
                    for tt in range(TGA // 128):
                        ti = ga * (TGA // 128) + tt
                        xb = xp[pc % 3]; sl_x = xsl[pc % 3]; sl_o = osl[pc % 3]; pc += 1
                        cx.sp.dma(xb[:], x_d[ti * 128:(ti + 1) * 128, nb * 512:(nb + 1) * 512], sl_x, writes=[xb])
                        pr = banks[2 + (tt % 2) * 2]; pf = banks[3 + (tt % 2) * 2]
                        for kc in range(16):
                            cx.pe.op(lambda: nc.tensor.matmul(pr[:, :], lhsT=yT[:, kc, tt * 128:(tt + 1) * 128], rhs=wbb[:, kc, :], start=(kc == 0), stop=(kc == 15)), reads=[yT, wbb], writes=[pr])
                        for kc in range(16, 32):
                            cx.pe.op(lambda: nc.tensor.matmul(pf[:, :], lhsT=yT[:, kc, tt * 128:(tt + 1) * 128], rhs=wbb[:, kc, :], start=(kc == 16), stop=(kc == 31)), reads=[yT, wbb], writes=[pf])
                        cx.dve.op(lambda: V.scalar_tensor_tensor(out=xb[:], in0=pf[:, :], scalar=rsf[:, ti:ti + 1], in1=xb[:], op0=ALU.mult, op1=ALU.add), reads=[pf, rsf, xb], writes=[xb])
                        cx.dve.op(lambda: V.tensor_tensor(out=xb[:], in0=xb[:], in1=pr[:, :], op=ALU.add), reads=[xb, pr], writes=[xb])
                        cx.act.op(lambda: nc.scalar.activation(out=junk[:], in_=xb[:], func=AF.Square, accum_out=ss2[:, ti, nb:nb + 1]), reads=[xb, ss2], writes=[junk, ss2])
                        cx.sp.dma(d_x2[ti * 128:(ti + 1) * 128, nb * 512:(nb + 1) * 512], xb[:], sl_o, reads=[xb], writes=[d_x2])
            cx.barrier()
        if stage < 1:
            return
        cx.dve.op(lambda: V.tensor_reduce(out=rs2[:], in_=ss2[:], axis=AX.X, op=ALU.add), reads=[ss2], writes=[rs2])
        cx.act.op(lambda: nc.scalar.activation(out=rs2[:], in_=rs2[:], func=AF.Sqrt, bias=epsc[:, 0:1], scale=1.0 / 4096), reads=[rs2, epsc], writes=[rs2])
        cx.dve.op(lambda: V.reciprocal(out=rs2[:], in_=rs2[:]), reads=[rs2], writes=[rs2])

        NGRP = TPC // T
        d_ub = cx.dram("ub_scr", [NCH, 128, 32, 128], BF16)
        d_vb = cx.dram("vb_scr", [NCH, 128, 4096], BF16)
        G = cx.sbuf(es, "G", [128, 128, T], BF16)
        kI1 = cx.sbuf(es, "kI1", [128, T], F32)
        kI2 = cx.sbuf(es, "kI2", [128, T], F32)
        kG = cx.sbuf(es, "kG", [128, T], F32)
        for gp_ in range(TPC // T):
            t0 = gp_ * T
            cx.new_epoch()
            with ExitStack() as esB:
                h2T = cx.sbuf(esB, "h2T", [128, 32, T], BF16)
                with ExitStack() as esa:
                    x2t = cx.sbuf(esa, "x2t", [128, 4096], F32)
                    xs_ = cx.slot()
                    for tt in range(NTG):
                        ti = gp_ * NTG + tt
                        cx.sp.dma(x2t[:], d_x2[ti * 128:(ti + 1) * 128, :], xs_, reads=[d_x2], writes=[x2t])
                        cx.act.op(lambda: nc.scalar.activation(out=x2t[:], in_=x2t[:], func=AF.Copy, scale=rs2[:, ti:ti + 1]), reads=[x2t, rs2], writes=[x2t])
                        for g in range(8):
                            pb = banks[g % 2]
                            for kk in range(4):
                                kc = 4 * g + kk
                                cx.pe.op(lambda: nc.tensor.transpose(out=pb[:, kk * 128:(kk + 1) * 128], in_=x2t[:, kc * 128:(kc + 1) * 128], identity=idt[:]), reads=[x2t, idt], writes=[pb])
                            for kk in range(4):
                                kc = 4 * g + kk
                                e_ = cx.act if kk % 2 == 0 else cx.dve
                                if kk % 2 == 0:
                                    cx.act.op(lambda: nc.scalar.activation(out=h2T[:, kc, tt * 128:(tt + 1) * 128], in_=pb[:, kk * 128:(kk + 1) * 128], func=AF.Identity, scale=g2p[:, kc:kc + 1], bias=sh2[:, kc:kc + 1]),
                                              reads=[pb, g2p, modT], writes=[h2T])
                                else:
                                    cx.dve.op(lambda: V.tensor_scalar(out=h2T[:, kc, tt * 128:(tt + 1) * 128], in0=pb[:, kk * 128:(kk + 1) * 128], scalar1=g2p[:, kc:kc + 1], scalar2=sh2[:, kc:kc + 1], op0=ALU.mult, op1=ALU.add),
                                              reads=[pb, g2p, modT], writes=[h2T])
                    cx.barrier()
                if stage < 2:
                    return
                with ExitStack() as esb:
                    sc = TopkScratch(cx, esb, nc)
                    scs = [TopkScratch.__new__(TopkScratch) for _ in range(NTG)]
                    t1s = [cx.sbuf(esb, f"t1s{i}", [128, 16, 16], F32) for i in range(NTG)]
                    i1s = [cx.sbuf(esb, f"i1s{i}", [128, 16, 16], U32) for i in range(NTG)]
                    wqst = [cx.sbuf(esb, f"wqst{i}", [128, 8, 128], F32) for i in range(2)]
                    wqsl = [cx.slot() for _ in range(2)]
                    wqb = [cx.sbuf(esb, f"wqb{i}", [128, 32, 128], BF16) for i in range(1)]
                    sksl = [cx.slot() for _ in range(2)]
                    skst = [cx.sbuf(esb, f"skst{i}", [128, 128], F32) for i in range(2)]
                    skb = [cx.sbuf(esb, f"skb{i}", [128, 128], BF16) for i in range(2)]
                    qTc = [cx.sbuf(esb, f"qTc{i}", [128, T], BF16) for i in range(2)]
                    s_sb = [cx.sbuf(esb, f"s_sb{i}", [128, 128], F32) for i in range(2)]
                    TI = [cx.sbuf(esb, n, [128, 128], F32) for n in ("TI1", "TI2", "TG")]
                    wqv = Wq_d
                    cnt = 0
                    for ch in range(16):
                        wq_ = wqb[0]; sks = skst[ch % 2]; sk_ = skb[ch % 2]; qc = qTc[ch % 2]
                        for hf in range(4):
                            st = wqst[hf % 2]
                            cx.sp.dma(st[:], wqv[ch, :, 8 * hf:8 * hf + 8, :], wqsl[hf % 2], writes=[st])
                            if hf % 2 == 0:
                                cx.pool.op(lambda: nc.gpsimd.tensor_copy(out=wq_[:, 8 * hf:8 * hf + 8, :], in_=st[:]), reads=[st], writes=[wq_])
                            else:
                                cx.dve.op(lambda: V.tensor_copy(out=wq_[:, 8 * hf:8 * hf + 8, :], in_=st[:]), reads=[st], writes=[wq_])
                        cx.sp.dma(sks[:], skT_d[ch], sksl[ch % 2], writes=[sks])
                        cx.dve.op(lambda: V.tensor_copy(out=sk_[:], in_=sks[:]), reads=[sks], writes=[sk_])
                        pq = banks[2 + ch % 2]
                        for kc in range(32):
                            cx.pe.op(lambda: nc.tensor.matmul(pq[:, 0:T], lhsT=wq_[:, kc, :], rhs=h2T[:, kc, :], start=(kc == 0), stop=(kc == 31)), reads=[wq_, h2T], writes=[pq])
                        cx.act.op(lambda: nc.scalar.copy(out=qc[:], in_=pq[:, 0:T]), reads=[pq], writes=[qc])
                        for tt in range(NTG):
                            ps = banks[4 + cnt % 2]; ss_ = s_sb[cnt % 2]; cnt += 1
                            cx.pe.op(lambda: nc.tensor.matmul(ps[:, 0:128], lhsT=qc[:, tt * 128:(tt + 1) * 128], rhs=sk_[:], start=True, stop=True), reads=[qc, sk_], writes=[ps])
                            cx.act.op(lambda: nc.scalar.copy(out=ss_[:], in_=ps[:, 0:128]), reads=[ps], writes=[ss_])
                            sc.t1, sc.i1u = t1s[tt], i1s[tt]
                            emit_level1(cx, nc, sc, ss_, ch)
                    for tt in range(NTG):
                        sc.t1, sc.i1u = t1s[tt], i1s[tt]
                        emit_level2(cx, nc, sc, *TI)
                        for (src, dst) in zip(TI, (kI1, kI2, kG)):
                            pt = banks[6]
                            cx.pe.op(lambda: nc.tensor.transpose(out=pt[:, 0:128], in_=src[:], identity=idt[:]), reads=[src, idt], writes=[pt])
                            cx.act.op(lambda: nc.scalar.copy(out=dst[:, tt * 128:(tt + 1) * 128], in_=pt[:, 0:128]), reads=[pt], writes=[dst])
                    cx.barrier()
                if stage < 3:
                    return
                with ExitStack() as ese:
                    At = [cx.sbuf(ese, f"At{i}", [128, 128], BF16) for i in range(4)]
                    Bt = [cx.sbuf(ese, f"Bt{i}", [128, 128], BF16) for i in range(4)]
                    for t4 in range(T // 4):
                        pg = banks[t4 % 2]
                        for j in range(4):
                            t_ = t4 * 4 + j
                            a_ = At[j]; b_ = Bt[j]
                            cx.dve.op(lambda: V.tensor_scalar(out=a_[:], in0=iota[:], scalar1=kI1[:, t_:t_ + 1], scalar2=kG[:, t_:t_ + 1], op0=ALU.is_equal, op1=ALU.mult), reads=[iota, kI1, kG], writes=[a_])
                            cx.dve.op(lambda: V.tensor_scalar(out=b_[:], in0=iota[:], scalar1=kI2[:, t_:t_ + 1], scalar2=None, op0=ALU.is_equal), reads=[iota, kI2], writes=[b_])
                            cx.pe.op(lambda: nc.tensor.matmul(pg[:, j * 128:(j + 1) * 128], lhsT=a_[:], rhs=b_[:], start=True, stop=True), reads=[a_, b_], writes=[pg])
                        cx.act.op(lambda: nc.scalar.copy(out=G[:, :, t4 * 4:t4 * 4 + 4].rearrange("p c t -> p t c"), in_=pg[:, :].rearrange("p (t c) -> p t c", t=4)), reads=[pg], writes=[G])
                    cx.barrier()
                if stage < 4:
                    return
                with ExitStack() as esf:
                    ust = [cx.sbuf(esf, f"ust{i}", [128, 16, 128], F32) for i in range(2)]
                    usl = [cx.slot() for _ in range(2)]
                    ub = [cx.sbuf(esf, f"ub{i}", [128, 32, 128], BF16) for i in range(2)]
                    ga = [cx.sbuf(esf, f"ga{i}", [128, T], BF16) for i in range(2)]
                    ubs = [cx.slot() for _ in range(2)]
                    uv = uT_d
                    hc = 0
                    for c in range(NCH):
                        ub_ = ub[c % 2]
                        if gp_ == 0:
                            for hf in range(2):
                                st = ust[hc % 2]; sl_ = usl[hc % 2]; hc += 1
                                cx.sp.dma(st[:], uv[c, :, 16 * hf:16 * hf + 16, :], sl_, writes=[st])
                                if hf == 0:
                                    cx.pool.op(lambda: nc.gpsimd.tensor_copy(out=ub_[:, 0:16, :], in_=st[:]), reads=[st], writes=[ub_])
                                else:
                                    cx.dve.op(lambda: V.tensor_copy(out=ub_[:, 16:32, :], in_=st[:]), reads=[st], writes=[ub_])
                            if NGRP > 1:
                                cx.sp.dma(d_ub[c], ub_[:], ubs[c % 2], reads=[ub_], writes=[d_ub])
                        else:
                            cx.sp.dma(ub_[:], d_ub[c], ubs[c % 2], reads=[d_ub], writes=[ub_])
                        pa = banks[2 + c % 2]
                        for kc in range(32):
                            cx.pe.op(lambda: nc.tensor.matmul(pa[:, 0:T], lhsT=ub_[:, kc, :], rhs=h2T[:, kc, :], start=(kc == 0), stop=(kc == 31)), reads=[ub_, h2T], writes=[pa])
                        g_ = ga[c % 2]
                        cx.act.op(lambda: nc.scalar.activation(out=g_[:], in_=pa[:, 0:T], func=AF.Gelu_apprx_tanh), reads=[pa], writes=[g_])
                        cx.dve.op(lambda: V.tensor_tensor(out=G[:, c, :], in0=G[:, c, :], in1=g_[:], op=ALU.mult), reads=[G, g_], writes=[G])
                    cx.barrier()
            if stage < 5:
                return
            with ExitStack() as esg:
                gate2 = cx.sbuf(esg, "gate2", [128, 4096], F32)
                cx.sp.dma(gate2[:], mod_row[0:1, 20480:24576].partition_broadcast(128), cx.slot(), writes=[gate2])
                vst = [cx.sbuf(esg, f"vst{i}", [128, 1024], F32) for i in range(3)]
                vsl = [cx.slot() for _ in range(3)]
                vbs = [cx.slot() for _ in range(3)]
                vb = [cx.sbuf(esg, f"vb{i}", [128, 1024], BF16) for i in range(3)]
                xq = [cx.sbuf(esg, f"xq{i}", [128, 512], F32) for i in range(3)]
                xqs = [cx.slot() for _ in range(3)]
                xos = [cx.slot() for _ in range(3)]
                junk = cx.sbuf(esg, "junkg", [128, 512], F32)
                vc = 0; pc = 0
                for p in range(4):
                    for c in range(NCH):
                        st = vst[vc % 3]; vb_ = vb[vc % 3]; sl_ = vsl[vc % 3]; sl2_ = vbs[vc % 3]; vc += 1
                        if gp_ == 0:
                            cx.sp.dma(st[:], v_d[c, :, p * 1024:(p + 1) * 1024], sl_, writes=[st])
                            if c % 2 == 0:
                                cx.pool.op(lambda: nc.gpsimd.tensor_tensor(out=vb_[:], in0=st[:], in1=gate2[:, p * 1024:(p + 1) * 1024], op=ALU.mult), reads=[st, gate2], writes=[vb_])
                            else:
                                cx.dve.op(lambda: V.tensor_tensor(out=vb_[:], in0=st[:], in1=gate2[:, p * 1024:(p + 1) * 1024], op=ALU.mult), reads=[st, gate2], writes=[vb_])
                            if NGRP > 1:
                                cx.sp.dma(d_vb[c, :, p * 1024:(p + 1) * 1024], vb_[:], sl2_, reads=[vb_], writes=[d_vb])
                        else:
                            cx.sp.dma(vb_[:], d_vb[c, :, p * 1024:(p + 1) * 1024], sl2_, reads=[d_vb], writes=[vb_])
                        for ts in range(NTG):
                            for j in range(2):
                                po = banks[ts * 2 + j]
                                cx.pe.op(lambda: nc.tensor.matmul(po[:, :], lhsT=G[:, c, ts * 128:(ts + 1) * 128], rhs=vb_[:, j * 512:(j + 1) * 512], start=(c == 0), stop=(c == NCH - 1)), reads=[G, vb_], writes=[po])
                    for ts in range(NTG):
                        ti = gp_ * NTG + ts
                        for j in range(2):
                            nb = p * 2 + j
                            po = banks[ts * 2 + j]
                            xb = xq[pc % 3]; s1_ = xqs[pc % 3]; s2_ = xos[pc % 3]; pc += 1
                            cx.sp.dma(xb[:], d_x2[ti * 128:(ti + 1) * 128, nb * 512:(nb + 1) * 512], s1_, reads=[d_x2], writes=[xb])
                            cx.dve.op(lambda: V.tensor_tensor(out=xb[:], in0=xb[:], in1=po[:, :], op=ALU.add), reads=[xb, po], writes=[xb])
                            cx.act.op(lambda: nc.scalar.activation(out=junk[:], in_=xb[:], func=AF.Square, accum_out=ss3[:, ti, nb:nb + 1]), reads=[xb, ss3], writes=[junk, ss3])
                            cx.sp.dma(d_x3[ti * 128:(ti + 1) * 128, nb * 512:(nb + 1) * 512], xb[:], s2_, reads=[xb], writes=[d_x3])
                cx.barrier()
        if stage < 6:
            return
        cx.dve.op(lambda: V.tensor_reduce(out=rs3[:], in_=ss3[:], axis=AX.X, op=ALU.add), reads=[ss3], writes=[rs3])
        cx.act.op(lambda: nc.scalar.activation(out=rs3[:], in_=rs3[:], func=AF.Sqrt, bias=epsc[:, 0:1], scale=1.0 / 4096), reads=[rs3, epsc], writes=[rs3])
        cx.dve.op(lambda: V.reciprocal(out=rs3[:], in_=rs3[:]), reads=[rs3], writes=[rs3])
        with ExitStack() as esh:
            fg = cx.sbuf(esh, "fg", [128, 4096], F32)
            cx.sp.dma(fg[:], fg_row[0:1, :].partition_broadcast(128), cx.slot(), writes=[fg])
            xt = [cx.sbuf(esh, f"x3t{i}", [128, 4096], F32) for i in range(2)]
            sl1 = [cx.slot() for _ in range(2)]; sl2 = [cx.slot() for _ in range(2)]
            for ti in range(NTT):
                xb = xt[ti % 2]
                cx.sp.dma(xb[:], d_x3[ti * 128:(ti + 1) * 128, :], sl1[ti % 2], reads=[d_x3], writes=[xb])
                cx.dve.op(lambda: V.scalar_tensor_tensor(out=xb[:], in0=xb[:], scalar=rs3[:, ti:ti + 1], in1=fg[:], op0=ALU.mult, op1=ALU.mult), reads=[xb, rs3, fg], writes=[xb])
                cx.sp.dma(out_d[ti * 128:(ti + 1) * 128, :], xb[:], sl2[ti % 2], reads=[xb], writes=[out_d])
            cx.barrier()


S_FULL = 16384

def build_l1(S):
    nc = bass.Bass("TRN2", target_bir_lowering=False)
    di = lambda n, s, d=F32: nc.dram_tensor(n, s, d, kind="ExternalInput").ap()
    x = di("x", [S, 4096]); wc = di("wc", [4096, NW]); modT = di("modT", [128, 192]); n1T = di("n1T", [128, 32]); ident = di("ident", [128, 128])
    posT = di("posT", [128, S // 128], I32); rc = di("rc", [128, 8]); maskT = di("maskT", [128, 128]); invf2 = di("invf2", [128, 128]); offs = di("offs", [128, 128])
    gn = di("gn", [128, 256]); bf = di("bf", [128, 2]); triU = di("triU", [128, 128]); ones = di("ones", [128, 128]); sel = di("sel", [128, 128])
    with ExitStack() as es:
        cx = Ctx(nc, es)
        d_fqk = cx.dram("fqk", [4, 128, S], BF16); d_rqkv = cx.dram("rqkv", [S, 512], F32); d_rgfv = cx.dram("rgfv", [S, 512], F32); d_ff = cx.dram("ffl", [S, 2], F32)
        d_yret = cx.dram("yret", [S, 256], BF16, kind="ExternalOutput"); d_yfox = cx.dram("yfox", [S, 256], BF16, kind="ExternalOutput")
        emit_inproj(cx, nc, S, x, wc, modT, n1T, ident, d_fqk, d_rqkv, d_rgfv, d_ff)
        emit_ret(cx, nc, S, d_rqkv, d_rgfv, posT, rc, maskT, invf2, offs, gn, ident, d_yret)
        emit_fox(cx, nc, S, d_fqk, d_rgfv, d_ff, bf, triU, ones, sel, d_yfox)
        cx.barrier()
        cx.finish([d_yret, d_yfox])
    return nc


def build_l2(TPC):
    nc = bass.Bass("TRN2", target_bir_lowering=False)
    di = lambda n, s, d=F32: nc.dram_tensor(n, s, d, kind="ExternalInput").ap()
    with ExitStack() as es:
        cx = Ctx(nc, es)
        x = di("x", [TPC, 4096]); yret = Buf(di("yret", [TPC, 2048], BF16)); yfox = Buf(di("yfox", [TPC, 2048], BF16))
        mod_row = di("mod_row", [1, 24576]); modT = di("modT", [128, 192]); n2T = di("n2T", [128, 32]); gfT = di("gfT", [128, 16]); fg_row = di("fg_row", [1, 4096])
        Wo = di("Wo", [4096, 4096]); Wq = di("Wq", [16, 128, 32, 128]); skT = di("skT", [16, 128, 128]); uT = di("uT", [128, 128, 32, 128]); v = di("v", [128, 128, 4096]); ident = di("ident", [128, 128])
        d_x2 = cx.dram("x2", [TPC, 4096], F32); d_x3 = cx.dram("x3", [TPC, 4096], F32)
        out = cx.dram("out", [TPC, 4096], F32, kind="ExternalOutput")
        emit_l2(cx, nc, TPC, x, yret, yfox, mod_row, modT, n2T, gfT, fg_row, Wo, Wq, skT, uT, v, ident, d_x2, d_x3, out)
        cx.barrier()
        cx.finish([out])
    return nc


def kernel(x, c, positions, w_ada, b_ada, norm1_g, w_in, b_forget, ret_gn_g, fox_norm_g, w_out, norm2_g, w_peer_q, peer_sub_keys, peer_u, peer_v, final_g):
    f32 = lambda a: np.ascontiguousarray(np.asarray(a, dtype=np.float32))
    x = f32(x); S = x.shape[1]; TPC = S // 8
    col = lambda v_: np.ascontiguousarray(np.asarray(v_, np.float32).reshape(-1, 128).T)
    cores = list(range(8))
    w_ada0 = np.asarray(w_ada, np.float32)[0]; b_ada0 = np.asarray(b_ada, np.float32)[0]
    nc0 = build_l0()
    maps = [{"cT": col(np.asarray(c, np.float32)[0]), "w": np.ascontiguousarray(w_ada0[:, i * 3072:(i + 1) * 3072]), "b": np.ascontiguousarray(b_ada0[None, i * 3072:(i + 1) * 3072])} for i in cores]
    r0 = run_bass_kernel_spmd(nc0, maps, core_ids=cores)
    mod = np.concatenate([np.asarray(r["mod"])[0] for r in r0.results])
    modT = col(mod)
    ident = np.eye(128, dtype=np.float32)
    w_in0 = np.asarray(w_in, np.float32)[0]
    pos = np.asarray(positions, np.int32)[0]
    invf2, offs = rot_consts(); triU, ones, sel = fox_consts()
    nc1 = build_l1(S)
    maps = []
    for i in cores:
        rc, maskT = ret_consts(i)
        maps.append({"x": x[0], "wc": np.ascontiguousarray(w_in0[:, core_cols(i)]), "modT": modT, "n1T": col(np.asarray(norm1_g)[0]), "ident": ident,
                     "posT": np.ascontiguousarray(pos.reshape(-1, 128).T), "rc": rc, "maskT": maskT, "invf2": invf2, "offs": offs,
                     "gn": np.ascontiguousarray(np.tile(np.asarray(ret_gn_g, np.float32)[0][None, i * 256:(i + 1) * 256], (128, 1))),
                     "bf": np.ascontiguousarray(np.tile(np.asarray(b_forget, np.float32)[0][None, 2 * i:2 * i + 2], (128, 1))), "triU": triU, "ones": ones, "sel": sel})
    r1 = run_bass_kernel_spmd(nc1, maps, core_ids=cores)
    yret = np.concatenate([np.asarray(r["yret"]) for r in r1.results], axis=1)
    yfox = np.concatenate([np.asarray(r["yfox"]) for r in r1.results], axis=1)
    u = np.asarray(peer_u, np.float32)[0]; v = np.asarray(peer_v, np.float32)[0]
    uT = np.ascontiguousarray(u.reshape(128, 128, 32, 128).transpose(1, 3, 2, 0))
    vr = np.ascontiguousarray(v.reshape(128, 128, 4096).transpose(1, 0, 2))
    skT = np.ascontiguousarray(np.asarray(peer_sub_keys, np.float32)[0].reshape(16, 128, 128).transpose(0, 2, 1))
    wm = {"mod_row": np.ascontiguousarray(mod[None, :]), "modT": modT, "n2T": col(np.asarray(norm2_g)[0]), "gfT": col(np.asarray(fox_norm_g)[0]),
          "fg_row": np.ascontiguousarray(np.asarray(final_g, np.float32)[None, :]), "Wo": f32(np.asarray(w_out)[0]), "Wq": np.ascontiguousarray(np.asarray(w_peer_q, np.float32)[0].reshape(32, 128, 16, 128).transpose(2, 1, 0, 3)),
          "skT": skT, "uT": uT, "v": vr, "ident": ident}
    nc2 = build_l2(TPC)
    maps = []
    for i in cores:
        sl = slice(i * TPC, (i + 1) * TPC)
        m = dict(wm); m.update({"x": np.ascontiguousarray(x[0, sl]), "yret": np.ascontiguousarray(yret[sl]), "yfox": np.ascontiguousarray(yfox[sl])})
        maps.append(m)
    r2 = run_bass_kernel_spmd(nc2, maps, core_ids=cores)
    out = np.concatenate([np.asarray(r["out"]) for r in r2.results], axis=0)
    return out.reshape(1, S, 4096).astype(np.float32)
```

```python
import numpy as np
from contextlib import ExitStack
import concourse.bass as bass
import concourse.mybir as mybir
from concourse.bass_utils import run_bass_kernel_spmd

F32 = mybir.dt.float32
BF16 = mybir.dt.bfloat16
I32 = mybir.dt.int32
U32 = mybir.dt.uint32
ALU = mybir.AluOpType
AF = mybir.ActivationFunctionType
AX = mybir.AxisListType


class Tok:
    __slots__ = ("sem", "val", "key")
    def __init__(self, sem, val, key):
        self.sem = sem; self.val = val; self.key = key


class Buf:
    def __init__(self, t, name=""):
        self.t = t
        self.name = name
        self.writers = {}
        self.readers = {}
    def __getitem__(self, idx):
        return self.t[idx]


class Slot:
    def __init__(self, sem, key):
        self.sem = sem; self.total = 0; self.key = key


class Eng:
    def __init__(self, h, sem, key, is_pe=False):
        self.h = h; self.sem = sem; self.count = 0; self.key = key
        self.seen = {}
        self.is_pe = is_pe
    def wait(self, tok, same_ok=False):
        if tok is None:
            return
        if tok.key == self.key and (self.is_pe or same_ok):
            return
        if self.seen.get(tok.key, 0) >= tok.val:
            return
        self.h.wait_ge(tok.sem, tok.val)
        self.seen[tok.key] = tok.val
    def _deps(self, reads, writes):
        for b in reads:
            for t in b.writers.values():
                self.wait(t)
        for b in writes:
            for t in b.writers.values():
                self.wait(t, same_ok=True)
            for t in b.readers.values():
                self.wait(t, same_ok=True)
    def _commit(self, tok, reads, writes):
        for b in reads:
            b.readers[tok.key] = tok
        for b in writes:
            b.writers[tok.key] = tok
            b.readers = {}
    def op(self, fn, reads=(), writes=()):
        self._deps(reads, writes)
        inst = fn()
        self.count += 1
        inst.then_inc(self.sem, 1)
        tok = Tok(self.sem, self.count, self.key)
        self._commit(tok, reads, writes)
        return tok
    def dma(self, out, in_, slot, reads=(), writes=(), cont=False, **kw):
        self._deps(reads, writes)
        if not cont:
            self.wait(Tok(slot.sem, slot.total, slot.key))
        inst = self.h.dma_start(out=out, in_=in_, **kw)
        slot.total += 16
        inst.then_inc(slot.sem, 16)
        tok = Tok(slot.sem, slot.total, slot.key)
        self._commit(tok, reads, writes)
        return tok


class Ctx:
    def __init__(self, nc, es):
        self.nc = nc; self.es = es
        self._n = 0
        def mk(h, name, is_pe=False):
            sem = es.enter_context(nc.semaphore("e_" + name))
            return Eng(h, sem, "e_" + name, is_pe)
        self.pe = mk(nc.tensor, "pe", True)
        self.act = mk(nc.scalar, "act")
        self.dve = mk(nc.vector, "dve")
        self.pool = mk(nc.gpsimd, "pool")
        self.sp = mk(nc.sync, "sp")
        self.engs = [self.pe, self.act, self.dve, self.pool, self.sp]
        self.slots = []
    def new_epoch(self):
        self._site_cnt = {}
    def slot(self, name=None):
        import sys as _sys
        ln = _sys._getframe(1).f_lineno
        if not hasattr(self, "_site_cnt"):
            self._site_cnt = {}; self._site_cache = {}
        k = self._site_cnt.get(ln, 0); self._site_cnt[ln] = k + 1
        if (ln, k) in self._site_cache:
            return self._site_cache[(ln, k)]
        sl = self._slot_new(name)
        self._site_cache[(ln, k)] = sl
        return sl
    def _slot_new(self, name=None):
        self._n += 1
        name = name or f"s{self._n}"
        sem = self.es.enter_context(self.nc.semaphore("sl_" + name + f"_{self._n}"))
        sl = Slot(sem, "sl_" + name + f"_{self._n}")
        self.slots.append(sl)
        return sl
    def sbuf(self, es, name, shape, dt):
        self._n += 1
        t = es.enter_context(self.nc.sbuf_tensor(f"{name}_{self._n}", list(shape), dt))
        return Buf(t, name)
    def psum(self, es, name, shape, dt=F32):
        self._n += 1
        t = es.enter_context(self.nc.psum_tensor(f"{name}_{self._n}", list(shape), dt))
        return Buf(t, name)
    def dram(self, name, shape, dt, kind="Internal"):
        t = self.nc.dram_tensor(name, list(shape), dt, kind=kind)
        return Buf(t.ap(), name)
    def barrier(self):
        toks = [Tok(e.sem, e.count, e.key) for e in self.engs]
        toks += [Tok(s.sem, s.total, s.key) for s in self.slots]
        for e in self.engs:
            for t in toks:
                if t.key != e.key and t.val > 0:
                    e.wait(t)
    def finish(self, bufs):
        for b in bufs:
            for t in b.writers.values():
                self.sp.wait(t)

import math

def build_l0(NCOL=3072):
    nc = bass.Bass("TRN2", target_bir_lowering=False)
    cT = nc.dram_tensor("cT", [128, 32], F32, kind="ExternalInput").ap()
    w = nc.dram_tensor("w", [4096, NCOL], F32, kind="ExternalInput").ap()
    b = nc.dram_tensor("b", [1, NCOL], F32, kind="ExternalInput").ap()
    o = nc.dram_tensor("mod", [1, NCOL], F32, kind="ExternalOutput").ap()
    NB = NCOL // 512
    with ExitStack() as es:
        cx = Ctx(nc, es)
        c_sb = cx.sbuf(es, "c", [128, 32], F32)
        ca = cx.sbuf(es, "ca", [128, 32], F32)
        bs = cx.sbuf(es, "b", [1, NCOL], F32)
        res = cx.sbuf(es, "res", [1, NCOL], F32)
        wb = [cx.sbuf(es, f"w{i}", [128, 32, 512], F32) for i in range(2)]
        ws = [cx.slot() for _ in range(2)]
        ps = [cx.psum(es, f"ps{i}", [1, 512]) for i in range(2)]
        s0 = cx.slot()
        cx.sp.dma(c_sb[:], cT, s0, writes=[c_sb])
        cx.sp.dma(bs[:], b, s0, writes=[bs], cont=True)
        cx.act.op(lambda: nc.scalar.activation(out=ca[:], in_=c_sb[:], func=AF.Silu), reads=[c_sb], writes=[ca])
        wv = w.rearrange("(k p) n -> p k n", p=128)
        for j in range(NB):
            wt = wb[j % 2]
            cx.sp.dma(wt[:], wv[:, :, j * 512:(j + 1) * 512], ws[j % 2], writes=[wt])
            p = ps[j % 2]
            for k in range(32):
                cx.pe.op(lambda: nc.tensor.matmul(p[:], lhsT=ca[:, k:k + 1], rhs=wt[:, k, :], start=(k == 0), stop=(k == 31)),
                         reads=[ca, wt], writes=[p])
            cx.dve.op(lambda: nc.vector.tensor_tensor(out=res[:, j * 512:(j + 1) * 512], in0=p[:], in1=bs[:, j * 512:(j + 1) * 512], op=ALU.add),
                      reads=[p, bs], writes=[res])
        so = cx.slot()
        ob = Buf(o, 'o')
        cx.sp.dma(o, res[:], so, reads=[res], writes=[ob])
        cx.finish([ob])
    return nc


NW = 1538
EPS = 1e-6

def core_cols(i):
    r = lambda a, n: list(range(a, a + n))
    fm = r(6144 + 256 * i, 256) + r(8192 + 256 * i, 256)
    tm = r(128 * i, 128) + r(1024 + 128 * i, 128) + r(2048 + 256 * i, 256) + r(4096 + 256 * i, 256) + r(10240 + 256 * i, 256) + r(12288 + 2 * i, 2)
    return np.array(fm + tm)


def emit_inproj(cx, nc, S, x, wc, modT_d, n1T_d, ident, d_fqk, d_rqkv, d_rgfv, d_ff, d_dbg=None):
    NT = S // 128
    NBLK = S // 512
    with ExitStack() as es:
        Wb = cx.sbuf(es, "Wb", [128, 32, NW], BF16)
        idt = cx.sbuf(es, "ident", [128, 128], F32)
        gp = cx.sbuf(es, "gp", [128, 32], F32)
        sh = cx.sbuf(es, "sh", [128, 32], F32)
        sW = cx.sbuf(es, "sW", [1, NW], F32)
        ones_r = cx.sbuf(es, "ones", [1, 128], F32)
        ss = cx.sbuf(es, "ss", [128, NT], F32)
        rs = cx.sbuf(es, "rs", [128, NT], F32)
        irs = cx.sbuf(es, "irs", [128, NT], F32)
        banks = [cx.psum(es, f"bank{i}", [128, 512]) for i in range(8)]
        s0 = cx.slot()
        cx.sp.dma(idt[:], ident, s0, writes=[idt])
        modT = cx.sbuf(es, "modT1", [128, 192], F32)
        n1T = cx.sbuf(es, "n1T", [128, 32], F32)
        cx.sp.dma(modT[:], modT_d, s0, writes=[modT], cont=True)
        cx.sp.dma(n1T[:], n1T_d, s0, writes=[n1T], cont=True)
        ft = Tok(s0.sem, s0.total, s0.key)
        for dst in (idt, modT, n1T):
            dst.writers[s0.key] = ft
        cx.dve.op(lambda: nc.vector.scalar_tensor_tensor(out=gp[:], in0=modT[:, 32:64], scalar=1.0, in1=n1T[:], op0=ALU.add, op1=ALU.mult), reads=[modT, n1T], writes=[gp])
        cx.dve.op(lambda: nc.vector.tensor_copy(out=sh[:], in_=modT[:, 0:32]), reads=[modT], writes=[sh])
        cx.dve.op(lambda: nc.vector.memset(ones_r[:], 1.0), writes=[ones_r])
        cx.dve.op(lambda: nc.vector.memset(ss[:], 0.0), writes=[ss])
        epsc = cx.sbuf(es, "epsc", [128, 1], F32)
        cx.dve.op(lambda: nc.vector.memset(epsc[:], EPS), writes=[epsc])
        with ExitStack() as es2:
            stg = [cx.sbuf(es2, f"wst{i}", [128, 4, NW], F32) for i in range(2)]
            sl = [cx.slot() for _ in range(2)]
            wv = wc.rearrange("(k p) n -> p k n", p=128)
            nsl = [(0, 512), (512, 512), (1024, 512), (1536, 2)]
            for g in range(8):
                st = stg[g % 2]
                cx.sp.dma(st[:], wv[:, 4 * g:4 * g + 4, :], sl[g % 2], writes=[st])
                for kk in range(4):
                    kc = 4 * g + kk
                    for bi, (a, n) in enumerate(nsl):
                        cx.pe.op(lambda: nc.tensor.matmul(banks[bi][0:1, 0:n], lhsT=sh[:, kc:kc + 1], rhs=st[:, kk, a:a + n],
                                                          start=(kc == 0), stop=(kc == 31)), reads=[sh, st], writes=[banks[bi]])
                    if kk % 2 == 0:
                        cx.dve.op(lambda: nc.vector.tensor_scalar(out=Wb[:, kc, :], in0=st[:, kk, :], scalar1=gp[:, kc:kc + 1], scalar2=None, op0=ALU.mult),
                                  reads=[st, gp], writes=[Wb])
                    else:
                        cx.act.op(lambda: nc.scalar.activation(out=Wb[:, kc, :], in_=st[:, kk, :], func=AF.Copy, scale=gp[:, kc:kc + 1]),
                                  reads=[st, gp], writes=[Wb])
            for bi, (a, n) in enumerate(nsl):
                cx.act.op(lambda: nc.scalar.copy(out=sW[0:1, a:a + n], in_=banks[bi][0:1, 0:n]), reads=[banks[bi]], writes=[sW])
        cx.barrier()
        if d_dbg is not None:
            cx.sp.dma(d_dbg[:, :], sW[:], cx.slot(), reads=[sW], writes=[d_dbg])
        with ExitStack() as es2:
            xt = [cx.sbuf(es2, f"xt{i}", [128, 4096], F32) for i in range(2)]
            xsl = [cx.slot() for _ in range(2)]
            junk = cx.sbuf(es2, "junk", [128, 4096], BF16)
            xT = [cx.sbuf(es2, f"xT{i}", [128, 32, 512], BF16) for i in range(1)]
            rrow = [cx.sbuf(es2, f"rrow{i}", [1, 512], F32) for i in range(2)]
            rbc = [cx.sbuf(es2, f"rbc{i}", [128, 512], F32) for i in range(2)]
            rrow2 = [cx.sbuf(es2, f"rrowb{i}", [1, 512], F32) for i in range(2)]
            ofm = [cx.sbuf(es2, f"ofm{i}", [128, 4, 512], BF16) for i in range(2)]
            otm = [cx.sbuf(es2, f"otm{i}", [128, 1026], F32) for i in range(2)]
            osl = [cx.slot() for _ in range(4)]
            pT = banks[0:2]; pFM = banks[2:4]; pTM = banks[4:7]; pM = banks[7]
            xv = x.rearrange("(n p) d -> n p d", p=128)
            for blk in range(NBLK):
                xTb = xT[0]
                for tt in range(4):
                    ti = blk * 4 + tt
                    xb = xt[ti % 2]
                    cx.sp.dma(xb[:], xv[ti], xsl[ti % 2], writes=[xb])
                    cx.act.op(lambda: nc.scalar.activation(out=junk[:], in_=xb[:], func=AF.Square, accum_out=ss[:, ti:ti + 1]),
                              reads=[xb], writes=[junk, ss])
                    cx.act.op(lambda: nc.scalar.activation(out=irs[:, ti:ti + 1], in_=ss[:, ti:ti + 1], func=AF.Sqrt, bias=epsc[:, 0:1], scale=1.0 / 4096),
                              reads=[ss, epsc], writes=[irs])
                    cx.dve.op(lambda: nc.vector.reciprocal(out=rs[:, ti:ti + 1], in_=irs[:, ti:ti + 1]), reads=[irs], writes=[rs])
                    for g in range(8):
                        pb = pT[g % 2]
                        for kk in range(4):
                            kc = 4 * g + kk
                            cx.pe.op(lambda: nc.tensor.transpose(out=pb[:, kk * 128:(kk + 1) * 128], in_=xb[:, kc * 128:(kc + 1) * 128], identity=idt[:]),
                                     reads=[xb, idt], writes=[pb])
                        cx.dve.op(lambda: nc.vector.tensor_copy(out=xTb[:, 4 * g:4 * g + 4, tt * 128:(tt + 1) * 128],
                                                                in_=pb[:].rearrange("p (a b) -> p a b", a=4)), reads=[pb], writes=[xTb])
                    for (col, row) in ((irs, rrow[blk % 2]), (rs, rrow2[blk % 2])):
                        cx.pe.op(lambda: nc.tensor.transpose(out=pM[0:1, 0:128], in_=col[:, ti:ti + 1], identity=idt[:]), reads=[col, idt], writes=[pM])
                        cx.act.op(lambda: nc.scalar.copy(out=row[0:1, tt * 128:(tt + 1) * 128], in_=pM[0:1, 0:128]), reads=[pM], writes=[row])
                cx.pe.op(lambda: nc.tensor.matmul(pM[:, :], lhsT=ones_r[0:1, :], rhs=rrow2[blk % 2][0:1, :], start=True, stop=True),
                         reads=[ones_r, rrow2[blk % 2]], writes=[pM])
                cx.act.op(lambda: nc.scalar.copy(out=rbc[blk % 2][:], in_=pM[:, :]), reads=[pM], writes=[rbc[blk % 2]])
                of = ofm[blk % 2]
                for j in range(4):
                    pf = pFM[j % 2]
                    for kc in range(32):
                        cx.pe.op(lambda: nc.tensor.matmul(pf[:, :], lhsT=Wb[:, kc, j * 128:(j + 1) * 128], rhs=xTb[:, kc, :], start=(kc == 0), stop=False),
                                 reads=[Wb, xTb], writes=[pf])
                    cx.pe.op(lambda: nc.tensor.matmul(pf[:, :], lhsT=sW[0:1, j * 128:(j + 1) * 128], rhs=rrow[blk % 2][0:1, :], start=False, stop=True),
                             reads=[sW, rrow[blk % 2]], writes=[pf])
                    cx.dve.op(lambda: nc.vector.tensor_tensor(out=of[:, j, :], in0=pf[:, :], in1=rbc[blk % 2][:], op=ALU.mult),
                              reads=[pf, rbc[blk % 2]], writes=[of])
                cx.sp.dma(d_fqk[:, :, blk * 512:(blk + 1) * 512].rearrange("j p t -> p j t"), of[:], osl[blk % 2], reads=[of], writes=[d_fqk])
                for tt in range(4):
                    ti = blk * 4 + tt
                    ot = otm[ti % 2]
                    for bi, (a, n) in enumerate(((512, 512), (1024, 512), (1536, 2))):
                        pt = pTM[bi]
                        for kc in range(32):
                            cx.pe.op(lambda: nc.tensor.matmul(pt[:, 0:n], lhsT=xTb[:, kc, tt * 128:(tt + 1) * 128], rhs=Wb[:, kc, a:a + n], start=(kc == 0), stop=False),
                                     reads=[Wb, xTb], writes=[pt])
                        cx.pe.op(lambda: nc.tensor.matmul(pt[:, 0:n], lhsT=rrow[blk % 2][0:1, tt * 128:(tt + 1) * 128], rhs=sW[0:1, a:a + n], start=False, stop=True),
                                 reads=[sW, rrow[blk % 2]], writes=[pt])
                        cx.act.op(lambda: nc.scalar.activation(out=ot[:, a - 512:a - 512 + n], in_=pt[:, 0:n], func=AF.Copy, scale=rs[:, ti:ti + 1]),
                                  reads=[pt, rs], writes=[ot])
                    sl_ = osl[2 + ti % 2]
                    cx.sp.dma(d_rqkv[ti * 128:(ti + 1) * 128, :], ot[:, 0:512], sl_, reads=[ot], writes=[d_rqkv])
                    cx.sp.dma(d_rgfv[ti * 128:(ti + 1) * 128, :], ot[:, 512:1024], sl_, reads=[ot], writes=[d_rgfv], cont=True)
                    cx.sp.dma(d_ff[ti * 128:(ti + 1) * 128, :], ot[:, 1024:1026], sl_, reads=[ot], writes=[d_ff], cont=True)
        cx.barrier()


def ret_consts(h):
    lg = math.log1p(-2.0 ** (-5.0 - h))
    c = np.arange(128, dtype=np.float64)
    t = np.zeros((128, 8), np.float32)
    t[:, 0] = np.exp((c + 1) * lg)
    t[:, 1] = -np.exp((c + 1) * lg)
    t[:, 2] = 128 ** -0.5
    t[:, 3] = -(128 ** -0.5)
    t[:, 4] = np.exp((127 - c) * lg) * 128 ** -0.5
    t[:, 5] = -np.exp((127 - c) * lg) * 128 ** -0.5
    t[:, 6] = np.exp(128 * lg)
    t[:, 7] = -math.pi
    s = c[:, None]; cc = c[None, :]
    maskT = np.where(cc >= s, np.exp((-s - 1) * lg) * np.ones_like(cc), 0.0).astype(np.float32)
    return t, maskT

def rot_consts():
    half = 64
    inv_freq = (10000.0 ** (-np.arange(half, dtype=np.float32) / half)).astype(np.float32)
    invf2 = np.tile(np.concatenate([inv_freq, inv_freq])[None, :], (128, 1)).astype(np.float32)
    offs = np.tile(np.concatenate([np.zeros(64), np.full(64, math.pi / 2)])[None, :], (128, 1)).astype(np.float32)
    return invf2, offs


def emit_ret(cx, nc, S, d_rqkv, d_rgfv, posT, rc, maskT_d, invf2_d, offs_d, gn_d, ident, d_yret, dbg=None):
    NT = S // 128
    TWO_PI = 2 * math.pi
    with ExitStack() as es:
        idt = cx.sbuf(es, "ident", [128, 128], F32)
        rcs = cx.sbuf(es, "rc", [128, 8], F32)
        mk = cx.sbuf(es, "maskT", [128, 128], F32)
        invf2 = cx.sbuf(es, "invf2", [128, 128], F32)
        offs = cx.sbuf(es, "offs", [128, 128], F32)
        gn = cx.sbuf(es, "gn", [128, 256], F32)
        posi = cx.sbuf(es, "posi", [128, NT], I32)
        posf = cx.sbuf(es, "posf", [128, NT], F32)
        epsc = cx.sbuf(es, "epsc", [128, 1], F32)
        state = cx.sbuf(es, "state", [128, 256], F32)
        state_bf = cx.sbuf(es, "state_bf", [128, 256], BF16)
        banks = [cx.psum(es, f"rbank{i}", [128, 512]) for i in range(8)]
        s0 = cx.slot()
        for i, (dst, src) in enumerate(((idt, ident), (rcs, rc), (mk, maskT_d), (invf2, invf2_d), (offs, offs_d), (gn, gn_d), (posi, posT))):
            cx.sp.dma(dst[:], src, s0, writes=[dst], cont=(i > 0))
        ft = Tok(s0.sem, s0.total, s0.key)
        for dst in (idt, rcs, mk, invf2, offs, gn, posi):
            dst.writers[s0.key] = ft
        cx.dve.op(lambda: nc.vector.tensor_copy(out=posf[:], in_=posi[:]), reads=[posi], writes=[posf])
        cx.dve.op(lambda: nc.vector.memset(epsc[:], 1e-6), writes=[epsc])
        NB = 2
        qkv = [cx.sbuf(es, f"qkv{i}", [128, 512], F32) for i in range(NB)]
        rg = [cx.sbuf(es, f"rg{i}", [128, 256], F32) for i in range(NB)]
        lsl = [cx.slot() for _ in range(NB)]
        ang = cx.sbuf(es, "ang", [128, 128], F32)
        sc = cx.sbuf(es, "sc", [128, 128], F32)
        tq = cx.sbuf(es, "tq", [128, 128], F32)
        ki = cx.sbuf(es, "ki", [128, 128], I32)
        tmp = [cx.sbuf(es, f"rt{i}", [128, 64], F32) for i in range(4)]
        qd = cx.sbuf(es, "qd", [128, 128], F32)
        kr = cx.sbuf(es, "kr", [128, 128], F32)
        kd = cx.sbuf(es, "kd", [128, 128], BF16)
        vb = cx.sbuf(es, "vb", [128, 256], BF16)
        qdT = cx.sbuf(es, "qdT", [128, 128], BF16)
        krT = cx.sbuf(es, "krT", [128, 128], BF16)
        scT = cx.sbuf(es, "scT", [128, 128], BF16)
        o = cx.sbuf(es, "o", [128, 256], F32)
        junk = cx.sbuf(es, "rjunk", [128, 256], F32)
        st = cx.sbuf(es, "st", [128, 8], F32)
        sg = cx.sbuf(es, "sg", [128, 256], F32)
        yo = [cx.sbuf(es, f"yo{i}", [128, 256], BF16) for i in range(2)]
        osl = [cx.slot() for _ in range(2)]
        pqT, pkT, pS, pKV, pO = banks[0], banks[1], banks[2], banks[3], banks[4]
        V = nc.vector
        for n in range(NT):
            t_ = qkv[n % NB]; g_ = rg[n % NB]
            cx.sp.dma(t_[:], d_rqkv[n * 128:(n + 1) * 128, :], lsl[n % NB], reads=[d_rqkv], writes=[t_])
            cx.sp.dma(g_[:], d_rgfv[n * 128:(n + 1) * 128, 0:256], lsl[n % NB], reads=[d_rgfv], writes=[g_], cont=True)
            ft = Tok(lsl[n % NB].sem, lsl[n % NB].total, lsl[n % NB].key)
            t_.writers[ft.key] = ft
            cx.dve.op(lambda: V.scalar_tensor_tensor(out=ang[:], in0=invf2[:], scalar=posf[:, n:n + 1], in1=offs[:], op0=ALU.mult, op1=ALU.add),
                      reads=[invf2, posf, offs], writes=[ang])
            cx.dve.op(lambda: V.tensor_scalar(out=tq[:], in0=ang[:], scalar1=1.0 / TWO_PI, scalar2=None, op0=ALU.mult), reads=[ang], writes=[tq])
            cx.dve.op(lambda: V.tensor_copy(out=ki[:], in_=tq[:]), reads=[tq], writes=[ki])
            cx.dve.op(lambda: V.tensor_copy(out=tq[:], in_=ki[:]), reads=[ki], writes=[tq])
            cx.dve.op(lambda: V.scalar_tensor_tensor(out=ang[:], in0=tq[:], scalar=-6.28125, in1=ang[:], op0=ALU.mult, op1=ALU.add), reads=[tq, ang], writes=[ang])
            cx.dve.op(lambda: V.scalar_tensor_tensor(out=ang[:], in0=tq[:], scalar=-0.0019353071795864769, in1=ang[:], op0=ALU.mult, op1=ALU.add), reads=[tq, ang], writes=[ang])
            cx.dve.op(lambda: V.tensor_scalar(out=ang[:], in0=ang[:], scalar1=3.1415925, scalar2=-3.1415925, op0=ALU.min, op1=ALU.max), reads=[ang], writes=[ang])
            cx.act.op(lambda: nc.scalar.activation(out=sc[:], in_=ang[:], func=AF.Sin), reads=[ang], writes=[sc])
            sinp = sc[:, 0:64]; cosp = sc[:, 64:128]
            for (base, outs) in ((0, ((qd, 0, 1),)), (128, ((kr, 2, 3), (kd, 4, 5)))):
                x1 = t_[:, base:base + 64]; x2 = t_[:, base + 64:base + 128]
                cx.dve.op(lambda: V.tensor_tensor(out=tmp[0][:], in0=x1, in1=cosp, op=ALU.mult), reads=[t_, sc], writes=[tmp[0]])
                cx.dve.op(lambda: V.tensor_tensor(out=tmp[1][:], in0=x2, in1=sinp, op=ALU.mult), reads=[t_, sc], writes=[tmp[1]])
                cx.dve.op(lambda: V.tensor_tensor(out=tmp[1][:], in0=tmp[0][:], in1=tmp[1][:], op=ALU.subtract), reads=[tmp[0], tmp[1]], writes=[tmp[1]])
                cx.dve.op(lambda: V.tensor_tensor(out=tmp[2][:], in0=x1, in1=sinp, op=ALU.mult), reads=[t_, sc], writes=[tmp[2]])
                cx.dve.op(lambda: V.tensor_tensor(out=tmp[3][:], in0=x2, in1=cosp, op=ALU.mult), reads=[t_, sc], writes=[tmp[3]])
                cx.dve.op(lambda: V.tensor_tensor(out=tmp[3][:], in0=tmp[3][:], in1=tmp[2][:], op=ALU.add), reads=[tmp[2], tmp[3]], writes=[tmp[3]])
                for (dst, cp, cn) in outs:
                    cx.dve.op(lambda: V.tensor_scalar(out=dst[:, 0:64], in0=tmp[1][:], scalar1=rcs[:, cp:cp + 1], scalar2=None, op0=ALU.mult), reads=[tmp[1], rcs], writes=[dst])
                    cx.dve.op(lambda: V.tensor_scalar(out=dst[:, 64:128], in0=tmp[3][:], scalar1=rcs[:, cp:cp + 1], scalar2=None, op0=ALU.mult), reads=[tmp[3], rcs], writes=[dst])
            cx.act.op(lambda: nc.scalar.copy(out=vb[:], in_=t_[:, 256:512]), reads=[t_], writes=[vb])
            cx.pe.op(lambda: nc.tensor.transpose(out=pqT[:, 0:128], in_=qd[:], identity=idt[:]), reads=[qd, idt], writes=[pqT])
            cx.act.op(lambda: nc.scalar.copy(out=qdT[:], in_=pqT[:, 0:128]), reads=[pqT], writes=[qdT])
            cx.pe.op(lambda: nc.tensor.transpose(out=pkT[:, 0:128], in_=kr[:], identity=idt[:]), reads=[kr, idt], writes=[pkT])
            cx.act.op(lambda: nc.scalar.copy(out=krT[:], in_=pkT[:, 0:128]), reads=[pkT], writes=[krT])
            cx.pe.op(lambda: nc.tensor.matmul(pS[:, 0:128], lhsT=krT[:], rhs=qdT[:], start=True, stop=True), reads=[krT, qdT], writes=[pS])
            cx.dve.op(lambda: V.tensor_tensor(out=scT[:], in0=pS[:, 0:128], in1=mk[:], op=ALU.mult), reads=[pS, mk], writes=[scT])
            cx.pe.op(lambda: nc.tensor.matmul(pKV[:, 0:256], lhsT=kd[:], rhs=vb[:], start=True, stop=True), reads=[kd, vb], writes=[pKV])
            if n > 0:
                cx.pe.op(lambda: nc.tensor.matmul(pO[:, 0:256], lhsT=qdT[:], rhs=state_bf[:], start=True, stop=False), reads=[qdT, state_bf], writes=[pO])
            cx.pe.op(lambda: nc.tensor.matmul(pO[:, 0:256], lhsT=scT[:], rhs=vb[:], start=(n == 0), stop=True), reads=[scT, vb], writes=[pO])
            if n == 0:
                cx.dve.op(lambda: V.tensor_copy(out=state[:], in_=pKV[:, 0:256]), reads=[pKV], writes=[state])
            else:
                cx.dve.op(lambda: V.scalar_tensor_tensor(out=state[:], in0=state[:], scalar=rcs[:, 6:7], in1=pKV[:, 0:256], op0=ALU.mult, op1=ALU.add),
                          reads=[state, rcs, pKV], writes=[state])
            cx.act.op(lambda: nc.scalar.copy(out=state_bf[:], in_=state[:]), reads=[state], writes=[state_bf])
            cx.dve.op(lambda: V.memset(st[:], 0.0), writes=[st])
            cx.act.op(lambda: nc.scalar.activation(out=o[:], in_=pO[:, 0:256], func=AF.Identity, accum_out=st[:, 0:1]), reads=[pO, st], writes=[o, st])
            cx.act.op(lambda: nc.scalar.activation(out=junk[:], in_=pO[:, 0:256], func=AF.Square, accum_out=st[:, 1:2]), reads=[pO, st], writes=[junk, st])
            cx.dve.op(lambda: V.tensor_scalar(out=st[:, 2:3], in0=st[:, 0:1], scalar1=1.0 / 256, scalar2=None, op0=ALU.mult), reads=[st], writes=[st])
            cx.dve.op(lambda: V.tensor_tensor(out=st[:, 3:4], in0=st[:, 2:3], in1=st[:, 2:3], op=ALU.mult), reads=[st], writes=[st])
            cx.dve.op(lambda: V.scalar_tensor_tensor(out=st[:, 4:5], in0=st[:, 1:2], scalar=1.0 / 256, in1=st[:, 3:4], op0=ALU.mult, op1=ALU.subtract),
                      reads=[st], writes=[st])
            cx.act.op(lambda: nc.scalar.activation(out=st[:, 5:6], in_=st[:, 4:5], func=AF.Sqrt, bias=epsc[:, 0:1], scale=1.0), reads=[st, epsc], writes=[st])
            cx.dve.op(lambda: V.reciprocal(out=st[:, 6:7], in_=st[:, 5:6]), reads=[st], writes=[st])
            cx.dve.op(lambda: V.tensor_scalar(out=o[:], in0=o[:], scalar1=st[:, 2:3], scalar2=st[:, 6:7], op0=ALU.subtract, op1=ALU.mult), reads=[o, st], writes=[o])
            cx.act.op(lambda: nc.scalar.activation(out=sg[:], in_=g_[:], func=AF.Silu), reads=[g_], writes=[sg])
            cx.dve.op(lambda: V.tensor_tensor(out=o[:], in0=o[:], in1=gn[:], op=ALU.mult), reads=[o, gn], writes=[o])
            y_ = yo[n % 2]
            cx.dve.op(lambda: V.tensor_tensor(out=y_[:], in0=o[:], in1=sg[:], op=ALU.mult), reads=[o, sg], writes=[y_])
            cx.sp.dma(d_yret[n * 128:(n + 1) * 128, :], y_[:], osl[n % 2], reads=[y_], writes=[d_yret])
        cx.barrier()

def fox_consts():
    s = np.arange(128)[:, None]; c = np.arange(128)[None, :]
    triU = (s <= c).astype(np.float32)
    ones = np.ones((128, 128), np.float32)
    sel = np.zeros((128, 128), np.float32); sel[64, :] = 1.0
    return triU, ones, sel


def emit_fox(cx, nc, S, d_fqk, d_rgfv, d_ff, bf_d, triU_d, ones_d, sel_d, d_yfox):
    NT = S // 128
    SCALE = 128 ** -0.5
    V = nc.vector
    with ExitStack() as es:
        triU = cx.sbuf(es, "triU", [128, 128], F32)
        triUb = cx.sbuf(es, "triUb", [128, 128], BF16)
        ones = cx.sbuf(es, "ones", [128, 128], F32)
        sel = cx.sbuf(es, "sel", [128, 128], F32)
        bfc = cx.sbuf(es, "bfc", [128, 2], F32)
        nbf = cx.sbuf(es, "nbf", [128, 2], F32)
        one1 = cx.sbuf(es, "one1", [128, 1], F32)
        ff = cx.sbuf(es, "ff", [128, NT, 2], F32)
        lf = cx.sbuf(es, "lf", [128, NT, 2], F32)
        cum = cx.sbuf(es, "cum", [128, NT, 2], F32)
        negc = cx.sbuf(es, "negc", [128, 2, NT], F32)
        tot = cx.sbuf(es, "tot", [128, NT, 2], F32)
        off = cx.sbuf(es, "off", [128, NT, 2], F32)
        rmid = cx.sbuf(es, "rmid", [128, 2, NT], F32)
        banks = [cx.psum(es, f"fbank{i}", [128, 512]) for i in range(8)]
        s0 = cx.slot()
        for i, (dst, src) in enumerate(((triU, triU_d), (ones, ones_d), (sel, sel_d), (bfc, bf_d), (ff, d_ff.t.rearrange("(n p) h -> p n h", p=128)))):
            cx.sp.dma(dst[:], src, s0, reads=([d_ff] if dst is ff else []), writes=[dst], cont=(i > 0))
        ft = Tok(s0.sem, s0.total, s0.key)
        for dst in (triU, ones, sel, bfc, ff):
            dst.writers[s0.key] = ft
        cx.dve.op(lambda: V.memset(one1[:], 1.0), writes=[one1])
        cx.dve.op(lambda: V.tensor_copy(out=triUb[:], in_=triU[:]), reads=[triU], writes=[triUb])
        cx.dve.op(lambda: V.tensor_scalar(out=nbf[:], in0=bfc[:], scalar1=-1.0, scalar2=None, op0=ALU.mult), reads=[bfc], writes=[nbf])
        for h in range(2):
            cx.act.op(lambda: nc.scalar.activation(out=lf[:, :, h], in_=ff[:, :, h], func=AF.Exp, bias=nbf[:, h:h + 1], scale=-1.0), reads=[ff, nbf], writes=[lf])
        cx.act.op(lambda: nc.scalar.activation(out=lf[:], in_=lf[:], func=AF.Ln, bias=one1[:, 0:1], scale=1.0), reads=[lf, one1], writes=[lf])
        cx.dve.op(lambda: V.tensor_scalar(out=lf[:], in0=lf[:], scalar1=-1.0, scalar2=None, op0=ALU.mult), reads=[lf], writes=[lf])
        lf2 = lf[:].rearrange("p n h -> p (n h)"); cum2 = cum[:].rearrange("p n h -> p (n h)"); tot2 = tot[:].rearrange("p n h -> p (n h)")
        W2 = NT * 2
        for a in range(0, W2, 512):
            n_ = min(512, W2 - a)
            cx.pe.op(lambda: nc.tensor.matmul(banks[0][:, 0:n_], lhsT=triU[:], rhs=lf2[:, a:a + n_], start=True, stop=True), reads=[triU, lf], writes=[banks[0]])
            cx.dve.op(lambda: V.tensor_copy(out=cum2[:, a:a + n_], in_=banks[0][:, 0:n_]), reads=[banks[0]], writes=[cum])
            cx.pe.op(lambda: nc.tensor.matmul(banks[1][:, 0:n_], lhsT=ones[:], rhs=lf2[:, a:a + n_], start=True, stop=True), reads=[ones, lf], writes=[banks[1]])
            cx.dve.op(lambda: V.tensor_copy(out=tot2[:, a:a + n_], in_=banks[1][:, 0:n_]), reads=[banks[1]], writes=[tot])
        cx.dve.op(lambda: V.memset(off[:, 0, :], 0.0), writes=[off])
        for n in range(1, NT):
            cx.dve.op(lambda: V.tensor_tensor(out=off[:, n, :], in0=off[:, n - 1, :], in1=tot[:, n - 1, :], op=ALU.add), reads=[off, tot], writes=[off])
        cx.dve.op(lambda: V.tensor_tensor(out=cum[:], in0=cum[:], in1=off[:], op=ALU.add), reads=[cum, off], writes=[cum])
        for h in range(2):
            cx.dve.op(lambda: V.tensor_scalar(out=negc[:, h, :], in0=cum[:, :, h], scalar1=-1.0, scalar2=None, op0=ALU.mult), reads=[cum], writes=[negc])
        for a in range(0, W2, 512):
            n_ = min(512, W2 - a)
            cx.pe.op(lambda: nc.tensor.matmul(banks[0][:, 0:n_], lhsT=sel[:], rhs=cum2[:, a:a + n_], start=True, stop=True), reads=[sel, cum], writes=[banks[0]])
            cx.dve.op(lambda: V.tensor_copy(out=tot2[:, a:a + n_], in_=banks[0][:, 0:n_]), reads=[banks[0]], writes=[tot])
        for h in range(2):
            cx.dve.op(lambda: V.tensor_copy(out=rmid[:, h, :], in_=tot[:, :, h]), reads=[tot], writes=[rmid])
        KT = cx.sbuf(es, "KT", [128, S], BF16)
        QT = cx.sbuf(es, "QT", [128, S], BF16)
        Va = cx.sbuf(es, "Va", [128, NT, 130], BF16)
        vst = [cx.sbuf(es, f"vst{i}", [128, 8, 128], F32) for i in range(2)]
        vsl = [cx.slot() for _ in range(2)]
        BQ = [cx.sbuf(es, f"BQ{i}", [128, NT], F32) for i in range(2)]
        PTw = [cx.sbuf(es, f"PTw{i}", [128, 512], BF16) for i in range(3)]
        boff = [cx.sbuf(es, f"boff{i}", [128, NT], F32) for i in range(2)]
        Osb = [cx.sbuf(es, f"Osb{i}", [128, 129], F32) for i in range(4)]
        fd = cx.sbuf(es, "fd", [128, 4], F32)
        fcol = cx.sbuf(es, "fcol", [128, 4], F32)
        rl = cx.sbuf(es, "rl", [128, 2], F32)
        yo = [cx.sbuf(es, f"fyo{i}", [128, 128], BF16) for i in range(2)]
        ysl = [cx.slot() for _ in range(2)]
        ksl = cx.slot()
        pS = banks[0:3]; pO = banks[4:8]
        cx.dve.op(lambda: V.memset(Va[:, :, 128:130], 1.0), writes=[Va])
        fv = d_rgfv.t.rearrange("(n p) c -> p n c", p=128)
        grp = 0
        for h in range(2):
            cx.sp.dma(QT[:], d_fqk[h], ksl, reads=[d_fqk], writes=[QT])
            cx.sp.dma(KT[:], d_fqk[2 + h], ksl, reads=[d_fqk], writes=[KT], cont=True)
            ft = Tok(ksl.sem, ksl.total, ksl.key); QT.writers[ksl.key] = ft
            for g in range(0, NT, 8):
                ng = min(8, NT - g); i_ = (g // 8) % 2
                cx.sp.dma(vst[i_][:, 0:ng, :], fv[:, g:g + ng, 256 + h * 128:256 + (h + 1) * 128], vsl[i_], reads=[d_rgfv], writes=[vst[i_]])
                cx.dve.op(lambda: V.tensor_copy(out=Va[:, g:g + ng, 0:128], in_=vst[i_][:, 0:ng, :]), reads=[vst[i_]], writes=[Va])
            for B in range(NT // 4):
                q0 = 4 * B
                nko = 4 * B
                if nko > 0:
                    bo = boff[B % 2]
                    cx.dve.op(lambda: V.tensor_scalar(out=bo[:, 0:nko], in0=negc[:, h, 0:nko], scalar1=rmid[:, h, q0:q0 + 1], scalar2=None, op0=ALU.add),
                              reads=[negc, rmid], writes=[bo])
                    cx.dve.op(lambda: V.tensor_scalar(out=fd[:, 0:4], in0=rmid[:, h, q0:q0 + 4], scalar1=rmid[:, h, q0:q0 + 1], scalar2=None, op0=ALU.subtract),
                              reads=[rmid], writes=[fd])
                    cx.act.op(lambda: nc.scalar.activation(out=fcol[:, 0:4], in_=fd[:, 0:4], func=AF.Exp), reads=[fd], writes=[fcol])
                    for kap in range(nko):
                        ps = pS[grp % 3]; pt = PTw[grp % 3]; grp += 1
                        cx.pe.op(lambda: nc.tensor.matmul(ps[:, :], lhsT=KT[:, kap * 128:(kap + 1) * 128], rhs=QT[:, q0 * 128:(q0 + 4) * 128], start=True, stop=True),
                                 reads=[KT, QT], writes=[ps])
                        cx.act.op(lambda: nc.scalar.activation(out=pt[:, :], in_=ps[:, :], func=AF.Exp, bias=bo[:, kap:kap + 1], scale=SCALE), reads=[ps, bo], writes=[pt])
                        for j in range(4):
                            cx.pe.op(lambda: nc.tensor.matmul(pO[j][:, 0:129], lhsT=pt[:, j * 128:(j + 1) * 128], rhs=Va[:, kap, 0:129], start=(kap == 0), stop=(kap == nko - 1)),
                                     reads=[pt, Va], writes=[pO[j]])
                    for j in range(4):
                        cx.act.op(lambda: nc.scalar.activation(out=Osb[j][:], in_=pO[j][:, 0:129], func=AF.Copy, scale=fcol[:, j:j + 1]), reads=[pO[j], fcol], writes=[Osb[j]])
                for j in range(4):
                    Q = q0 + j
                    bq = BQ[Q % 2]
                    cx.dve.op(lambda: V.tensor_scalar(out=bq[:, q0:Q + 1], in0=negc[:, h, q0:Q + 1], scalar1=rmid[:, h, Q:Q + 1], scalar2=None, op0=ALU.add),
                              reads=[negc, rmid], writes=[bq])
                    po = pO[j]
                    nk = j + 1
                    ps = pS[grp % 3]; pt = PTw[grp % 3]; grp += 1
                    for jj in range(nk):
                        kap = q0 + jj
                        cx.pe.op(lambda: nc.tensor.matmul(ps[:, jj * 128:(jj + 1) * 128], lhsT=KT[:, kap * 128:(kap + 1) * 128], rhs=QT[:, Q * 128:(Q + 1) * 128],
                                                          start=True, stop=True), reads=[KT, QT], writes=[ps])
                    for jj in range(nk):
                        kap = q0 + jj
                        cx.act.op(lambda: nc.scalar.activation(out=pt[:, jj * 128:(jj + 1) * 128], in_=ps[:, jj * 128:(jj + 1) * 128], func=AF.Exp, bias=bq[:, kap:kap + 1], scale=SCALE),
                                  reads=[ps, bq], writes=[pt])
                        if kap == Q:
                            cx.dve.op(lambda: V.tensor_tensor(out=pt[:, jj * 128:(jj + 1) * 128], in0=pt[:, jj * 128:(jj + 1) * 128], in1=triUb[:], op=ALU.mult), reads=[pt, triUb], writes=[pt])
                    for jj in range(nk):
                        kap = q0 + jj
                        cx.pe.op(lambda: nc.tensor.matmul(po[:, 0:129], lhsT=pt[:, jj * 128:(jj + 1) * 128], rhs=Va[:, kap, 0:129], start=(jj == 0), stop=(jj == nk - 1)),
                                 reads=[pt, Va], writes=[po])
                    if nko > 0:
                        cx.dve.op(lambda: V.tensor_tensor(out=Osb[j][:], in0=Osb[j][:], in1=po[:, 0:129], op=ALU.add), reads=[Osb[j], po], writes=[Osb[j]])
                    else:
                        cx.dve.op(lambda: V.tensor_copy(out=Osb[j][:], in_=po[:, 0:129]), reads=[po], writes=[Osb[j]])
                    cx.dve.op(lambda: V.reciprocal(out=rl[:, Q % 2:Q % 2 + 1], in_=Osb[j][:, 128:129]), reads=[Osb[j]], writes=[rl])
                    y_ = yo[Q % 2]
                    cx.dve.op(lambda: V.tensor_scalar(out=y_[:], in0=Osb[j][:, 0:128], scalar1=rl[:, Q % 2:Q % 2 + 1], scalar2=None, op0=ALU.mult), reads=[Osb[j], rl], writes=[y_])
                    cx.sp.dma(d_yfox[Q * 128:(Q + 1) * 128, h * 128:(h + 1) * 128], y_[:], ysl[Q % 2], reads=[y_], writes=[d_yfox])
        cx.barrier()

class TopkScratch:
    def __init__(self, cx, es, nc):
        self.s2 = cx.sbuf(es, "tk_s2", [128, 128], F32)
        self.t1 = None
        self.i1u = None
        self.i1 = cx.sbuf(es, "tk_i1", [128, 16, 16], F32)
        self.cand = cx.sbuf(es, "tk_cand", [128, 256], F32)
        self.cand2 = cx.sbuf(es, "tk_cand2", [128, 256], F32)
        self.ct = cx.sbuf(es, "tk_ct", [128, 16], F32)
        self.pu = cx.sbuf(es, "tk_pu", [128, 16], U32)
        self.au = cx.sbuf(es, "tk_au", [128, 16], U32)
        self.bu = cx.sbuf(es, "tk_bu", [128, 16], U32)
        self.af = cx.sbuf(es, "tk_af", [128, 16], F32)
        self.bf = cx.sbuf(es, "tk_bf", [128, 16], F32)
        self.eq = cx.sbuf(es, "tk_eq", [128, 16, 16], F32)
        self.io16 = cx.sbuf(es, "tk_io16", [128, 16], F32)
        self.ncm = cx.sbuf(es, "tk_ncm", [128, 1], F32)
        self.z = cx.sbuf(es, "tk_z", [128, 2], F32)
        self.ex = cx.sbuf(es, "tk_ex", [128, 16], F32)
        cx.pool.op(lambda: nc.gpsimd.iota(self.io16[:], pattern=[[1, 16]], base=0, channel_multiplier=0, allow_small_or_imprecise_dtypes=True), writes=[self.io16])


def emit_level1(cx, nc, sc, s_sb, chunk):
    V = nc.vector
    cx.dve.op(lambda: V.max(out=sc.t1[:, chunk, 0:8], in_=s_sb[:]), reads=[s_sb], writes=[sc.t1])
    cx.dve.op(lambda: V.max_index(out=sc.i1u[:, chunk, 0:8], in_max=sc.t1[:, chunk, 0:8], in_values=s_sb[:]), reads=[s_sb, sc.t1], writes=[sc.i1u])
    cx.dve.op(lambda: V.match_replace(out=sc.s2[:], in_to_replace=sc.t1[:, chunk, 0:8], in_values=s_sb[:], imm_value=-1e30), reads=[s_sb, sc.t1], writes=[sc.s2])
    cx.dve.op(lambda: V.max(out=sc.t1[:, chunk, 8:16], in_=sc.s2[:]), reads=[sc.s2], writes=[sc.t1])
    cx.dve.op(lambda: V.max_index(out=sc.i1u[:, chunk, 8:16], in_max=sc.t1[:, chunk, 8:16], in_values=sc.s2[:]), reads=[sc.s2, sc.t1], writes=[sc.i1u])


def emit_level2(cx, nc, sc, TI1, TI2, TG):
    V = nc.vector
    cx.dve.op(lambda: V.tensor_copy(out=sc.i1[:], in_=sc.i1u[:]), reads=[sc.i1u], writes=[sc.i1])
    for h in range(8):
        ta = sc.t1[:, 2 * h, :]; tb = sc.t1[:, 2 * h + 1, :]
        cand3 = sc.cand[:].rearrange("p (a b) -> p a b", a=16)
        cx.dve.op(lambda: V.tensor_tensor(out=cand3, in0=ta.unsqueeze(2).to_broadcast([128, 16, 16]), in1=tb.unsqueeze(1).to_broadcast([128, 16, 16]), op=ALU.add),
                  reads=[sc.t1], writes=[sc.cand])
        cx.dve.op(lambda: V.max(out=sc.ct[:, 0:8], in_=sc.cand[:]), reads=[sc.cand], writes=[sc.ct])
        cx.dve.op(lambda: V.max_index(out=sc.pu[:, 0:8], in_max=sc.ct[:, 0:8], in_values=sc.cand[:]), reads=[sc.cand, sc.ct], writes=[sc.pu])
        cx.dve.op(lambda: V.match_replace(out=sc.cand2[:], in_to_replace=sc.ct[:, 0:8], in_values=sc.cand[:], imm_value=-1e30), reads=[sc.cand, sc.ct], writes=[sc.cand2])
        cx.dve.op(lambda: V.max(out=sc.ct[:, 8:16], in_=sc.cand2[:]), reads=[sc.cand2], writes=[sc.ct])
        cx.dve.op(lambda: V.max_index(out=sc.pu[:, 8:16], in_max=sc.ct[:, 8:16], in_values=sc.cand2[:]), reads=[sc.cand2, sc.ct], writes=[sc.pu])
        cx.dve.op(lambda: V.tensor_single_scalar(out=sc.au[:], in_=sc.pu[:], scalar=4, op=ALU.logical_shift_right), reads=[sc.pu], writes=[sc.au])
        cx.dve.op(lambda: V.tensor_single_scalar(out=sc.bu[:], in_=sc.pu[:], scalar=15, op=ALU.bitwise_and), reads=[sc.pu], writes=[sc.bu])
        cx.dve.op(lambda: V.tensor_copy(out=sc.af[:], in_=sc.au[:]), reads=[sc.au], writes=[sc.af])
        cx.dve.op(lambda: V.tensor_copy(out=sc.bf[:], in_=sc.bu[:]), reads=[sc.bu], writes=[sc.bf])
        for (sel, chunk, TI) in ((sc.af, 2 * h, TI1), (sc.bf, 2 * h + 1, TI2)):
            cx.dve.op(lambda: V.tensor_tensor(out=sc.eq[:], in0=sel[:].unsqueeze(2).to_broadcast([128, 16, 16]), in1=sc.io16[:].unsqueeze(1).to_broadcast([128, 16, 16]), op=ALU.is_equal),
                      reads=[sel, sc.io16], writes=[sc.eq])
            cx.dve.op(lambda: V.tensor_tensor(out=sc.eq[:], in0=sc.eq[:], in1=sc.i1[:, chunk, :].unsqueeze(1).to_broadcast([128, 16, 16]), op=ALU.mult),
                      reads=[sc.eq, sc.i1], writes=[sc.eq])
            cx.dve.op(lambda: V.tensor_reduce(out=TI[:, h * 16:(h + 1) * 16], in_=sc.eq[:], axis=AX.X, op=ALU.add), reads=[sc.eq], writes=[TI])
        cx.dve.op(lambda: V.tensor_scalar(out=sc.ncm[:], in0=sc.ct[:, 0:1], scalar1=-1.0, scalar2=None, op0=ALU.mult), reads=[sc.ct], writes=[sc.ncm])
        cx.dve.op(lambda: V.memset(sc.z[:], 0.0), writes=[sc.z])
        cx.act.op(lambda: nc.scalar.activation(out=sc.ex[:], in_=sc.ct[:], func=AF.Exp, bias=sc.ncm[:, 0:1], scale=1.0, accum_out=sc.z[:, 0:1]), reads=[sc.ct, sc.ncm, sc.z], writes=[sc.ex, sc.z])
        cx.dve.op(lambda: V.reciprocal(out=sc.z[:, 1:2], in_=sc.z[:, 0:1]), reads=[sc.z], writes=[sc.z])
        cx.dve.op(lambda: V.tensor_scalar(out=TG[:, h * 16:(h + 1) * 16], in0=sc.ex[:], scalar1=sc.z[:, 1:2], scalar2=None, op0=ALU.mult), reads=[sc.ex, sc.z], writes=[TG])

EPS = 1e-6

def emit_l2(cx, nc, TPC, x_d, yret_d, yfox_d, mod_row, modT_d, n2T_d, gfT_d, fg_row, Wo_d, Wq_d, skT_d, uT_d, v_d, ident_d, d_x2, d_x3, out_d, stage=9, NCH=128):
    V = nc.vector
    NTT = TPC // 128
    TGA = min(TPC, 1024)
    T = min(TPC, 512)
    NTG = T // 128
    with ExitStack() as es:
        banks = [cx.psum(es, f"l2bank{i}", [128, 512]) for i in range(8)]
        idt = cx.sbuf(es, "ident", [128, 128], F32)
        idtb = cx.sbuf(es, "identb", [128, 128], BF16)
        modT = cx.sbuf(es, "modT", [128, 192], F32)
        n2T = cx.sbuf(es, "n2T", [128, 32], F32)
        gfT = cx.sbuf(es, "gfT", [128, 16], F32)
        g2p = cx.sbuf(es, "g2p", [128, 32], F32)
        epsc = cx.sbuf(es, "epsc", [128, 1], F32)
        ssf = cx.sbuf(es, "ssf", [128, NTT], F32)
        rsf = cx.sbuf(es, "rsf", [128, NTT], F32)
        ss2 = cx.sbuf(es, "ss2", [128, NTT, 8], F32)
        rs2 = cx.sbuf(es, "rs2", [128, NTT], F32)
        ss3 = cx.sbuf(es, "ss3", [128, NTT, 8], F32)
        rs3 = cx.sbuf(es, "rs3", [128, NTT], F32)
        iota = cx.sbuf(es, "iota", [128, 128], F32)
        s0 = cx.slot()
        for i, (dst, src) in enumerate(((idt, ident_d), (modT, modT_d), (n2T, n2T_d), (gfT, gfT_d))):
            cx.sp.dma(dst[:], src, s0, writes=[dst], cont=(i > 0))
        ft = Tok(s0.sem, s0.total, s0.key)
        for dst in (idt, modT, n2T, gfT):
            dst.writers[s0.key] = ft
        cx.dve.op(lambda: V.tensor_copy(out=idtb[:], in_=idt[:]), reads=[idt], writes=[idtb])
        cx.dve.op(lambda: V.memset(epsc[:], EPS), writes=[epsc])
        for b_ in (ssf, ss2, ss3):
            cx.dve.op(lambda: V.memset(b_[:], 0.0), writes=[b_])
        cx.pool.op(lambda: nc.gpsimd.iota(iota[:], pattern=[[1, 128]], base=0, channel_multiplier=0, allow_small_or_imprecise_dtypes=True), writes=[iota])
        cx.dve.op(lambda: V.scalar_tensor_tensor(out=g2p[:], in0=modT[:, 128:160], scalar=1.0, in1=n2T[:], op0=ALU.add, op1=ALU.mult), reads=[modT, n2T], writes=[g2p])
        sh2 = modT[:, 96:128]

        with ExitStack() as esA:
            gate1 = cx.sbuf(esA, "gate1", [128, 4096], F32)
            cx.sp.dma(gate1[:], mod_row[0:1, 8192:12288].partition_broadcast(128), cx.slot(), writes=[gate1])
            yT = cx.sbuf(esA, "yT", [128, 32, TGA], BF16)
            yt = [cx.sbuf(esA, f"yt{i}", [128, 4096], BF16) for i in range(2)]
            ysl = [cx.slot() for _ in range(2)]
            junkb = cx.sbuf(esA, "junkb", [128, 2048], BF16)
            wst = [cx.sbuf(esA, f"wost{i}", [128, 8, 512], F32) for i in range(2)]
            wsl = [cx.slot() for _ in range(2)]
            wb = [cx.sbuf(esA, f"wob{i}", [128, 32, 512], BF16) for i in range(2)]
            xp = [cx.sbuf(esA, f"xp{i}", [128, 512], F32) for i in range(3)]
            xsl = [cx.slot() for _ in range(3)]
            osl = [cx.slot() for _ in range(3)]
            junk = cx.sbuf(esA, "junkA", [128, 512], F32)
            pTb = [banks[i][:].bitcast(BF16) for i in (0, 1)]
            wov = Wo_d.rearrange("(k p) n -> p k n", p=128)
            pc = 0
            for ga in range(TPC // TGA):
                for tt in range(TGA // 128):
                    ti = ga * (TGA // 128) + tt
                    yb = yt[ti % 2]
                    cx.sp.dma(yb[:, 0:2048], yret_d[ti * 128:(ti + 1) * 128, :], ysl[ti % 2], reads=[yret_d], writes=[yb])
                    cx.sp.dma(yb[:, 2048:4096], yfox_d[ti * 128:(ti + 1) * 128, :], ysl[ti % 2], reads=[yfox_d], writes=[yb], cont=True)
                    ft = Tok(ysl[ti % 2].sem, ysl[ti % 2].total, ysl[ti % 2].key); yb.writers[ft.key] = ft
                    cx.act.op(lambda: nc.scalar.activation(out=junkb[:], in_=yb[:, 2048:4096], func=AF.Square, accum_out=ssf[:, ti:ti + 1]), reads=[yb, ssf], writes=[junkb, ssf])
                    cx.act.op(lambda: nc.scalar.activation(out=rsf[:, ti:ti + 1], in_=ssf[:, ti:ti + 1], func=AF.Sqrt, bias=epsc[:, 0:1], scale=1.0 / 2048), reads=[ssf, epsc], writes=[rsf])
                    cx.dve.op(lambda: V.reciprocal(out=rsf[:, ti:ti + 1], in_=rsf[:, ti:ti + 1]), reads=[rsf], writes=[rsf])
                    for g in range(4):
                        pb = banks[g % 2]; pv = pTb[g % 2]
                        for kk in range(8):
                            kc = 8 * g + kk
                            cx.pe.op(lambda: nc.tensor.transpose(out=pv[:, kk * 128:(kk + 1) * 128], in_=yb[:, kc * 128:(kc + 1) * 128], identity=idtb[:]), reads=[yb, idtb], writes=[pb])
                        cx.dve.op(lambda: V.tensor_copy(out=yT[:, 8 * g:8 * g + 8, tt * 128:(tt + 1) * 128], in_=pv[:, :].rearrange("p (a b) -> p a b", a=8)), reads=[pb], writes=[yT])
                for nb in range(8):
                    wbb = wb[nb % 2]
                    for q4 in range(4):
                        st = wst[(nb * 4 + q4) % 2]
                        cx.sp.dma(st[:], wov[:, 8 * q4:8 * q4 + 8, nb * 512:(nb + 1) * 512], wsl[(nb * 4 + q4) % 2], writes=[st])
                        if q4 < 2:
                            cx.dve.op(lambda: V.tensor_tensor(out=wbb[:, 8 * q4:8 * q4 + 8, :], in0=st[:], in1=gate1[:, nb * 512:(nb + 1) * 512].unsqueeze(1).to_broadcast([128, 8, 512]), op=ALU.mult),
                                      reads=[st, gate1], writes=[wbb])
                        else:
                            for kk in range(8):
                                kc = 8 * q4 + kk
                                cx.dve.op(lambda: V.scalar_tensor_tensor(out=wbb[:, kc, :], in0=st[:, kk, :], scalar=gfT[:, kc - 16:kc - 15], in1=gate1[:, nb * 512:(nb + 1) * 512], op0=ALU.mult, op1=ALU.mult),
                                          reads=[st, gfT, gate1], writes=[wbb])
                    for tt in range(TGA // 128):
                        ti = ga * (TGA // 128) + tt
                        xb = xp[pc % 3]; sl_x = xsl[pc % 3]; sl_o = osl[pc % 3]; pc += 1
                        cx.sp.dma(xb[:], x_d[ti * 128:(ti + 1) * 128, nb * 512:(nb + 1) * 512], sl_x, writes=[xb])
                        pr = banks[2 + (tt % 2) * 2]; pf = banks[3 + (tt % 2) * 2]
                        for kc in range(16):
                            cx.pe.op(lambda: nc.tensor.matmul(pr[:, :], lhsT=yT[:, kc, tt * 128:(tt + 1) * 128], rhs=wbb[:, kc, :], start=(kc == 0), stop=(kc == 15)), reads=[yT, wbb], writes=[pr])
                        for kc in range(16, 32):
                            cx.pe.op(lambda: nc.tensor.matmul(pf[:, :], lhsT=yT[:, kc, tt * 128:(tt + 1) * 128], rhs=wbb[:, kc, :], start=(kc == 16), stop=(kc == 31)), reads=[yT, wbb], writes=[pf])
                        cx.dve.op(lambda: V.scalar_tensor_tensor(out=xb[:], in0=pf[:, :], scalar=rsf[:, ti:ti + 1], in1=xb[:], op0=ALU.mult, op1=ALU.add), reads=[pf, rsf, xb], writes=[xb])
                        cx.dve.op(lambda: V.tensor_tensor(out=xb[:], in0=xb[:], in1=pr[:, :], op=ALU.add), reads=[xb, pr], writes=[xb])
                        cx.act.op(lambda: nc.scalar.activation(out=junk[:], in_=xb[:], func=AF.Square, accum_out=ss2[:, ti, nb:nb + 1]), reads=[xb, ss2], writes=[junk, ss2])
                        cx.sp.dma(d_x2[ti * 128:(ti + 1) * 128, nb * 512:(nb + 1) * 512], xb[:], sl_o, reads=[xb], writes=[d_x2])
            cx.barrier()
        if stage < 1:
            return
        cx.dve.op(lambda: V.tensor_reduce(out=rs2[:], in_=ss2[:], axis=AX.X, op=ALU.add), reads=[ss2], writes=[rs2])
        cx.act.op(lambda: nc.scalar.activation(out=rs2[:], in_=rs2[:], func=AF.Sqrt, bias=epsc[:, 0:1], scale=1.0 / 4096), reads=[rs2, epsc], writes=[rs2])
        cx.dve.op(lambda: V.reciprocal(out=rs2[:], in_=rs2[:]), reads=[rs2], writes=[rs2])

        NGRP = TPC // T
        d_ub = cx.dram("ub_scr", [NCH, 128, 32, 128], BF16)
        d_vb = cx.dram("vb_scr", [NCH, 128, 4096], BF16)
        G = cx.sbuf(es, "G", [128, 128, T], BF16)
        kI1 = cx.sbuf(es, "kI1", [128, T], F32)
        kI2 = cx.sbuf(es, "kI2", [128, T], F32)
        kG = cx.sbuf(es, "kG", [128, T], F32)
        for gp_ in range(TPC // T):
            t0 = gp_ * T
            cx.new_epoch()
            with ExitStack() as esB:
                h2T = cx.sbuf(esB, "h2T", [128, 32, T], BF16)
                with ExitStack() as esa:
                    x2t = cx.sbuf(esa, "x2t", [128, 4096], F32)
                    xs_ = cx.slot()
                    for tt in range(NTG):
                        ti = gp_ * NTG + tt
                        cx.sp.dma(x2t[:], d_x2[ti * 128:(ti + 1) * 128, :], xs_, reads=[d_x2], writes=[x2t])
                        cx.act.op(lambda: nc.scalar.activation(out=x2t[:], in_=x2t[:], func=AF.Copy, scale=rs2[:, ti:ti + 1]), reads=[x2t, rs2], writes=[x2t])
                        for g in range(8):
                            pb = banks[g % 2]
                            for kk in range(4):
                                kc = 4 * g + kk
                                cx.pe.op(lambda: nc.tensor.transpose(out=pb[:, kk * 128:(kk + 1) * 128], in_=x2t[:, kc * 128:(kc + 1) * 128], identity=idt[:]), reads=[x2t, idt], writes=[pb])
                            for kk in range(4):
                                kc = 4 * g + kk
                                e_ = cx.act if kk % 2 == 0 else cx.dve
                                if kk % 2 == 0:
                                    cx.act.op(lambda: nc.scalar.activation(out=h2T[:, kc, tt * 128:(tt + 1) * 128], in_=pb[:, kk * 128:(kk + 1) * 128], func=AF.Identity, scale=g2p[:, kc:kc + 1], bias=sh2[:, kc:kc + 1]),
                                              reads=[pb, g2p, modT], writes=[h2T])
                                else:
                                    cx.dve.op(lambda: V.tensor_scalar(out=h2T[:, kc, tt * 128:(tt + 1) * 128], in0=pb[:, kk * 128:(kk + 1) * 128], scalar1=g2p[:, kc:kc + 1], scalar2=sh2[:, kc:kc + 1], op0=ALU.mult, op1=ALU.add),
                                              reads=[pb, g2p, modT], writes=[h2T])
                    cx.barrier()
                if stage < 2:
                    return
                with ExitStack() as esb:
                    sc = TopkScratch(cx, esb, nc)
                    scs = [TopkScratch.__new__(TopkScratch) for _ in range(NTG)]
                    t1s = [cx.sbuf(esb, f"t1s{i}", [128, 16, 16], F32) for i in range(NTG)]
                    i1s = [cx.sbuf(esb, f"i1s{i}", [128, 16, 16], U32) for i in range(NTG)]
                    wqst = [cx.sbuf(esb, f"wqst{i}", [128, 8, 128], F32) for i in range(2)]
                    wqsl = [cx.slot() for _ in range(2)]
                    wqb = [cx.sbuf(esb, f"wqb{i}", [128, 32, 128], BF16) for i in range(1)]
                    sksl = [cx.slot() for _ in range(2)]
                    skst = [cx.sbuf(esb, f"skst{i}", [128, 128], F32) for i in range(2)]
                    skb = [cx.sbuf(esb, f"skb{i}", [128, 128], BF16) for i in range(2)]
                    qTc = [cx.sbuf(esb, f"qTc{i}", [128, T], BF16) for i in range(2)]
                    s_sb = [cx.sbuf(esb, f"s_sb{i}", [128, 128], F32) for i in range(2)]
                    TI = [cx.sbuf(esb, n, [128, 128], F32) for n in ("TI1", "TI2", "TG")]
                    wqv = Wq_d
                    cnt = 0
                    for ch in range(16):
                        wq_ = wqb[0]; sks = skst[ch % 2]; sk_ = skb[ch % 2]; qc = qTc[ch % 2]
                        for hf in range(4):
                            st = wqst[hf % 2]
                            cx.sp.dma(st[:], wqv[ch, :, 8 * hf:8 * hf + 8, :], wqsl[hf % 2], writes=[st])
                            if hf % 2 == 0:
                                cx.pool.op(lambda: nc.gpsimd.tensor_copy(out=wq_[:, 8 * hf:8 * hf + 8, :], in_=st[:]), reads=[st], writes=[wq_])
                            else:
                                cx.dve.op(lambda: V.tensor_copy(out=wq_[:, 8 * hf:8 * hf + 8, :], in_=st[:]), reads=[st], writes=[wq_])
                        cx.sp.dma(sks[:], skT_d[ch], sksl[ch % 2], writes=[sks])
                        cx.dve.op(lambda: V.tensor_copy(out=sk_[:], in_=sks[:]), reads=[sks], writes=[sk_])
                        pq = banks[2 + ch % 2]
                        for kc in range(32):
                            cx.pe.op(lambda: nc.tensor.matmul(pq[:, 0:T], lhsT=wq_[:, kc, :], rhs=h2T[:, kc, :], start=(kc == 0), stop=(kc == 31)), reads=[wq_, h2T], writes=[pq])
                        cx.act.op(lambda: nc.scalar.copy(out=qc[:], in_=pq[:, 0:T]), reads=[pq], writes=[qc])
                        for tt in range(NTG):
                            ps = banks[4 + cnt % 2]; ss_ = s_sb[cnt % 2]; cnt += 1
                            cx.pe.op(lambda: nc.tensor.matmul(ps[:, 0:128], lhsT=qc[:, tt * 128:(tt + 1) * 128], rhs=sk_[:], start=True, stop=True), reads=[qc, sk_], writes=[ps])
                            cx.act.op(lambda: nc.scalar.copy(out=ss_[:], in_=ps[:, 0:128]), reads=[ps], writes=[ss_])
                            sc.t1, sc.i1u = t1s[tt], i1s[tt]
                            emit_level1(cx, nc, sc, ss_, ch)
                    for tt in range(NTG):
                        sc.t1, sc.i1u = t1s[tt], i1s[tt]
                        emit_level2(cx, nc, sc, *TI)
                        for (src, dst) in zip(TI, (kI1, kI2, kG)):
                            pt = banks[6]
                            cx.pe.op(lambda: nc.tensor.transpose(out=pt[:, 0:128], in_=src[:], identity=idt[:]), reads=[src, idt], writes=[pt])
                            cx.act.op(lambda: nc.scalar.copy(out=dst[:, tt * 128:(tt + 1) * 128], in_=pt[:, 0:128]), reads=[pt], writes=[dst])
                    cx.barrier()
                if stage < 3:
                    return
                with ExitStack() as ese:
                    At = [cx.sbuf(ese, f"At{i}", [128, 128], BF16) for i in range(4)]
                    Bt = [cx.sbuf(ese, f"Bt{i}", [128, 128], BF16) for i in range(4)]
                    for t4 in range(T // 4):
                        pg = banks[t4 % 2]
                        for j in range(4):
                            t_ = t4 * 4 + j
                            a_ = At[j]; b_ = Bt[j]
                            cx.dve.op(lambda: V.tensor_scalar(out=a_[:], in0=iota[:], scalar1=kI1[:, t_:t_ + 1], scalar2=kG[:, t_:t_ + 1], op0=ALU.is_equal, op1=ALU.mult), reads=[iota, kI1, kG], writes=[a_])
                            cx.dve.op(lambda: V.tensor_scalar(out=b_[:], in0=iota[:], scalar1=kI2[:, t_:t_ + 1], scalar2=None, op0=ALU.is_equal), reads=[iota, kI2], writes=[b_])
                            cx.pe.op(lambda: nc.tensor.matmul(pg[:, j * 128:(j + 1) * 128], lhsT=a_[:], rhs=b_[:], start=True, stop=True), reads=[a_, b_], writes=[pg])
                        cx.act.op(lambda: nc.scalar.copy(out=G[:, :, t4 * 4:t4 * 4 + 4].rearrange("p c t -> p t c"), in_=pg[:, :].rearrange("p (t c) -> p t c", t=4)), reads=[pg], writes=[G])
                    cx.barrier()
                if stage < 4:
                    return
                with ExitStack() as esf:
                    ust = [cx.sbuf(esf, f"ust{i}", [128, 16, 128], F32) for i in range(2)]
                    usl = [cx.slot() for _ in range(2)]
                    ub = [cx.sbuf(esf, f"ub{i}", [128, 32, 128], BF16) for i in range(2)]
                    ga = [cx.sbuf(esf, f"ga{i}", [128, T], BF16) for i in range(2)]
                    ubs = [cx.slot() for _ in range(2)]
                    uv = uT_d
                    hc = 0
                    for c in range(NCH):
                        ub_ = ub[c % 2]
                        if gp_ == 0:
                            for hf in range(2):
                                st = ust[hc % 2]; sl_ = usl[hc % 2]; hc += 1
                                cx.sp.dma(st[:], uv[c, :, 16 * hf:16 * hf + 16, :], sl_, writes=[st])
                                if hf == 0:
                                    cx.pool.op(lambda: nc.gpsimd.tensor_copy(out=ub_[:, 0:16, :], in_=st[:]), reads=[st], writes=[ub_])
                                else:
                                    cx.dve.op(lambda: V.tensor_copy(out=ub_[:, 16:32, :], in_=st[:]), reads=[st], writes=[ub_])
                            if NGRP > 1:
                                cx.sp.dma(d_ub[c], ub_[:], ubs[c % 2], reads=[ub_], writes=[d_ub])
                        else:
                            cx.sp.dma(ub_[:], d_ub[c], ubs[c % 2], reads=[d_ub], writes=[ub_])
                        pa = banks[2 + c % 2]
                        for kc in range(32):
                            cx.pe.op(lambda: nc.tensor.matmul(pa[:, 0:T], lhsT=ub_[:, kc, :], rhs=h2T[:, kc, :], start=(kc == 0), stop=(kc == 31)), reads=[ub_, h2T], writes=[pa])
                        g_ = ga[c % 2]
                        cx.act.op(lambda: nc.scalar.activation(out=g_[:], in_=pa[:, 0:T], func=AF.Gelu_apprx_tanh), reads=[pa], writes=[g_])
                        cx.dve.op(lambda: V.tensor_tensor(out=G[:, c, :], in0=G[:, c, :], in1=g_[:], op=ALU.mult), reads=[G, g_], writes=[G])
                    cx.barrier()
            if stage < 5:
                return
            with ExitStack() as esg:
                gate2 = cx.sbuf(esg, "gate2", [128, 4096], F32)
                cx.sp.dma(gate2[:], mod_row[0:1, 20480:24576].partition_broadcast(128), cx.slot(), writes=[gate2])
                vst = [cx.sbuf(esg, f"vst{i}", [128, 1024], F32) for i in range(3)]
                vsl = [cx.slot() for _ in range(3)]
                vbs = [cx.slot() for _ in range(3)]
                vb = [cx.sbuf(esg, f"vb{i}", [128, 1024], BF16) for i in range(3)]
                xq = [cx.sbuf(esg, f"xq{i}", [128, 512], F32) for i in range(3)]
                xqs = [cx.slot() for _ in range(3)]
                xos = [cx.slot() for _ in range(3)]
                junk = cx.sbuf(esg, "junkg", [128, 512], F32)
                vc = 0; pc = 0
                for p in range(4):
                    for c in range(NCH):
                        st = vst[vc % 3]; vb_ = vb[vc % 3]; sl_ = vsl[vc % 3]; sl2_ = vbs[vc % 3]; vc += 1
                        if gp_ == 0:
                            cx.sp.dma(st[:], v_d[c, :, p * 1024:(p + 1) * 1024], sl_, writes=[st])
                            if c % 2 == 0:
                                cx.pool.op(lambda: nc.gpsimd.tensor_tensor(out=vb_[:], in0=st[:], in1=gate2[:, p * 1024:(p + 1) * 1024], op=ALU.mult), reads=[st, gate2], writes=[vb_])
                            else:
                                cx.dve.op(lambda: V.tensor_tensor(out=vb_[:], in0=st[:], in1=gate2[:, p * 1024:(p + 1) * 1024], op=ALU.mult), reads=[st, gate2], writes=[vb_])
                            if NGRP > 1:
                                cx.sp.dma(d_vb[c, :, p * 1024:(p + 1) * 1024], vb_[:], sl2_, reads=[vb_], writes=[d_vb])
                        else:
                            cx.sp.dma(vb_[:], d_vb[c, :, p * 1024:(p + 1) * 1024], sl2_, reads=[d_vb], writes=[vb_])
                        for ts in range(NTG):
                            for j in range(2):
                                po = banks[ts * 2 + j]
                                cx.pe.op(lambda: nc.tensor.matmul(po[:, :], lhsT=G[:, c, ts * 128:(ts + 1) * 128], rhs=vb_[:, j * 512:(j + 1) * 512], start=(c == 0), stop=(c == NCH - 1)), reads=[G, vb_], writes=[po])
                    for ts in range(NTG):
                        ti = gp_ * NTG + ts
                        for j in range(2):
                            nb = p * 2 + j
                            po = banks[ts * 2 + j]
                            xb = xq[pc % 3]; s1_ = xqs[pc % 3]; s2_ = xos[pc % 3]; pc += 1
                            cx.sp.dma(xb[:], d_x2[ti * 128:(ti + 1) * 128, nb * 512:(nb + 1) * 512], s1_, reads=[d_x2], writes=[xb])
                            cx.dve.op(lambda: V.tensor_tensor(out=xb[:], in0=xb[:], in1=po[:, :], op=ALU.add), reads=[xb, po], writes=[xb])
                            cx.act.op(lambda: nc.scalar.activation(out=junk[:], in_=xb[:], func=AF.Square, accum_out=ss3[:, ti, nb:nb + 1]), reads=[xb, ss3], writes=[junk, ss3])
                            cx.sp.dma(d_x3[ti * 128:(ti + 1) * 128, nb * 512:(nb + 1) * 512], xb[:], s2_, reads=[xb], writes=[d_x3])
                cx.barrier()
        if stage < 6:
            return
        cx.dve.op(lambda: V.tensor_reduce(out=rs3[:], in_=ss3[:], axis=AX.X, op=ALU.add), reads=[ss3], writes=[rs3])
        cx.act.op(lambda: nc.scalar.activation(out=rs3[:], in_=rs3[:], func=AF.Sqrt, bias=epsc[:, 0:1], scale=1.0 / 4096), reads=[rs3, epsc], writes=[rs3])
        cx.dve.op(lambda: V.reciprocal(out=rs3[:], in_=rs3[:]), reads=[rs3], writes=[rs3])
        with ExitStack() as esh:
            fg = cx.sbuf(esh, "fg", [128, 4096], F32)
            cx.sp.dma(fg[:], fg_row[0:1, :].partition_broadcast(128), cx.slot(), writes=[fg])
            xt = [cx.sbuf(esh, f"x3t{i}", [128, 4096], F32) for i in range(2)]
            sl1 = [cx.slot() for _ in range(2)]; sl2 = [cx.slot() for _ in range(2)]
            for ti in range(NTT):
                xb = xt[ti % 2]
                cx.sp.dma(xb[:], d_x3[ti * 128:(ti + 1) * 128, :], sl1[ti % 2], reads=[d_x3], writes=[xb])
                cx.dve.op(lambda: V.scalar_tensor_tensor(out=xb[:], in0=xb[:], scalar=rs3[:, ti:ti + 1], in1=fg[:], op0=ALU.mult, op1=ALU.mult), reads=[xb, rs3, fg], writes=[xb])
                cx.sp.dma(out_d[ti * 128:(ti + 1) * 128, :], xb[:], sl2[ti % 2], reads=[xb], writes=[out_d])
            cx.barrier()


S_FULL = 16384

def build_l1(S):
    nc = bass.Bass("TRN2", target_bir_lowering=False)
    di = lambda n, s, d=F32: nc.dram_tensor(n, s, d, kind="ExternalInput").ap()
    x = di("x", [S, 4096]); wc = di("wc", [4096, NW]); modT = di("modT", [128, 192]); n1T = di("n1T", [128, 32]); ident = di("ident", [128, 128])
    posT = di("posT", [128, S // 128], I32); rc = di("rc", [128, 8]); maskT = di("maskT", [128, 128]); invf2 = di("invf2", [128, 128]); offs = di("offs", [128, 128])
    gn = di("gn", [128, 256]); bf = di("bf", [128, 2]); triU = di("triU", [128, 128]); ones = di("ones", [128, 128]); sel = di("sel", [128, 128])
    with ExitStack() as es:
        cx = Ctx(nc, es)
        d_fqk = cx.dram("fqk", [4, 128, S], BF16); d_rqkv = cx.dram("rqkv", [S, 512], F32); d_rgfv = cx.dram("rgfv", [S, 512], F32); d_ff = cx.dram("ffl", [S, 2], F32)
        d_yret = cx.dram("yret", [S, 256], BF16, kind="ExternalOutput"); d_yfox = cx.dram("yfox", [S, 256], BF16, kind="ExternalOutput")
        emit_inproj(cx, nc, S, x, wc, modT, n1T, ident, d_fqk, d_rqkv, d_rgfv, d_ff)
        emit_ret(cx, nc, S, d_rqkv, d_rgfv, posT, rc, maskT, invf2, offs, gn, ident, d_yret)
        emit_fox(cx, nc, S, d_fqk, d_rgfv, d_ff, bf, triU, ones, sel, d_yfox)
        cx.barrier()
        cx.finish([d_yret, d_yfox])
    return nc


def build_l2(TPC):
    nc = bass.Bass("TRN2", target_bir_lowering=False)
    di = lambda n, s, d=F32: nc.dram_tensor(n, s, d, kind="ExternalInput").ap()
    with ExitStack() as es:
        cx = Ctx(nc, es)
        x = di("x", [TPC, 4096]); yret = Buf(di("yret", [TPC, 2048], BF16)); yfox = Buf(di("yfox", [TPC, 2048], BF16))
        mod_row = di("mod_row", [1, 24576]); modT = di("modT", [128, 192]); n2T = di("n2T", [128, 32]); gfT = di("gfT", [128, 16]); fg_row = di("fg_row", [1, 4096])
        Wo = di("Wo", [4096, 4096]); Wq = di("Wq", [16, 128, 32, 128]); skT = di("skT", [16, 128, 128]); uT = di("uT", [128, 128, 32, 128]); v = di("v", [128, 128, 4096]); ident = di("ident", [128, 128])
        d_x2 = cx.dram("x2", [TPC, 4096], F32); d_x3 = cx.dram("x3", [TPC, 4096], F32)
        out = cx.dram("out", [TPC, 4096], F32, kind="ExternalOutput")
        emit_l2(cx, nc, TPC, x, yret, yfox, mod_row, modT, n2T, gfT, fg_row, Wo, Wq, skT, uT, v, ident, d_x2, d_x3, out)
        cx.barrier()
        cx.finish([out])
    return nc


def kernel(x, c, positions, w_ada, b_ada, norm1_g, w_in, b_forget, ret_gn_g, fox_norm_g, w_out, norm2_g, w_peer_q, peer_sub_keys, peer_u, peer_v, final_g):
    f32 = lambda a: np.ascontiguousarray(np.asarray(a, dtype=np.float32))
    x = f32(x); S = x.shape[1]; TPC = S // 8
    col = lambda v_: np.ascontiguousarray(np.asarray(v_, np.float32).reshape(-1, 128).T)
    cores = list(range(8))
    w_ada0 = np.asarray(w_ada, np.float32)[0]; b_ada0 = np.asarray(b_ada, np.float32)[0]
    nc0 = build_l0()
    maps = [{"cT": col(np.asarray(c, np.float32)[0]), "w": np.ascontiguousarray(w_ada0[:, i * 3072:(i + 1) * 3072]), "b": np.ascontiguousarray(b_ada0[None, i * 3072:(i + 1) * 3072])} for i in cores]
    r0 = run_bass_kernel_spmd(nc0, maps, core_ids=cores)
    mod = np.concatenate([np.asarray(r["mod"])[0] for r in r0.results])
    modT = col(mod)
    ident = np.eye(128, dtype=np.float32)
    w_in0 = np.asarray(w_in, np.float32)[0]
    pos = np.asarray(positions, np.int32)[0]
    invf2, offs = rot_consts(); triU, ones, sel = fox_consts()
    nc1 = build_l1(S)
    maps = []
    for i in cores:
        rc, maskT = ret_consts(i)
        maps.append({"x": x[0], "wc": np.ascontiguousarray(w_in0[:, core_cols(i)]), "modT": modT, "n1T": col(np.asarray(norm1_g)[0]), "ident": ident,
                     "posT": np.ascontiguousarray(pos.reshape(-1, 128).T), "rc": rc, "maskT": maskT, "invf2": invf2, "offs": offs,
                     "gn": np.ascontiguousarray(np.tile(np.asarray(ret_gn_g, np.float32)[0][None, i * 256:(i + 1) * 256], (128, 1))),
                     "bf": np.ascontiguousarray(np.tile(np.asarray(b_forget, np.float32)[0][None, 2 * i:2 * i + 2], (128, 1))), "triU": triU, "ones": ones, "sel": sel})
    r1 = run_bass_kernel_spmd(nc1, maps, core_ids=cores)
    yret = np.concatenate([np.asarray(r["yret"]) for r in r1.results], axis=1)
    yfox = np.concatenate([np.asarray(r["yfox"]) for r in r1.results], axis=1)
    u = np.asarray(peer_u, np.float32)[0]; v = np.asarray(peer_v, np.float32)[0]
    uT = np.ascontiguousarray(u.reshape(128, 128, 32, 128).transpose(1, 3, 2, 0))
    vr = np.ascontiguousarray(v.reshape(128, 128, 4096).transpose(1, 0, 2))
    skT = np.ascontiguousarray(np.asarray(peer_sub_keys, np.float32)[0].reshape(16, 128, 128).transpose(0, 2, 1))
    wm = {"mod_row": np.ascontiguousarray(mod[None, :]), "modT": modT, "n2T": col(np.asarray(norm2_g)[0]), "gfT": col(np.asarray(fox_norm_g)[0]),
          "fg_row": np.ascontiguousarray(np.asarray(final_g, np.float32)[None, :]), "Wo": f32(np.asarray(w_out)[0]), "Wq": np.ascontiguousarray(np.asarray(w_peer_q, np.float32)[0].reshape(32, 128, 16, 128).transpose(2, 1, 0, 3)),
          "skT": skT, "uT": uT, "v": vr, "ident": ident}
    nc2 = build_l2(TPC)
    maps = []
    for i in cores:
        sl = slice(i * TPC, (i + 1) * TPC)
        m = dict(wm); m.update({"x": np.ascontiguousarray(x[0, sl]), "yret": np.ascontiguousarray(yret[sl]), "yfox": np.ascontiguousarray(yfox[sl])})
        maps.append(m)
    r2 = run_bass_kernel_spmd(nc2, maps, core_ids=cores)
    out = np.concatenate([np.asarray(r["out"]) for r in r2.results], axis=0)
    return out.reshape(1, S, 4096).astype(np.float32)
```

```python
import numpy as np
from contextlib import ExitStack
import concourse.bass as bass
import concourse.mybir as mybir
from concourse.bass_utils import run_bass_kernel_spmd

F32 = mybir.dt.float32
BF16 = mybir.dt.bfloat16
I32 = mybir.dt.int32
U32 = mybir.dt.uint32
ALU = mybir.AluOpType
AF = mybir.ActivationFunctionType
AX = mybir.AxisListType


class Tok:
    __slots__ = ("sem", "val", "key")
    def __init__(self, sem, val, key):
        self.sem = sem; self.val = val; self.key = key


class Buf:
    def __init__(self, t, name=""):
        self.t = t
        self.name = name
        self.writers = {}
        self.readers = {}
    def __getitem__(self, idx):
        return self.t[idx]


class Slot:
    def __init__(self, sem, key):
        self.sem = sem; self.total = 0; self.key = key


class Eng:
    def __init__(self, h, sem, key, is_pe=False):
        self.h = h; self.sem = sem; self.count = 0; self.key = key
        self.seen = {}
        self.is_pe = is_pe
    def wait(self, tok, same_ok=False):
        if tok is None:
            return
        if tok.key == self.key and (self.is_pe or same_ok):
            return
        if self.seen.get(tok.key, 0) >= tok.val:
            return
        self.h.wait_ge(tok.sem, tok.val)
        self.seen[tok.key] = tok.val
    def _deps(self, reads, writes):
        for b in reads:
            for t in b.writers.values():
                self.wait(t)
        for b in writes:
            for t in b.writers.values():
                self.wait(t, same_ok=True)
            for t in b.readers.values():
                self.wait(t, same_ok=True)
    def _commit(self, tok, reads, writes):
        for b in reads:
            b.readers[tok.key] = tok
        for b in writes:
            b.writers[tok.key] = tok
            b.readers = {}
    def op(self, fn, reads=(), writes=()):
        self._deps(reads, writes)
        inst = fn()
        self.count += 1
        inst.then_inc(self.sem, 1)
        tok = Tok(self.sem, self.count, self.key)
        self._commit(tok, reads, writes)
        return tok
    def dma(self, out, in_, slot, reads=(), writes=(), cont=False, **kw):
        self._deps(reads, writes)
        if not cont:
            self.wait(Tok(slot.sem, slot.total, slot.key))
        inst = self.h.dma_start(out=out, in_=in_, **kw)
        slot.total += 16
        inst.then_inc(slot.sem, 16)
        tok = Tok(slot.sem, slot.total, slot.key)
        self._commit(tok, reads, writes)
        return tok


class Ctx:
    def __init__(self, nc, es):
        self.nc = nc; self.es = es
        self._n = 0
        def mk(h, name, is_pe=False):
            sem = es.enter_context(nc.semaphore("e_" + name))
            return Eng(h, sem, "e_" + name, is_pe)
        self.pe = mk(nc.tensor, "pe", True)
        self.act = mk(nc.scalar, "act")
        self.dve = mk(nc.vector, "dve")
        self.pool = mk(nc.gpsimd, "pool")
        self.sp = mk(nc.sync, "sp")
        self.engs = [self.pe, self.act, self.dve, self.pool, self.sp]
        self.slots = []
    def new_epoch(self):
        self._site_cnt = {}
    def slot(self, name=None):
        import sys as _sys
        ln = _sys._getframe(1).f_lineno
        if not hasattr(self, "_site_cnt"):
            self._site_cnt = {}; self._site_cache = {}
        k = self._site_cnt.get(ln, 0); self._site_cnt[ln] = k + 1
        if (ln, k) in self._site_cache:
            return self._site_cache[(ln, k)]
        sl = self._slot_new(name)
        self._site_cache[(ln, k)] = sl
        return sl
    def _slot_new(self, name=None):
        self._n += 1
        name = name or f"s{self._n}"
        sem = self.es.enter_context(self.nc.semaphore("sl_" + name + f"_{self._n}"))
        sl = Slot(sem, "sl_" + name + f"_{self._n}")
        self.slots.append(sl)
        return sl
    def sbuf(self, es, name, shape, dt):
        self._n += 1
        t = es.enter_context(self.nc.sbuf_tensor(f"{name}_{self._n}", list(shape), dt))
        return Buf(t, name)
    def psum(self, es, name, shape, dt=F32):
        self._n += 1
        t = es.enter_context(self.nc.psum_tensor(f"{name}_{self._n}", list(shape), dt))
        return Buf(t, name)
    def dram(self, name, shape, dt, kind="Internal"):
        t = self.nc.dram_tensor(name, list(shape), dt, kind=kind)
        return Buf(t.ap(), name)
    def barrier(self):
        toks = [Tok(e.sem, e.count, e.key) for e in self.engs]
        toks += [Tok(s.sem, s.total, s.key) for s in self.slots]
        for e in self.engs:
            for t in toks:
                if t.key != e.key and t.val > 0:
                    e.wait(t)
    def finish(self, bufs):
        for b in bufs:
            for t in b.writers.values():
                self.sp.wait(t)

import math

def build_l0(NCOL=3072):
    nc = bass.Bass("TRN2", target_bir_lowering=False)
    cT = nc.dram_tensor("cT", [128, 32], F32, kind="ExternalInput").ap()
    w = nc.dram_tensor("w", [4096, NCOL], F32, kind="ExternalInput").ap()
    b = nc.dram_tensor("b", [1, NCOL], F32, kind="ExternalInput").ap()
    o = nc.dram_tensor("mod", [1, NCOL], F32, kind="ExternalOutput").ap()
    NB = NCOL // 512
    with ExitStack() as es:
        cx = Ctx(nc, es)
        c_sb = cx.sbuf(es, "c", [128, 32], F32)
        ca = cx.sbuf(es, "ca", [128, 32], F32)
        bs = cx.sbuf(es, "b", [1, NCOL], F32)
        res = cx.sbuf(es, "res", [1, NCOL], F32)
        wb = [cx.sbuf(es, f"w{i}", [128, 32, 512], F32) for i in range(2)]
        ws = [cx.slot() for _ in range(2)]
        ps = [cx.psum(es, f"ps{i}", [1, 512]) for i in range(2)]
        s0 = cx.slot()
        cx.sp.dma(c_sb[:], cT, s0, writes=[c_sb])
        cx.sp.dma(bs[:], b, s0, writes=[bs], cont=True)
        cx.act.op(lambda: nc.scalar.activation(out=ca[:], in_=c_sb[:], func=AF.Silu), reads=[c_sb], writes=[ca])
        wv = w.rearrange("(k p) n -> p k n", p=128)
        for j in range(NB):
            wt = wb[j % 2]
            cx.sp.dma(wt[:], wv[:, :, j * 512:(j + 1) * 512], ws[j % 2], writes=[wt])
            p = ps[j % 2]
            for k in range(32):
                cx.pe.op(lambda: nc.tensor.matmul(p[:], lhsT=ca[:, k:k + 1], rhs=wt[:, k, :], start=(k == 0), stop=(k == 31)),
                         reads=[ca, wt], writes=[p])
            cx.dve.op(lambda: nc.vector.tensor_tensor(out=res[:, j * 512:(j + 1) * 512], in0=p[:], in1=bs[:, j * 512:(j + 1) * 512], op=ALU.add),
                      reads=[p, bs], writes=[res])
        so = cx.slot()
        ob = Buf(o, 'o')
        cx.sp.dma(o, res[:], so, reads=[res], writes=[ob])
        cx.finish([ob])
    return nc


NW = 1538
EPS = 1e-6

def core_cols(i):
    r = lambda a, n: list(range(a, a + n))
    fm = r(6144 + 256 * i, 256) + r(8192 + 256 * i, 256)
    tm = r(128 * i, 128) + r(1024 + 128 * i, 128) + r(2048 + 256 * i, 256) + r(4096 + 256 * i, 256) + r(10240 + 256 * i, 256) + r(12288 + 2 * i, 2)
    return np.array(fm + tm)


def emit_inproj(cx, nc, S, x, wc, modT_d, n1T_d, ident, d_fqk, d_rqkv, d_rgfv, d_ff, d_dbg=None):
    NT = S // 128
    NBLK = S // 512
    with ExitStack() as es:
        Wb = cx.sbuf(es, "Wb", [128, 32, NW], BF16)
        idt = cx.sbuf(es, "ident", [128, 128], F32)
        gp = cx.sbuf(es, "gp", [128, 32], F32)
        sh = cx.sbuf(es, "sh", [128, 32], F32)
        sW = cx.sbuf(es, "sW", [1, NW], F32)
        ones_r = cx.sbuf(es, "ones", [1, 128], F32)
        ss = cx.sbuf(es, "ss", [128, NT], F32)
        rs = cx.sbuf(es, "rs", [128, NT], F32)
        irs = cx.sbuf(es, "irs", [128, NT], F32)
        banks = [cx.psum(es, f"bank{i}", [128, 512]) for i in range(8)]
        s0 = cx.slot()
        cx.sp.dma(idt[:], ident, s0, writes=[idt])
        modT = cx.sbuf(es, "modT1", [128, 192], F32)
        n1T = cx.sbuf(es, "n1T", [128, 32], F32)
        cx.sp.dma(modT[:], modT_d, s0, writes=[modT], cont=True)
        cx.sp.dma(n1T[:], n1T_d, s0, writes=[n1T], cont=True)
        ft = Tok(s0.sem, s0.total, s0.key)
        for dst in (idt, modT, n1T):
            dst.writers[s0.key] = ft
        cx.dve.op(lambda: nc.vector.scalar_tensor_tensor(out=gp[:], in0=modT[:, 32:64], scalar=1.0, in1=n1T[:], op0=ALU.add, op1=ALU.mult), reads=[modT, n1T], writes=[gp])
        cx.dve.op(lambda: nc.vector.tensor_copy(out=sh[:], in_=modT[:, 0:32]), reads=[modT], writes=[sh])
        cx.dve.op(lambda: nc.vector.memset(ones_r[:], 1.0), writes=[ones_r])
        cx.dve.op(lambda: nc.vector.memset(ss[:], 0.0), writes=[ss])
        epsc = cx.sbuf(es, "epsc", [128, 1], F32)
        cx.dve.op(lambda: nc.vector.memset(epsc[:], EPS), writes=[epsc])
        with ExitStack() as es2:
            stg = [cx.sbuf(es2, f"wst{i}", [128, 4, NW], F32) for i in range(2)]
            sl = [cx.slot() for _ in range(2)]
            wv = wc.rearrange("(k p) n -> p k n", p=128)
            nsl = [(0, 512), (512, 512), (1024, 512), (1536, 2)]
            for g in range(8):
                st = stg[g % 2]
                cx.sp.dma(st[:], wv[:, 4 * g:4 * g + 4, :], sl[g % 2], writes=[st])
                for kk in range(4):
                    kc = 4 * g + kk
                    for bi, (a, n) in enumerate(nsl):
                        cx.pe.op(lambda: nc.tensor.matmul(banks[bi][0:1, 0:n], lhsT=sh[:, kc:kc + 1], rhs=st[:, kk, a:a + n],
                                                          start=(kc == 0), stop=(kc == 31)), reads=[sh, st], writes=[banks[bi]])
                    if kk % 2 == 0:
                        cx.dve.op(lambda: nc.vector.tensor_scalar(out=Wb[:, kc, :], in0=st[:, kk, :], scalar1=gp[:, kc:kc + 1], scalar2=None, op0=ALU.mult),
                                  reads=[st, gp], writes=[Wb])
                    else:
                        cx.act.op(lambda: nc.scalar.activation(out=Wb[:, kc, :], in_=st[:, kk, :], func=AF.Copy, scale=gp[:, kc:kc + 1]),
                                  reads=[st, gp], writes=[Wb])
            for bi, (a, n) in enumerate(nsl):
                cx.act.op(lambda: nc.scalar.copy(out=sW[0:1, a:a + n], in_=banks[bi][0:1, 0:n]), reads=[banks[bi]], writes=[sW])
        cx.barrier()
        if d_dbg is not None:
            cx.sp.dma(d_dbg[:, :], sW[:], cx.slot(), reads=[sW], writes=[d_dbg])
        with ExitStack() as es2:
            xt = [cx.sbuf(es2, f"xt{i}", [128, 4096], F32) for i in range(2)]
            xsl = [cx.slot() for _ in range(2)]
            junk = cx.sbuf(es2, "junk", [128, 4096], BF16)
            xT = [cx.sbuf(es2, f"xT{i}", [128, 32, 512], BF16) for i in range(1)]
            rrow = [cx.sbuf(es2, f"rrow{i}", [1, 512], F32) for i in range(2)]
            rbc = [cx.sbuf(es2, f"rbc{i}", [128, 512], F32) for i in range(2)]
            rrow2 = [cx.sbuf(es2, f"rrowb{i}", [1, 512], F32) for i in range(2)]
            ofm = [cx.sbuf(es2, f"ofm{i}", [128, 4, 512], BF16) for i in range(2)]
            otm = [cx.sbuf(es2, f"otm{i}", [128, 1026], F32) for i in range(2)]
            osl = [cx.slot() for _ in range(4)]
            pT = banks[0:2]; pFM = banks[2:4]; pTM = banks[4:7]; pM = banks[7]
            xv = x.rearrange("(n p) d -> n p d", p=128)
            for blk in range(NBLK):
                xTb = xT[0]
                for tt in range(4):
                    ti = blk * 4 + tt
                    xb = xt[ti % 2]
                    cx.sp.dma(xb[:], xv[ti], xsl[ti % 2], writes=[xb])
                    cx.act.op(lambda: nc.scalar.activation(out=junk[:], in_=xb[:], func=AF.Square, accum_out=ss[:, ti:ti + 1]),
                              reads=[xb], writes=[junk, ss])
                    cx.act.op(lambda: nc.scalar.activation(out=irs[:, ti:ti + 1], in_=ss[:, ti:ti + 1], func=AF.Sqrt, bias=epsc[:, 0:1], scale=1.0 / 4096),
                              reads=[ss, epsc], writes=[irs])
                    cx.dve.op(lambda: nc.vector.reciprocal(out=rs[:, ti:ti + 1], in_=irs[:, ti:ti + 1]), reads=[irs], writes=[rs])
                    for g in range(8):
                        pb = pT[g % 2]
                        for kk in range(4):
                            kc = 4 * g + kk
                            cx.pe.op(lambda: nc.tensor.transpose(out=pb[:, kk * 128:(kk + 1) * 128], in_=xb[:, kc * 128:(kc + 1) * 128], identity=idt[:]),
                                     reads=[xb, idt], writes=[pb])
                        cx.dve.op(lambda: nc.vector.tensor_copy(out=xTb[:, 4 * g:4 * g + 4, tt * 128:(tt + 1) * 128],
                                                                in_=pb[:].rearrange("p (a b) -> p a b", a=4)), reads=[pb], writes=[xTb])
                    for (col, row) in ((irs, rrow[blk % 2]), (rs, rrow2[blk % 2])):
                        cx.pe.op(lambda: nc.tensor.transpose(out=pM[0:1, 0:128], in_=col[:, ti:ti + 1], identity=idt[:]), reads=[col, idt], writes=[pM])
                        cx.act.op(lambda: nc.scalar.copy(out=row[0:1, tt * 128:(tt + 1) * 128], in_=pM[0:1, 0:128]), reads=[pM], writes=[row])
                cx.pe.op(lambda: nc.tensor.matmul(pM[:, :], lhsT=ones_r[0:1, :], rhs=rrow2[blk % 2][0:1, :], start=True, stop=True),
                         reads=[ones_r, rrow2[blk % 2]], writes=[pM])
                cx.act.op(lambda: nc.scalar.copy(out=rbc[blk % 2][:], in_=pM[:, :]), reads=[pM], writes=[rbc[blk % 2]])
                of = ofm[blk % 2]
                for j in range(4):
                    pf = pFM[j % 2]
                    for kc in range(32):
                        cx.pe.op(lambda: nc.tensor.matmul(pf[:, :], lhsT=Wb[:, kc, j * 128:(j + 1) * 128], rhs=xTb[:, kc, :], start=(kc == 0), stop=False),
                                 reads=[Wb, xTb], writes=[pf])
                    cx.pe.op(lambda: nc.tensor.matmul(pf[:, :], lhsT=sW[0:1, j * 128:(j + 1) * 128], rhs=rrow[blk % 2][0:1, :], start=False, stop=True),
                             reads=[sW, rrow[blk % 2]], writes=[pf])
                    cx.dve.op(lambda: nc.vector.tensor_tensor(out=of[:, j, :], in0=pf[:, :], in1=rbc[blk % 2][:], op=ALU.mult),
                              reads=[pf, rbc[blk % 2]], writes=[of])
                cx.sp.dma(d_fqk[:, :, blk * 512:(blk + 1) * 512].rearrange("j p t -> p j t"), of[:], osl[blk % 2], reads=[of], writes=[d_fqk])
                for tt in range(4):
                    ti = blk * 4 + tt
                    ot = otm[ti % 2]
                    for bi, (a, n) in enumerate(((512, 512), (1024, 512), (1536, 2))):
                        pt = pTM[bi]
                        for kc in range(32):
                            cx.pe.op(lambda: nc.tensor.matmul(pt[:, 0:n], lhsT=xTb[:, kc, tt * 128:(tt + 1) * 128], rhs=Wb[:, kc, a:a + n], start=(kc == 0), stop=False),
                                     reads=[Wb, xTb], writes=[pt])
                        cx.pe.op(lambda: nc.tensor.matmul(pt[:, 0:n], lhsT=rrow[blk % 2][0:1, tt * 128:(tt + 1) * 128], rhs=sW[0:1, a:a + n], start=False, stop=True),
                                 reads=[sW, rrow[blk % 2]], writes=[pt])
                        cx.act.op(lambda: nc.scalar.activation(out=ot[:, a - 512:a - 512 + n], in_=pt[:, 0:n], func=AF.Copy, scale=rs[:, ti:ti + 1]),
                                  reads=[pt, rs], writes=[ot])
                    sl_ = osl[2 + ti % 2]
                    cx.sp.dma(d_rqkv[ti * 128:(ti + 1) * 128, :], ot[:, 0:512], sl_, reads=[ot], writes=[d_rqkv])
                    cx.sp.dma(d_rgfv[ti * 128:(ti + 1) * 128, :], ot[:, 512:1024], sl_, reads=[ot], writes=[d_rgfv], cont=True)
                    cx.sp.dma(d_ff[ti * 128:(ti + 1) * 128, :], ot[:, 1024:1026], sl_, reads=[ot], writes=[d_ff], cont=True)
        cx.barrier()


def ret_consts(h):
    lg = math.log1p(-2.0 ** (-5.0 - h))
    c = np.arange(128, dtype=np.float64)
    t = np.zeros((128, 8), np.float32)
    t[:, 0] = np.exp((c + 1) * lg)
    t[:, 1] = -np.exp((c + 1) * lg)
    t[:, 2] = 128 ** -0.5
    t[:, 3] = -(128 ** -0.5)
    t[:, 4] = np.exp((127 - c) * lg) * 128 ** -0.5
    t[:, 5] = -np.exp((127 - c) * lg) * 128 ** -0.5
    t[:, 6] = np.exp(128 * lg)
    t[:, 7] = -math.pi
    s = c[:, None]; cc = c[None, :]
    maskT = np.where(cc >= s, np.exp((-s - 1) * lg) * np.ones_like(cc), 0.0).astype(np.float32)
    return t, maskT

def rot_consts():
    half = 64
    inv_freq = (10000.0 ** (-np.arange(half, dtype=np.float32) / half)).astype(np.float32)
    invf2 = np.tile(np.concatenate([inv_freq, inv_freq])[None, :], (128, 1)).astype(np.float32)
    offs = np.tile(np.concatenate([np.zeros(64), np.full(64, math.pi / 2)])[None, :], (128, 1)).astype(np.float32)
    return invf2, offs


def emit_ret(cx, nc, S, d_rqkv, d_rgfv, posT, rc, maskT_d, invf2_d, offs_d, gn_d, ident, d_yret, dbg=None):
    NT = S // 128
    TWO_PI = 2 * math.pi
    with ExitStack() as es:
        idt = cx.sbuf(es, "ident", [128, 128], F32)
        rcs = cx.sbuf(es, "rc", [128, 8], F32)
        mk = cx.sbuf(es, "maskT", [128, 128], F32)
        invf2 = cx.sbuf(es, "invf2", [128, 128], F32)
        offs = cx.sbuf(es, "offs", [128, 128], F32)
        gn = cx.sbuf(es, "gn", [128, 256], F32)
        posi = cx.sbuf(es, "posi", [128, NT], I32)
        posf = cx.sbuf(es, "posf", [128, NT], F32)
        epsc = cx.sbuf(es, "epsc", [128, 1], F32)
        state = cx.sbuf(es, "state", [128, 256], F32)
        state_bf = cx.sbuf(es, "state_bf", [128, 256], BF16)
        banks = [cx.psum(es, f"rbank{i}", [128, 512]) for i in range(8)]
        s0 = cx.slot()
        for i, (dst, src) in enumerate(((idt, ident), (rcs, rc), (mk, maskT_d), (invf2, invf2_d), (offs, offs_d), (gn, gn_d), (posi, posT))):
            cx.sp.dma(dst[:], src, s0, writes=[dst], cont=(i > 0))
        ft = Tok(s0.sem, s0.total, s0.key)
        for dst in (idt, rcs, mk, invf2, offs, gn, posi):
            dst.writers[s0.key] = ft
        cx.dve.op(lambda: nc.vector.tensor_copy(out=posf[:], in_=posi[:]), reads=[posi], writes=[posf])
        cx.dve.op(lambda: nc.vector.memset(epsc[:], 1e-6), writes=[epsc])
        NB = 2
        qkv = [cx.sbuf(es, f"qkv{i}", [128, 512], F32) for i in range(NB)]
        rg = [cx.sbuf(es, f"rg{i}", [128, 256], F32) for i in range(NB)]
        lsl = [cx.slot() for _ in range(NB)]
        ang = cx.sbuf(es, "ang", [128, 128], F32)
        sc = cx.sbuf(es, "sc", [128, 128], F32)
        tq = cx.sbuf(es, "tq", [128, 128], F32)
        ki = cx.sbuf(es, "ki", [128, 128], I32)
        tmp = [cx.sbuf(es, f"rt{i}", [128, 64], F32) for i in range(4)]
        qd = cx.sbuf(es, "qd", [128, 128], F32)
        kr = cx.sbuf(es, "kr", [128, 128], F32)
        kd = cx.sbuf(es, "kd", [128, 128], BF16)
        vb = cx.sbuf(es, "vb", [128, 256], BF16)
        qdT = cx.sbuf(es, "qdT", [128, 128], BF16)
        krT = cx.sbuf(es, "krT", [128, 128], BF16)
        scT = cx.sbuf(es, "scT", [128, 128], BF16)
        o = cx.sbuf(es, "o", [128, 256], F32)
        junk = cx.sbuf(es, "rjunk", [128, 256], F32)
        st = cx.sbuf(es, "st", [128, 8], F32)
        sg = cx.sbuf(es, "sg", [128, 256], F32)
        yo = [cx.sbuf(es, f"yo{i}", [128, 256], BF16) for i in range(2)]
        osl = [cx.slot() for _ in range(2)]
        pqT, pkT, pS, pKV, pO = banks[0], banks[1], banks[2], banks[3], banks[4]
        V = nc.vector
        for n in range(NT):
            t_ = qkv[n % NB]; g_ = rg[n % NB]
            cx.sp.dma(t_[:], d_rqkv[n * 128:(n + 1) * 128, :], lsl[n % NB], reads=[d_rqkv], writes=[t_])
            cx.sp.dma(g_[:], d_rgfv[n * 128:(n + 1) * 128, 0:256], lsl[n % NB], reads=[d_rgfv], writes=[g_], cont=True)
            ft = Tok(lsl[n % NB].sem, lsl[n % NB].total, lsl[n % NB].key)
            t_.writers[ft.key] = ft
            cx.dve.op(lambda: V.scalar_tensor_tensor(out=ang[:], in0=invf2[:], scalar=posf[:, n:n + 1], in1=offs[:], op0=ALU.mult, op1=ALU.add),
                      reads=[invf2, posf, offs], writes=[ang])
            cx.dve.op(lambda: V.tensor_scalar(out=tq[:], in0=ang[:], scalar1=1.0 / TWO_PI, scalar2=None, op0=ALU.mult), reads=[ang], writes=[tq])
            cx.dve.op(lambda: V.tensor_copy(out=ki[:], in_=tq[:]), reads=[tq], writes=[ki])
            cx.dve.op(lambda: V.tensor_copy(out=tq[:], in_=ki[:]), reads=[ki], writes=[tq])
            cx.dve.op(lambda: V.scalar_tensor_tensor(out=ang[:], in0=tq[:], scalar=-6.28125, in1=ang[:], op0=ALU.mult, op1=ALU.add), reads=[tq, ang], writes=[ang])
            cx.dve.op(lambda: V.scalar_tensor_tensor(out=ang[:], in0=tq[:], scalar=-0.0019353071795864769, in1=ang[:], op0=ALU.mult, op1=ALU.add), reads=[tq, ang], writes=[ang])
            cx.dve.op(lambda: V.tensor_scalar(out=ang[:], in0=ang[:], scalar1=3.1415925, scalar2=-3.1415925, op0=ALU.min, op1=ALU.max), reads=[ang], writes=[ang])
            cx.act.op(lambda: nc.scalar.activation(out=sc[:], in_=ang[:], func=AF.Sin), reads=[ang], writes=[sc])
            sinp = sc[:, 0:64]; cosp = sc[:, 64:128]
            for (base, outs) in ((0, ((qd, 0, 1),)), (128, ((kr, 2, 3), (kd, 4, 5)))):
                x1 = t_[:, base:base + 64]; x2 = t_[:, base + 64:base + 128]
                cx.dve.op(lambda: V.tensor_tensor(out=tmp[0][:], in0=x1, in1=cosp, op=ALU.mult), reads=[t_, sc], writes=[tmp[0]])
                cx.dve.op(lambda: V.tensor_tensor(out=tmp[1][:], in0=x2, in1=sinp, op=ALU.mult), reads=[t_, sc], writes=[tmp[1]])
                cx.dve.op(lambda: V.tensor_tensor(out=tmp[1][:], in0=tmp[0][:], in1=tmp[1][:], op=ALU.subtract), reads=[tmp[0], tmp[1]], writes=[tmp[1]])
                cx.dve.op(lambda: V.tensor_tensor(out=tmp[2][:], in0=x1, in1=sinp, op=ALU.mult), reads=[t_, sc], writes=[tmp[2]])
                cx.dve.op(lambda: V.tensor_tensor(out=tmp[3][:], in0=x2, in1=cosp, op=ALU.mult), reads=[t_, sc], writes=[tmp[3]])
                cx.dve.op(lambda: V.tensor_tensor(out=tmp[3][:], in0=tmp[3][:], in1=tmp[2][:], op=ALU.add), reads=[tmp[2], tmp[3]], writes=[tmp[3]])
                for (dst, cp, cn) in outs:
                    cx.dve.op(lambda: V.tensor_scalar(out=dst[:, 0:64], in0=tmp[1][:], scalar1=rcs[:, cp:cp + 1], scalar2=None, op0=ALU.mult), reads=[tmp[1], rcs], writes=[dst])
                    cx.dve.op(lambda: V.tensor_scalar(out=dst[:, 64:128], in0=tmp[3][:], scalar1=rcs[:, cp:cp + 1], scalar2=None, op0=ALU.mult), reads=[tmp[3], rcs], writes=[dst])
            cx.act.op(lambda: nc.scalar.copy(out=vb[:], in_=t_[:, 256:512]), reads=[t_], writes=[vb])
            cx.pe.op(lambda: nc.tensor.transpose(out=pqT[:, 0:128], in_=qd[:], identity=idt[:]), reads=[qd, idt], writes=[pqT])
            cx.act.op(lambda: nc.scalar.copy(out=qdT[:], in_=pqT[:, 0:128]), reads=[pqT], writes=[qdT])
            cx.pe.op(lambda: nc.tensor.transpose(out=pkT[:, 0:128], in_=kr[:], identity=idt[:]), reads=[kr, idt], writes=[pkT])
            cx.act.op(lambda: nc.scalar.copy(out=krT[:], in_=pkT[:, 0:128]), reads=[pkT], writes=[krT])
            cx.pe.op(lambda: nc.tensor.matmul(pS[:, 0:128], lhsT=krT[:], rhs=qdT[:], start=True, stop=True), reads=[krT, qdT], writes=[pS])
            cx.dve.op(lambda: V.tensor_tensor(out=scT[:], in0=pS[:, 0:128], in1=mk[:], op=ALU.mult), reads=[pS, mk], writes=[scT])
            cx.pe.op(lambda: nc.tensor.matmul(pKV[:, 0:256], lhsT=kd[:], rhs=vb[:], start=True, stop=True), reads=[kd, vb], writes=[pKV])
            if n > 0:
                cx.pe.op(lambda: nc.tensor.matmul(pO[:, 0:256], lhsT=qdT[:], rhs=state_bf[:], start=True, stop=False), reads=[qdT, state_bf], writes=[pO])
            cx.pe.op(lambda: nc.tensor.matmul(pO[:, 0:256], lhsT=scT[:], rhs=vb[:], start=(n == 0), stop=True), reads=[scT, vb], writes=[pO])
            if n == 0:
                cx.dve.op(lambda: V.tensor_copy(out=state[:], in_=pKV[:, 0:256]), reads=[pKV], writes=[state])
            else:
                cx.dve.op(lambda: V.scalar_tensor_tensor(out=state[:], in0=state[:], scalar=rcs[:, 6:7], in1=pKV[:, 0:256], op0=ALU.mult, op1=ALU.add),
                          reads=[state, rcs, pKV], writes=[state])
            cx.act.op(lambda: nc.scalar.copy(out=state_bf[:], in_=state[:]), reads=[state], writes=[state_bf])
            cx.dve.op(lambda: V.memset(st[:], 0.0), writes=[st])
            cx.act.op(lambda: nc.scalar.activation(out=o[:], in_=pO[:, 0:256], func=AF.Identity, accum_out=st[:, 0:1]), reads=[pO, st], writes=[o, st])
            cx.act.op(lambda: nc.scalar.activation(out=junk[:], in_=pO[:, 0:256], func=AF.Square, accum_out=st[:, 1:2]), reads=[pO, st], writes=[junk, st])
            cx.dve.op(lambda: V.tensor_scalar(out=st[:, 2:3], in0=st[:, 0:1], scalar1=1.0 / 256, scalar2=None, op0=ALU.mult), reads=[st], writes=[st])
            cx.dve.op(lambda: V.tensor_tensor(out=st[:, 3:4], in0=st[:, 2:3], in1=st[:, 2:3], op=ALU.mult), reads=[st], writes=[st])
            cx.dve.op(lambda: V.scalar_tensor_tensor(out=st[:, 4:5], in0=st[:, 1:2], scalar=1.0 / 256, in1=st[:, 3:4], op0=ALU.mult, op1=ALU.subtract),
                      reads=[st], writes=[st])
            cx.act.op(lambda: nc.scalar.activation(out=st[:, 5:6], in_=st[:, 4:5], func=AF.Sqrt, bias=epsc[:, 0:1], scale=1.0), reads=[st, epsc], writes=[st])
            cx.dve.op(lambda: V.reciprocal(out=st[:, 6:7], in_=st[:, 5:6]), reads=[st], writes=[st])
            cx.dve.op(lambda: V.tensor_scalar(out=o[:], in0=o[:], scalar1=st[:, 2:3], scalar2=st[:, 6:7], op0=ALU.subtract, op1=ALU.mult), reads=[o, st], writes=[o])
            cx.act.op(lambda: nc.scalar.activation(out=sg[:], in_=g_[:], func=AF.Silu), reads=[g_], writes=[sg])
            cx.dve.op(lambda: V.tensor_tensor(out=o[:], in0=o[:], in1=gn[:], op=ALU.mult), reads=[o, gn], writes=[o])
            y_ = yo[n % 2]
            cx.dve.op(lambda: V.tensor_tensor(out=y_[:], in0=o[:], in1=sg[:], op=ALU.mult), reads=[o, sg], writes=[y_])
            cx.sp.dma(d_yret[n * 128:(n + 1) * 128, :], y_[:], osl[n % 2], reads=[y_], writes=[d_yret])
        cx.barrier()

def fox_consts():
    s = np.arange(128)[:, None]; c = np.arange(128)[None, :]
    triU = (s <= c).astype(np.float32)
    ones = np.ones((128, 128), np.float32)
    sel = np.zeros((128, 128), np.float32); sel[64, :] = 1.0
    return triU, ones, sel


def emit_fox(cx, nc, S, d_fqk, d_rgfv, d_ff, bf_d, triU_d, ones_d, sel_d, d_yfox):
    NT = S // 128
    SCALE = 128 ** -0.5
    V = nc.vector
    with ExitStack() as es:
        triU = cx.sbuf(es, "triU", [128, 128], F32)
        triUb = cx.sbuf(es, "triUb", [128, 128], BF16)
        ones = cx.sbuf(es, "ones", [128, 128], F32)
        sel = cx.sbuf(es, "sel", [128, 128], F32)
        bfc = cx.sbuf(es, "bfc", [128, 2], F32)
        nbf = cx.sbuf(es, "nbf", [128, 2], F32)
        one1 = cx.sbuf(es, "one1", [128, 1], F32)
        ff = cx.sbuf(es, "ff", [128, NT, 2], F32)
        lf = cx.sbuf(es, "lf", [128, NT, 2], F32)
        cum = cx.sbuf(es, "cum", [128, NT, 2], F32)
        negc = cx.sbuf(es, "negc", [128, 2, NT], F32)
        tot = cx.sbuf(es, "tot", [128, NT, 2], F32)
        off = cx.sbuf(es, "off", [128, NT, 2], F32)
        rmid = cx.sbuf(es, "rmid", [128, 2, NT], F32)
        banks = [cx.psum(es, f"fbank{i}", [128, 512]) for i in range(8)]
        s0 = cx.slot()
        for i, (dst, src) in enumerate(((triU, triU_d), (ones, ones_d), (sel, sel_d), (bfc, bf_d), (ff, d_ff.t.rearrange("(n p) h -> p n h", p=128)))):
            cx.sp.dma(dst[:], src, s0, reads=([d_ff] if dst is ff else []), writes=[dst], cont=(i > 0))
        ft = Tok(s0.sem, s0.total, s0.key)
        for dst in (triU, ones, sel, bfc, ff):
            dst.writers[s0.key] = ft
        cx.dve.op(lambda: V.memset(one1[:], 1.0), writes=[one1])
        cx.dve.op(lambda: V.tensor_copy(out=triUb[:], in_=triU[:]), reads=[triU], writes=[triUb])
        cx.dve.op(lambda: V.tensor_scalar(out=nbf[:], in0=bfc[:], scalar1=-1.0, scalar2=None, op0=ALU.mult), reads=[bfc], writes=[nbf])
        for h in range(2):
            cx.act.op(lambda: nc.scalar.activation(out=lf[:, :, h], in_=ff[:, :, h], func=AF.Exp, bias=nbf[:, h:h + 1], scale=-1.0), reads=[ff, nbf], writes=[lf])
        cx.act.op(lambda: nc.scalar.activation(out=lf[:], in_=lf[:], func=AF.Ln, bias=one1[:, 0:1], scale=1.0), reads=[lf, one1], writes=[lf])
        cx.dve.op(lambda: V.tensor_scalar(out=lf[:], in0=lf[:], scalar1=-1.0, scalar2=None, op0=ALU.mult), reads=[lf], writes=[lf])
        lf2 = lf[:].rearrange("p n h -> p (n h)"); cum2 = cum[:].rearrange("p n h -> p (n h)"); tot2 = tot[:].rearrange("p n h -> p (n h)")
        W2 = NT * 2
        for a in range(0, W2, 512):
            n_ = min(512, W2 - a)
            cx.pe.op(lambda: nc.tensor.matmul(banks[0][:, 0:n_], lhsT=triU[:], rhs=lf2[:, a:a + n_], start=True, stop=True), reads=[triU, lf], writes=[banks[0]])
            cx.dve.op(lambda: V.tensor_copy(out=cum2[:, a:a + n_], in_=banks[0][:, 0:n_]), reads=[banks[0]], writes=[cum])
            cx.pe.op(lambda: nc.tensor.matmul(banks[1][:, 0:n_], lhsT=ones[:], rhs=lf2[:, a:a + n_], start=True, stop=True), reads=[ones, lf], writes=[banks[1]])
            cx.dve.op(lambda: V.tensor_copy(out=tot2[:, a:a + n_], in_=banks[1][:, 0:n_]), reads=[banks[1]], writes=[tot])
        cx.dve.op(lambda: V.memset(off[:, 0, :], 0.0), writes=[off])
        for n in range(1, NT):
            cx.dve.op(lambda: V.tensor_tensor(out=off[:, n, :], in0=off[:, n - 1, :], in1=tot[:, n - 1, :], op=ALU.add), reads=[off, tot], writes=[off])
        cx.dve.op(lambda: V.tensor_tensor(out=cum[:], in0=cum[:], in1=off[:], op=ALU.add), reads=[cum, off], writes=[cum])
        for h in range(2):
            cx.dve.op(lambda: V.tensor_scalar(out=negc[:, h, :], in0=cum[:, :, h], scalar1=-1.0, scalar2=None, op0=ALU.mult), reads=[cum], writes=[negc])
        for a in range(0, W2, 512):
            n_ = min(512, W2 - a)
            cx.pe.op(lambda: nc.tensor.matmul(banks[0][:, 0:n_], lhsT=sel[:], rhs=cum2[:, a:a + n_], start=True, stop=True), reads=[sel, cum], writes=[banks[0]])
            cx.dve.op(lambda: V.tensor_copy(out=tot2[:, a:a + n_], in_=banks[0][:, 0:n_]), reads=[banks[0]], writes=[tot])
        for h in range(2):
            cx.dve.op(lambda: V.tensor_copy(out=rmid[:, h, :], in_=tot[:, :, h]), reads=[tot], writes=[rmid])
        KT = cx.sbuf(es, "KT", [128, S], BF16)
        QT = cx.sbuf(es, "QT", [128, S], BF16)
        Va = cx.sbuf(es, "Va", [128, NT, 130], BF16)
        vst = [cx.sbuf(es, f"vst{i}", [128, 8, 128], F32) for i in range(2)]
        vsl = [cx.slot() for _ in range(2)]
        BQ = [cx.sbuf(es, f"BQ{i}", [128, NT], F32) for i in range(2)]
        PTw = [cx.sbuf(es, f"PTw{i}", [128, 512], BF16) for i in range(3)]
        boff = [cx.sbuf(es, f"boff{i}", [128, NT], F32) for i in range(2)]
        Osb = [cx.sbuf(es, f"Osb{i}", [128, 129], F32) for i in range(4)]
        fd = cx.sbuf(es, "fd", [128, 4], F32)
        fcol = cx.sbuf(es, "fcol", [128, 4], F32)
        rl = cx.sbuf(es, "rl", [128, 2], F32)
        yo = [cx.sbuf(es, f"fyo{i}", [128, 128], BF16) for i in range(2)]
        ysl = [cx.slot() for _ in range(2)]
        ksl = cx.slot()
        pS = banks[0:3]; pO = banks[4:8]
        cx.dve.op(lambda: V.memset(Va[:, :, 128:130], 1.0), writes=[Va])
        fv = d_rgfv.t.rearrange("(n p) c -> p n c", p=128)
        grp = 0
        for h in range(2):
            cx.sp.dma(QT[:], d_fqk[h], ksl, reads=[d_fqk], writes=[QT])
            cx.sp.dma(KT[:], d_fqk[2 + h], ksl, reads=[d_fqk], writes=[KT], cont=True)
            ft = Tok(ksl.sem, ksl.total, ksl.key); QT.writers[ksl.key] = ft
            for g in range(0, NT, 8):
                ng = min(8, NT - g); i_ = (g // 8) % 2
                cx.sp.dma(vst[i_][:, 0:ng, :], fv[:, g:g + ng, 256 + h * 128:256 + (h + 1) * 128], vsl[i_], reads=[d_rgfv], writes=[vst[i_]])
                cx.dve.op(lambda: V.tensor_copy(out=Va[:, g:g + ng, 0:128], in_=vst[i_][:, 0:ng, :]), reads=[vst[i_]], writes=[Va])
            for B in range(NT // 4):
                q0 = 4 * B
                nko = 4 * B
                if nko > 0:
                    bo = boff[B % 2]
                    cx.dve.op(lambda: V.tensor_scalar(out=bo[:, 0:nko], in0=negc[:, h, 0:nko], scalar1=rmid[:, h, q0:q0 + 1], scalar2=None, op0=ALU.add),
                              reads=[negc, rmid], writes=[bo])
                    cx.dve.op(lambda: V.tensor_scalar(out=fd[:, 0:4], in0=rmid[:, h, q0:q0 + 4], scalar1=rmid[:, h, q0:q0 + 1], scalar2=None, op0=ALU.subtract),
                              reads=[rmid], writes=[fd])
                    cx.act.op(lambda: nc.scalar.activation(out=fcol[:, 0:4], in_=fd[:, 0:4], func=AF.Exp), reads=[fd], writes=[fcol])
                    for kap in range(nko):
                        ps = pS[grp % 3]; pt = PTw[grp % 3]; grp += 1
                        cx.pe.op(lambda: nc.tensor.matmul(ps[:, :], lhsT=KT[:, kap * 128:(kap + 1) * 128], rhs=QT[:, q0 * 128:(q0 + 4) * 128], start=True, stop=True),
                                 reads=[KT, QT], writes=[ps])
                        cx.act.op(lambda: nc.scalar.activation(out=pt[:, :], in_=ps[:, :], func=AF.Exp, bias=bo[:, kap:kap + 1], scale=SCALE), reads=[ps, bo], writes=[pt])
                        for j in range(4):
                            cx.pe.op(lambda: nc.tensor.matmul(pO[j][:, 0:129], lhsT=pt[:, j * 128:(j + 1) * 128], rhs=Va[:, kap, 0:129], start=(kap == 0), stop=(kap == nko - 1)),
                                     reads=[pt, Va], writes=[pO[j]])
                    for j in range(4):
                        cx.act.op(lambda: nc.scalar.activation(out=Osb[j][:], in_=pO[j][:, 0:129], func=AF.Copy, scale=fcol[:, j:j + 1]), reads=[pO[j], fcol], writes=[Osb[j]])
                for j in range(4):
                    Q = q0 + j
                    bq = BQ[Q % 2]
                    cx.dve.op(lambda: V.tensor_scalar(out=bq[:, q0:Q + 1], in0=negc[:, h, q0:Q + 1], scalar1=rmid[:, h, Q:Q + 1], scalar2=None, op0=ALU.add),
                              reads=[negc, rmid], writes=[bq])
                    po = pO[j]
                    nk = j + 1
                    ps = pS[grp % 3]; pt = PTw[grp % 3]; grp += 1
                    for jj in range(nk):
                        kap = q0 + jj
                        cx.pe.op(lambda: nc.tensor.matmul(ps[:, jj * 128:(jj + 1) * 128], lhsT=KT[:, kap * 128:(kap + 1) * 128], rhs=QT[:, Q * 128:(Q + 1) * 128],
                                                          start=True, stop=True), reads=[KT, QT], writes=[ps])
                    for jj in range(nk):
                        kap = q0 + jj
                        cx.act.op(lambda: nc.scalar.activation(out=pt[:, jj * 128:(jj + 1) * 128], in_=ps[:, jj * 128:(jj + 1) * 128], func=AF.Exp, bias=bq[:, kap:kap + 1], scale=SCALE),
                                  reads=[ps, bq], writes=[pt])
                        if kap == Q:
                            cx.dve.op(lambda: V.tensor_tensor(out=pt[:, jj * 128:(jj + 1) * 128], in0=pt[:, jj * 128:(jj + 1) * 128], in1=triUb[:], op=ALU.mult), reads=[pt, triUb], writes=[pt])
                    for jj in range(nk):
                        kap = q0 + jj
                        cx.pe.op(lambda: nc.tensor.matmul(po[:, 0:129], lhsT=pt[:, jj * 128:(jj + 1) * 128], rhs=Va[:, kap, 0:129], start=(jj == 0), stop=(jj == nk - 1)),
                                 reads=[pt, Va], writes=[po])
                    if nko > 0:
                        cx.dve.op(lambda: V.tensor_tensor(out=Osb[j][:], in0=Osb[j][:], in1=po[:, 0:129], op=ALU.add), reads=[Osb[j], po], writes=[Osb[j]])
                    else:
                        cx.dve.op(lambda: V.tensor_copy(out=Osb[j][:], in_=po[:, 0:129]), reads=[po], writes=[Osb[j]])
                    cx.dve.op(lambda: V.reciprocal(out=rl[:, Q % 2:Q % 2 + 1], in_=Osb[j][:, 128:129]), reads=[Osb[j]], writes=[rl])
                    y_ = yo[Q % 2]
                    cx.dve.op(lambda: V.tensor_scalar(out=y_[:], in0=Osb[j][:, 0:128], scalar1=rl[:, Q % 2:Q % 2 + 1], scalar2=None, op0=ALU.mult), reads=[Osb[j], rl], writes=[y_])
                    cx.sp.dma(d_yfox[Q * 128:(Q + 1) * 128, h * 128:(h + 1) * 128], y_[:], ysl[Q % 2], reads=[y_], writes=[d_yfox])
        cx.barrier()

class TopkScratch:
    def __init__(self, cx, es, nc):
        self.s2 = cx.sbuf(es, "tk_s2", [128, 128], F32)
        self.t1 = None
        self.i1u = None
        self.i1 = cx.sbuf(es, "tk_i1", [128, 16, 16], F32)
        self.cand = cx.sbuf(es, "tk_cand", [128, 256], F32)
        self.cand2 = cx.sbuf(es, "tk_cand2", [128, 256], F32)
        self.cta = cx.sbuf(es, "tk_cta", [128, 8, 16], F32)
        self.pua = cx.sbuf(es, "tk_pua", [128, 8, 16], U32)
        self.aua = cx.sbuf(es, "tk_aua", [128, 8, 16], U32)
        self.bua = self.aua
        self.afa = cx.sbuf(es, "tk_afa", [128, 8, 16], F32)
        self.bfa = cx.sbuf(es, "tk_bfa", [128, 8, 16], F32)
        self.eq4 = cx.sbuf(es, "tk_eq4", [128, 2, 16, 16], BF16)
        self.io16 = cx.sbuf(es, "tk_io16", [128, 16], F32)
        self.exa = cx.sbuf(es, "tk_exa", [128, 8, 16], F32)
        self.za = cx.sbuf(es, "tk_za", [128, 8], F32)
        cx.pool.op(lambda: nc.gpsimd.iota(self.io16[:], pattern=[[1, 16]], base=0, channel_multiplier=0, allow_small_or_imprecise_dtypes=True), writes=[self.io16])


def emit_level1(cx, nc, sc, s_sb, chunk):
    V = nc.vector
    cx.dve.op(lambda: V.max(out=sc.t1[:, chunk, 0:8], in_=s_sb[:]), reads=[s_sb], writes=[sc.t1])
    cx.dve.op(lambda: V.max_index(out=sc.i1u[:, chunk, 0:8], in_max=sc.t1[:, chunk, 0:8], in_values=s_sb[:]), reads=[s_sb, sc.t1], writes=[sc.i1u])
    cx.dve.op(lambda: V.match_replace(out=sc.s2[:], in_to_replace=sc.t1[:, chunk, 0:8], in_values=s_sb[:], imm_value=-1e30), reads=[s_sb, sc.t1], writes=[sc.s2])
    cx.dve.op(lambda: V.max(out=sc.t1[:, chunk, 8:16], in_=sc.s2[:]), reads=[sc.s2], writes=[sc.t1])
    cx.dve.op(lambda: V.max_index(out=sc.i1u[:, chunk, 8:16], in_max=sc.t1[:, chunk, 8:16], in_values=sc.s2[:]), reads=[sc.s2, sc.t1], writes=[sc.i1u])


def emit_level2(cx, nc, sc, TI1, TI2, TG):
    V = nc.vector
    cx.dve.op(lambda: V.tensor_copy(out=sc.i1[:], in_=sc.i1u[:]), reads=[sc.i1u], writes=[sc.i1])
    for h in range(8):
        ta = sc.t1[:, 2 * h, :]; tb = sc.t1[:, 2 * h + 1, :]
        cand3 = sc.cand[:].rearrange("p (a b) -> p a b", a=16)
        cx.dve.op(lambda: V.tensor_tensor(out=cand3, in0=ta.unsqueeze(2).to_broadcast([128, 16, 16]), in1=tb.unsqueeze(1).to_broadcast([128, 16, 16]), op=ALU.add),
                  reads=[sc.t1], writes=[sc.cand])
        cx.dve.op(lambda: V.max(out=sc.cta[:, h, 0:8], in_=sc.cand[:]), reads=[sc.cand], writes=[sc.cta])
        cx.dve.op(lambda: V.max_index(out=sc.pua[:, h, 0:8], in_max=sc.cta[:, h, 0:8], in_values=sc.cand[:]), reads=[sc.cand, sc.cta], writes=[sc.pua])
        cx.dve.op(lambda: V.match_replace(out=sc.cand2[:], in_to_replace=sc.cta[:, h, 0:8], in_values=sc.cand[:], imm_value=-1e30), reads=[sc.cand, sc.cta], writes=[sc.cand2])
        cx.dve.op(lambda: V.max(out=sc.cta[:, h, 8:16], in_=sc.cand2[:]), reads=[sc.cand2], writes=[sc.cta])
        cx.dve.op(lambda: V.max_index(out=sc.pua[:, h, 8:16], in_max=sc.cta[:, h, 8:16], in_values=sc.cand2[:]), reads=[sc.cand2, sc.cta], writes=[sc.pua])
    cx.dve.op(lambda: V.tensor_single_scalar(out=sc.aua[:], in_=sc.pua[:], scalar=4, op=ALU.logical_shift_right), reads=[sc.pua], writes=[sc.aua])
    cx.dve.op(lambda: V.tensor_copy(out=sc.afa[:], in_=sc.aua[:]), reads=[sc.aua], writes=[sc.afa])
    cx.dve.op(lambda: V.tensor_single_scalar(out=sc.bua[:], in_=sc.pua[:], scalar=15, op=ALU.bitwise_and), reads=[sc.pua], writes=[sc.bua])
    cx.dve.op(lambda: V.tensor_copy(out=sc.bfa[:], in_=sc.bua[:]), reads=[sc.bua], writes=[sc.bfa])
    i1v = sc.i1[:].rearrange("p (h two) a -> p h two a", two=2)
    for (sel, half, TI) in ((sc.afa, 0, TI1), (sc.bfa, 1, TI2)):
        for hb in range(4):
            hs = slice(2 * hb, 2 * hb + 2)
            cx.dve.op(lambda: V.tensor_tensor(out=sc.eq4[:], in0=sel[:, hs, :].unsqueeze(3).to_broadcast([128, 2, 16, 16]),
                                              in1=sc.io16[:].unsqueeze(1).unsqueeze(1).to_broadcast([128, 2, 16, 16]), op=ALU.is_equal),
                      reads=[sel, sc.io16], writes=[sc.eq4])
            cx.dve.op(lambda: V.tensor_tensor(out=sc.eq4[:], in0=sc.eq4[:], in1=i1v[:, hs, half, :].unsqueeze(2).to_broadcast([128, 2, 16, 16]), op=ALU.mult),
                      reads=[sc.eq4, sc.i1], writes=[sc.eq4])
            cx.dve.op(lambda: V.tensor_reduce(out=TI[:, 32 * hb:32 * hb + 32].rearrange("p (h j) -> p h j", h=2), in_=sc.eq4[:], axis=AX.X, op=ALU.add), reads=[sc.eq4], writes=[TI])
    cx.dve.op(lambda: V.tensor_tensor(out=sc.exa[:], in0=sc.cta[:], in1=sc.cta[:, :, 0:1].to_broadcast([128, 8, 16]), op=ALU.subtract), reads=[sc.cta], writes=[sc.exa])
    cx.act.op(lambda: nc.scalar.activation(out=sc.exa[:], in_=sc.exa[:], func=AF.Exp), reads=[sc.exa], writes=[sc.exa])
    cx.dve.op(lambda: V.tensor_reduce(out=sc.za[:], in_=sc.exa[:], axis=AX.X, op=ALU.add), reads=[sc.exa], writes=[sc.za])
    cx.dve.op(lambda: V.reciprocal(out=sc.za[:], in_=sc.za[:]), reads=[sc.za], writes=[sc.za])
    cx.dve.op(lambda: V.tensor_tensor(out=TG[:].rearrange("p (h j) -> p h j", h=8), in0=sc.exa[:], in1=sc.za[:].unsqueeze(2).to_broadcast([128, 8, 16]), op=ALU.mult),
              reads=[sc.exa, sc.za], writes=[TG])


EPS = 1e-6

def emit_l2(cx, nc, TPC, x_d, yret_d, yfox_d, mod_row, modT_d, n2T_d, gfT_d, fg_row, Wo_d, Wq_d, skT_d, uT_d, v_d, ident_d, d_x2, d_x3, out_d, stage=9, NCH=128):
    V = nc.vector
    NTT = TPC // 128
    TGA = min(TPC, 1024)
    T = min(TPC, 512)
    NTG = T // 128
    with ExitStack() as es:
        banks = [cx.psum(es, f"l2bank{i}", [128, 512]) for i in range(8)]
        idt = cx.sbuf(es, "ident", [128, 128], F32)
        idtb = cx.sbuf(es, "identb", [128, 128], BF16)
        modT = cx.sbuf(es, "modT", [128, 192], F32)
        n2T = cx.sbuf(es, "n2T", [128, 32], F32)
        gfT = cx.sbuf(es, "gfT", [128, 16], F32)
        g2p = cx.sbuf(es, "g2p", [128, 32], F32)
        epsc = cx.sbuf(es, "epsc", [128, 1], F32)
        ssf = cx.sbuf(es, "ssf", [128, NTT], F32)
        rsf = cx.sbuf(es, "rsf", [128, NTT], F32)
        ss2 = cx.sbuf(es, "ss2", [128, NTT, 8], F32)
        rs2 = cx.sbuf(es, "rs2", [128, NTT], F32)
        ss3 = cx.sbuf(es, "ss3", [128, NTT, 8], F32)
        rs3 = cx.sbuf(es, "rs3", [128, NTT], F32)
        iota = cx.sbuf(es, "iota", [128, 128], F32)
        s0 = cx.slot()
        for i, (dst, src) in enumerate(((idt, ident_d), (modT, modT_d), (n2T, n2T_d), (gfT, gfT_d))):
            cx.sp.dma(dst[:], src, s0, writes=[dst], cont=(i > 0))
        ft = Tok(s0.sem, s0.total, s0.key)
        for dst in (idt, modT, n2T, gfT):
            dst.writers[s0.key] = ft
        cx.dve.op(lambda: V.tensor_copy(out=idtb[:], in_=idt[:]), reads=[idt], writes=[idtb])
        cx.dve.op(lambda: V.memset(epsc[:], EPS), writes=[epsc])
        for b_ in (ssf, ss2, ss3):
            cx.dve.op(lambda: V.memset(b_[:], 0.0), writes=[b_])
        cx.pool.op(lambda: nc.gpsimd.iota(iota[:], pattern=[[1, 128]], base=0, channel_multiplier=0, allow_small_or_imprecise_dtypes=True), writes=[iota])
        cx.dve.op(lambda: V.scalar_tensor_tensor(out=g2p[:], in0=modT[:, 128:160], scalar=1.0, in1=n2T[:], op0=ALU.add, op1=ALU.mult), reads=[modT, n2T], writes=[g2p])
        sh2 = modT[:, 96:128]

        with ExitStack() as esA:
            gate1 = cx.sbuf(esA, "gate1", [128, 4096], F32)
            cx.sp.dma(gate1[:], mod_row[0:1, 8192:12288].partition_broadcast(128), cx.slot(), writes=[gate1])
            yT = cx.sbuf(esA, "yT", [128, 32, TGA], BF16)
            yt = [cx.sbuf(esA, f"yt{i}", [128, 4096], BF16) for i in range(2)]
            ysl = [cx.slot() for _ in range(2)]
            junkb = cx.sbuf(esA, "junkb", [128, 2048], BF16)
            wst = [cx.sbuf(esA, f"wost{i}", [128, 8, 512], F32) for i in range(2)]
            wsl = [cx.slot() for _ in range(2)]
            wb = [cx.sbuf(esA, f"wob{i}", [128, 32, 512], BF16) for i in range(2)]
            xp = [cx.sbuf(esA, f"xp{i}", [128, 512], F32) for i in range(3)]
            xsl = [cx.slot() for _ in range(3)]
            osl = [cx.slot() for _ in range(3)]
            junk = cx.sbuf(esA, "junkA", [128, 512], F32)
            pTb = [banks[i][:].bitcast(BF16) for i in (0, 1)]
            wov = Wo_d.rearrange("(k p) n -> p k n", p=128)
            pc = 0
            for ga in range(TPC // TGA):
                for tt in range(TGA // 128):
                    ti = ga * (TGA // 128) + tt
                    yb = yt[ti % 2]
                    cx.sp.dma(yb[:, 0:2048], yret_d[ti * 128:(ti + 1) * 128, :], ysl[ti % 2], reads=[yret_d], writes=[yb])
                    cx.sp.dma(yb[:, 2048:4096], yfox_d[ti * 128:(ti + 1) * 128, :], ysl[ti % 2], reads=[yfox_d], writes=[yb], cont=True)
                    ft = Tok(ysl[ti % 2].sem, ysl[ti % 2].total, ysl[ti % 2].key); yb.writers[ft.key] = ft
                    cx.act.op(lambda: nc.scalar.activation(out=junkb[:], in_=yb[:, 2048:4096], func=AF.Square, accum_out=ssf[:, ti:ti + 1]), reads=[yb, ssf], writes=[junkb, ssf])
                    cx.act.op(lambda: nc.scalar.activation(out=rsf[:, ti:ti + 1], in_=ssf[:, ti:ti + 1], func=AF.Sqrt, bias=epsc[:, 0:1], scale=1.0 / 2048), reads=[ssf, epsc], writes=[rsf])
                    cx.dve.op(lambda: V.reciprocal(out=rsf[:, ti:ti + 1], in_=rsf[:, ti:ti + 1]), reads=[rsf], writes=[rsf])
                    for g in range(4):
                        pb = banks[g % 2]; pv = pTb[g % 2]
                        for kk in range(8):
                            kc = 8 * g + kk
                            cx.pe.op(lambda: nc.tensor.transpose(out=pv[:, kk * 128:(kk + 1) * 128], in_=yb[:, kc * 128:(kc + 1) * 128], identity=idtb[:]), reads=[yb, idtb], writes=[pb])
                        cx.dve.op(lambda: V.tensor_copy(out=yT[:, 8 * g:8 * g + 8, tt * 128:(tt + 1) * 128], in_=pv[:, :].rearrange("p (a b) -> p a b", a=8)), reads=[pb], writes=[yT])
                for nb in range(8):
                    wbb = wb[nb % 2]
                    for q4 in range(4):
                        st = wst[(nb * 4 + q4) % 2]
                        cx.sp.dma(st[:], wov[:, 8 * q4:8 * q4 + 8, nb * 512:(nb + 1) * 512], wsl[(nb * 4 + q4) % 2], writes=[st])
                        if q4 < 2:
                            cx.dve.op(lambda: V.tensor_tensor(out=wbb[:, 8 * q4:8 * q4 + 8, :], in0=st[:], in1=gate1[:, nb * 512:(nb + 1) * 512].unsqueeze(1).to_broadcast([128, 8, 512]), op=ALU.mult),
                                      reads=[st, gate1], writes=[wbb])
                        else:
                            for kk in range(8):
                                kc = 8 * q4 + kk
                                cx.dve.op(lambda: V.scalar_tensor_tensor(out=wbb[:, kc, :], in0=st[:, kk, :], scalar=gfT[:, kc - 16:kc - 15], in1=gate1[:, nb * 512:(nb + 1) * 512], op0=ALU.mult, op1=ALU.mult),
                                          reads=[st, gfT, gate1], writes=[wbb])
                    for tt in range(TGA // 128):
                        ti = ga * (TGA // 128) + tt
                        xb = xp[pc % 3]; sl_x = xsl[pc % 3]; sl_o = osl[pc % 3]; pc += 1
                        cx.sp.dma(xb[:], x_d[ti * 128:(ti + 1) * 128, nb * 512:(nb + 1) * 512], sl_x, writes=[xb])
                        pr = banks[2 + (tt % 2) * 2]; pf = banks[3 + (tt % 2) * 2]
                        for kc in range(16):
                            cx.pe.op(lambda: nc.tensor.matmul(pr[:, :], lhsT=yT[:, kc, tt * 128:(tt + 1) * 128], rhs=wbb[:, kc, :], start=(kc == 0), stop=(kc == 15)), reads=[yT, wbb], writes=[pr])
                        for kc in range(16, 32):
                            cx.pe.op(lambda: nc.tensor.matmul(pf[:, :], lhsT=yT[:, kc, tt * 128:(tt + 1) * 128], rhs=wbb[:, kc, :], start=(kc == 16), stop=(kc == 31)), reads=[yT, wbb], writes=[pf])
                        cx.dve.op(lambda: V.scalar_tensor_tensor(out=xb[:], in0=pf[:, :], scalar=rsf[:, ti:ti + 1], in1=xb[:], op0=ALU.mult, op1=ALU.add), reads=[pf, rsf, xb], writes=[xb])
                        cx.dve.op(lambda: V.tensor_tensor(out=xb[:], in0=xb[:], in1=pr[:, :], op=ALU.add), reads=[xb, pr], writes=[xb])
                        cx.act.op(lambda: nc.scalar.activation(out=junk[:], in_=xb[:], func=AF.Square, accum_out=ss2[:, ti, nb:nb + 1]), reads=[xb, ss2], writes=[junk, ss2])
                        cx.sp.dma(d_x2[ti * 128:(ti + 1) * 128, nb * 512:(nb + 1) * 512], xb[:], sl_o, reads=[xb], writes=[d_x2])
            cx.barrier()
        if stage < 1:
            return
        cx.dve.op(lambda: V.tensor_reduce(out=rs2[:], in_=ss2[:], axis=AX.X, op=ALU.add), reads=[ss2], writes=[rs2])
        cx.act.op(lambda: nc.scalar.activation(out=rs2[:], in_=rs2[:], func=AF.Sqrt, bias=epsc[:, 0:1], scale=1.0 / 4096), reads=[rs2, epsc], writes=[rs2])
        cx.dve.op(lambda: V.reciprocal(out=rs2[:], in_=rs2[:]), reads=[rs2], writes=[rs2])

        NGRP = TPC // T
        d_ub = cx.dram("ub_scr", [NCH, 128, 32, 128], BF16)
        d_vb = cx.dram("vb_scr", [NCH, 128, 4096], BF16)
        G = cx.sbuf(es, "G", [128, 128, T], BF16)
        kI1 = cx.sbuf(es, "kI1", [128, T], F32)
        kI2 = cx.sbuf(es, "kI2", [128, T], F32)
        kG = cx.sbuf(es, "kG", [128, T], F32)
        for gp_ in range(TPC // T):
            t0 = gp_ * T
            cx.new_epoch()
            with ExitStack() as esB:
                h2T = cx.sbuf(esB, "h2T", [128, 32, T], BF16)
                with ExitStack() as esa:
                    x2t = cx.sbuf(esa, "x2t", [128, 4096], F32)
                    xs_ = cx.slot()
                    for tt in range(NTG):
                        ti = gp_ * NTG + tt
                        cx.sp.dma(x2t[:], d_x2[ti * 128:(ti + 1) * 128, :], xs_, reads=[d_x2], writes=[x2t])
                        cx.act.op(lambda: nc.scalar.activation(out=x2t[:], in_=x2t[:], func=AF.Copy, scale=rs2[:, ti:ti + 1]), reads=[x2t, rs2], writes=[x2t])
                        for g in range(8):
                            pb = banks[g % 2]
                            for kk in range(4):
                                kc = 4 * g + kk
                                cx.pe.op(lambda: nc.tensor.transpose(out=pb[:, kk * 128:(kk + 1) * 128], in_=x2t[:, kc * 128:(kc + 1) * 128], identity=idt[:]), reads=[x2t, idt], writes=[pb])
                            for kk in range(4):
                                kc = 4 * g + kk
                                e_ = cx.act if kk % 2 == 0 else cx.dve
                                if kk % 2 == 0:
                                    cx.act.op(lambda: nc.scalar.activation(out=h2T[:, kc, tt * 128:(tt + 1) * 128], in_=pb[:, kk * 128:(kk + 1) * 128], func=AF.Identity, scale=g2p[:, kc:kc + 1], bias=sh2[:, kc:kc + 1]),
                                              reads=[pb, g2p, modT], writes=[h2T])
                                else:
                                    cx.dve.op(lambda: V.tensor_scalar(out=h2T[:, kc, tt * 128:(tt + 1) * 128], in0=pb[:, kk * 128:(kk + 1) * 128], scalar1=g2p[:, kc:kc + 1], scalar2=sh2[:, kc:kc + 1], op0=ALU.mult, op1=ALU.add),
                                              reads=[pb, g2p, modT], writes=[h2T])
                    cx.barrier()
                if stage < 2:
                    return
                with ExitStack() as esb:
                    sc = TopkScratch(cx, esb, nc)
                    scs = [TopkScratch.__new__(TopkScratch) for _ in range(NTG)]
                    t1s = [cx.sbuf(esb, f"t1s{i}", [128, 16, 16], F32) for i in range(NTG)]
                    i1s = [cx.sbuf(esb, f"i1s{i}", [128, 16, 16], U32) for i in range(NTG)]
                    wqst = [cx.sbuf(esb, f"wqst{i}", [128, 8, 128], F32) for i in range(2)]
                    wqsl = [cx.slot() for _ in range(2)]
                    wqb = [cx.sbuf(esb, f"wqb{i}", [128, 32, 128], BF16) for i in range(1)]
                    sksl = [cx.slot() for _ in range(2)]
                    skst = [cx.sbuf(esb, f"skst{i}", [128, 128], F32) for i in range(1)] * 2
                    skb = [cx.sbuf(esb, f"skb{i}", [128, 128], BF16) for i in range(2)]
                    qTc = [cx.sbuf(esb, f"qTc{i}", [128, T], BF16) for i in range(2)]
                    s_sb = [cx.sbuf(esb, f"s_sb{i}", [128, 128], F32) for i in range(2)]
                    TI = [cx.sbuf(esb, n, [128, 128], F32) for n in ("TI1", "TI2", "TG")]
                    wqv = Wq_d
                    cnt = 0
                    for ch in range(16):
                        wq_ = wqb[0]; sks = skst[ch % 2]; sk_ = skb[ch % 2]; qc = qTc[ch % 2]
                        for hf in range(4):
                            st = wqst[hf % 2]
                            cx.sp.dma(st[:], wqv[ch, :, 8 * hf:8 * hf + 8, :], wqsl[hf % 2], writes=[st])
                            if hf % 2 == 0:
                                cx.pool.op(lambda: nc.gpsimd.tensor_copy(out=wq_[:, 8 * hf:8 * hf + 8, :], in_=st[:]), reads=[st], writes=[wq_])
                            else:
                                cx.dve.op(lambda: V.tensor_copy(out=wq_[:, 8 * hf:8 * hf + 8, :], in_=st[:]), reads=[st], writes=[wq_])
                        cx.sp.dma(sks[:], skT_d[ch], sksl[ch % 2], writes=[sks])
                        cx.dve.op(lambda: V.tensor_copy(out=sk_[:], in_=sks[:]), reads=[sks], writes=[sk_])
                        pq = banks[2 + ch % 2]
                        for kc in range(32):
                            cx.pe.op(lambda: nc.tensor.matmul(pq[:, 0:T], lhsT=wq_[:, kc, :], rhs=h2T[:, kc, :], start=(kc == 0), stop=(kc == 31)), reads=[wq_, h2T], writes=[pq])
                        cx.act.op(lambda: nc.scalar.copy(out=qc[:], in_=pq[:, 0:T]), reads=[pq], writes=[qc])
                        for tt in range(NTG):
                            ps = banks[4 + cnt % 2]; ss_ = s_sb[cnt % 2]; cnt += 1
                            cx.pe.op(lambda: nc.tensor.matmul(ps[:, 0:128], lhsT=qc[:, tt * 128:(tt + 1) * 128], rhs=sk_[:], start=True, stop=True), reads=[qc, sk_], writes=[ps])
                            cx.act.op(lambda: nc.scalar.copy(out=ss_[:], in_=ps[:, 0:128]), reads=[ps], writes=[ss_])
                            sc.t1, sc.i1u = t1s[tt], i1s[tt]
                            emit_level1(cx, nc, sc, ss_, ch)
                    for tt in range(NTG):
                        sc.t1, sc.i1u = t1s[tt], i1s[tt]
                        emit_level2(cx, nc, sc, *TI)
                        for (src, dst) in zip(TI, (kI1, kI2, kG)):
                            pt = banks[6]
                            cx.pe.op(lambda: nc.tensor.transpose(out=pt[:, 0:128], in_=src[:], identity=idt[:]), reads=[src, idt], writes=[pt])
                            cx.act.op(lambda: nc.scalar.copy(out=dst[:, tt * 128:(tt + 1) * 128], in_=pt[:, 0:128]), reads=[pt], writes=[dst])
                    cx.barrier()
                if stage < 3:
                    return
                with ExitStack() as ese:
                    At = [cx.sbuf(ese, f"At{i}", [128, 128], BF16) for i in range(4)]
                    Bt = [cx.sbuf(ese, f"Bt{i}", [128, 128], BF16) for i in range(4)]
                    for t4 in range(T // 4):
                        pg = banks[t4 % 2]
                        for j in range(4):
                            t_ = t4 * 4 + j
                            a_ = At[j]; b_ = Bt[j]
                            cx.dve.op(lambda: V.tensor_scalar(out=a_[:], in0=iota[:], scalar1=kI1[:, t_:t_ + 1], scalar2=kG[:, t_:t_ + 1], op0=ALU.is_equal, op1=ALU.mult), reads=[iota, kI1, kG], writes=[a_])
                            cx.dve.op(lambda: V.tensor_scalar(out=b_[:], in0=iota[:], scalar1=kI2[:, t_:t_ + 1], scalar2=None, op0=ALU.is_equal), reads=[iota, kI2], writes=[b_])
                            cx.pe.op(lambda: nc.tensor.matmul(pg[:, j * 128:(j + 1) * 128], lhsT=a_[:], rhs=b_[:], start=True, stop=True), reads=[a_, b_], writes=[pg])
                        cx.act.op(lambda: nc.scalar.copy(out=G[:, :, t4 * 4:t4 * 4 + 4].rearrange("p c t -> p t c"), in_=pg[:, :].rearrange("p (t c) -> p t c", t=4)), reads=[pg], writes=[G])
                    cx.barrier()
                if stage < 4:
                    return
                with ExitStack() as esf:
                    ust = [cx.sbuf(esf, f"ust{i}", [128, 16, 128], F32) for i in range(2)]
                    usl = [cx.slot() for _ in range(2)]
                    ub = [cx.sbuf(esf, f"ub{i}", [128, 32, 128], BF16) for i in range(2)]
                    ga = [cx.sbuf(esf, f"ga{i}", [128, T], BF16) for i in range(2)]
                    ubs = [cx.slot() for _ in range(2)]
                    uv = uT_d
                    hc = 0
                    for c in range(NCH):
                        ub_ = ub[c % 2]
                        if gp_ == 0:
                            for hf in range(2):
                                st = ust[hc % 2]; sl_ = usl[hc % 2]; hc += 1
                                cx.sp.dma(st[:], uv[c, :, 16 * hf:16 * hf + 16, :], sl_, writes=[st])
                                if hf == 0:
                                    cx.pool.op(lambda: nc.gpsimd.tensor_copy(out=ub_[:, 0:16, :], in_=st[:]), reads=[st], writes=[ub_])
                                else:
                                    cx.dve.op(lambda: V.tensor_copy(out=ub_[:, 16:32, :], in_=st[:]), reads=[st], writes=[ub_])
                            if NGRP > 1:
                                cx.sp.dma(d_ub[c], ub_[:], ubs[c % 2], reads=[ub_], writes=[d_ub])
                        else:
                            cx.sp.dma(ub_[:], d_ub[c], ubs[c % 2], reads=[d_ub], writes=[ub_])
                        pa = banks[2 + c % 2]
                        for kc in range(32):
                            cx.pe.op(lambda: nc.tensor.matmul(pa[:, 0:T], lhsT=ub_[:, kc, :], rhs=h2T[:, kc, :], start=(kc == 0), stop=(kc == 31)), reads=[ub_, h2T], writes=[pa])
                        g_ = ga[c % 2]
                        cx.act.op(lambda: nc.scalar.activation(out=g_[:], in_=pa[:, 0:T], func=AF.Gelu_apprx_tanh), reads=[pa], writes=[g_])
                        cx.dve.op(lambda: V.tensor_tensor(out=G[:, c, :], in0=G[:, c, :], in1=g_[:], op=ALU.mult), reads=[G, g_], writes=[G])
                    cx.barrier()
            if stage < 5:
                return
            with ExitStack() as esg:
                gate2 = cx.sbuf(esg, "gate2", [128, 4096], F32)
                cx.sp.dma(gate2[:], mod_row[0:1, 20480:24576].partition_broadcast(128), cx.slot(), writes=[gate2])
                vst = [cx.sbuf(esg, f"vst{i}", [128, 1024], F32) for i in range(3)]
                vsl = [cx.slot() for _ in range(3)]
                vbs = [cx.slot() for _ in range(3)]
                vb = [cx.sbuf(esg, f"vb{i}", [128, 1024], BF16) for i in range(3)]
                xq = [cx.sbuf(esg, f"xq{i}", [128, 512], F32) for i in range(3)]
                xqs = [cx.slot() for _ in range(3)]
                xos = [cx.slot() for _ in range(3)]
                junk = cx.sbuf(esg, "junkg", [128, 512], F32)
                vc = 0; pc = 0
                for p in range(4):
                    for c in range(NCH):
                        st = vst[vc % 3]; vb_ = vb[vc % 3]; sl_ = vsl[vc % 3]; sl2_ = vbs[vc % 3]; vc += 1
                        if gp_ == 0:
                            cx.sp.dma(st[:], v_d[c, :, p * 1024:(p + 1) * 1024], sl_, writes=[st])
                            if c % 2 == 0:
                                cx.pool.op(lambda: nc.gpsimd.tensor_tensor(out=vb_[:], in0=st[:], in1=gate2[:, p * 1024:(p + 1) * 1024], op=ALU.mult), reads=[st, gate2], writes=[vb_])
                            else:
                                cx.dve.op(lambda: V.tensor_tensor(out=vb_[:], in0=st[:], in1=gate2[:, p * 1024:(p + 1) * 1024], op=ALU.mult), reads=[st, gate2], writes=[vb_])
                            if NGRP > 1:
                                cx.sp.dma(d_vb[c, :, p * 1024:(p + 1) * 1024], vb_[:], sl2_, reads=[vb_], writes=[d_vb])
                        else:
                            cx.sp.dma(vb_[:], d_vb[c, :, p * 1024:(p + 1) * 1024], sl2_, reads=[d_vb], writes=[vb_])
                        for ts in range(NTG):
                            for j in range(2):
                                po = banks[ts * 2 + j]
                                cx.pe.op(lambda: nc.tensor.matmul(po[:, :], lhsT=G[:, c, ts * 128:(ts + 1) * 128], rhs=vb_[:, j * 512:(j + 1) * 512], start=(c == 0), stop=(c == NCH - 1)), reads=[G, vb_], writes=[po])
                    for ts in range(NTG):
                        ti = gp_ * NTG + ts
                        for j in range(2):
                            nb = p * 2 + j
                            po = banks[ts * 2 + j]
                            xb = xq[pc % 3]; s1_ = xqs[pc % 3]; s2_ = xos[pc % 3]; pc += 1
                            cx.sp.dma(xb[:], d_x2[ti * 128:(ti + 1) * 128, nb * 512:(nb + 1) * 512], s1_, reads=[d_x2], writes=[xb])
                            cx.dve.op(lambda: V.tensor_tensor(out=xb[:], in0=xb[:], in1=po[:, :], op=ALU.add), reads=[xb, po], writes=[xb])
                            cx.act.op(lambda: nc.scalar.activation(out=junk[:], in_=xb[:], func=AF.Square, accum_out=ss3[:, ti, nb:nb + 1]), reads=[xb, ss3], writes=[junk, ss3])
                            cx.sp.dma(d_x3[ti * 128:(ti + 1) * 128, nb * 512:(nb + 1) * 512], xb[:], s2_, reads=[xb], writes=[d_x3])
                cx.barrier()
        if stage < 6:
            return
        cx.dve.op(lambda: V.tensor_reduce(out=rs3[:], in_=ss3[:], axis=AX.X, op=ALU.add), reads=[ss3], writes=[rs3])
        cx.act.op(lambda: nc.scalar.activation(out=rs3[:], in_=rs3[:], func=AF.Sqrt, bias=epsc[:, 0:1], scale=1.0 / 4096), reads=[rs3, epsc], writes=[rs3])
        cx.dve.op(lambda: V.reciprocal(out=rs3[:], in_=rs3[:]), reads=[rs3], writes=[rs3])
        with ExitStack() as esh:
            fg = cx.sbuf(esh, "fg", [128, 4096], F32)
            cx.sp.dma(fg[:], fg_row[0:1, :].partition_broadcast(128), cx.slot(), writes=[fg])
            xt = [cx.sbuf(esh, f"x3t{i}", [128, 4096], F32) for i in range(2)]
            sl1 = [cx.slot() for _ in range(2)]; sl2 = [cx.slot() for _ in range(2)]
            for ti in range(NTT):
                xb = xt[ti % 2]
                cx.sp.dma(xb[:], d_x3[ti * 128:(ti + 1) * 128, :], sl1[ti % 2], reads=[d_x3], writes=[xb])
                cx.dve.op(lambda: V.scalar_tensor_tensor(out=xb[:], in0=xb[:], scalar=rs3[:, ti:ti + 1], in1=fg[:], op0=ALU.mult, op1=ALU.mult), reads=[xb, rs3, fg], writes=[xb])
                cx.sp.dma(out_d[ti * 128:(ti + 1) * 128, :], xb[:], sl2[ti % 2], reads=[xb], writes=[out_d])
            cx.barrier()


S_FULL = 16384

def build_l1(S):
    nc = bass.Bass("TRN2", target_bir_lowering=False)
    di = lambda n, s, d=F32: nc.dram_tensor(n, s, d, kind="ExternalInput").ap()
    x = di("x", [S, 4096]); wc = di("wc", [4096, NW]); modT = di("modT", [128, 192]); n1T = di("n1T", [128, 32]); ident = di("ident", [128, 128])
    posT = di("posT", [128, S // 128], I32); rc = di("rc", [128, 8]); maskT = di("maskT", [128, 128]); invf2 = di("invf2", [128, 128]); offs = di("offs", [128, 128])
    gn = di("gn", [128, 256]); bf = di("bf", [128, 2]); triU = di("triU", [128, 128]); ones = di("ones", [128, 128]); sel = di("sel", [128, 128])
    with ExitStack() as es:
        cx = Ctx(nc, es)
        d_fqk = cx.dram("fqk", [4, 128, S], BF16); d_rqkv = cx.dram("rqkv", [S, 512], F32); d_rgfv = cx.dram("rgfv", [S, 512], F32); d_ff = cx.dram("ffl", [S, 2], F32)
        d_yret = cx.dram("yret", [S, 256], BF16, kind="ExternalOutput"); d_yfox = cx.dram("yfox", [S, 256], BF16, kind="ExternalOutput")
        emit_inproj(cx, nc, S, x, wc, modT, n1T, ident, d_fqk, d_rqkv, d_rgfv, d_ff)
        emit_ret(cx, nc, S, d_rqkv, d_rgfv, posT, rc, maskT, invf2, offs, gn, ident, d_yret)
        emit_fox(cx, nc, S, d_fqk, d_rgfv, d_ff, bf, triU, ones, sel, d_yfox)
        cx.barrier()
        cx.finish([d_yret, d_yfox])
    return nc


def build_l2(TPC):
    nc = bass.Bass("TRN2", target_bir_lowering=False)
    di = lambda n, s, d=F32: nc.dram_tensor(n, s, d, kind="ExternalInput").ap()
    with ExitStack() as es:
        cx = Ctx(nc, es)
        x = di("x", [TPC, 4096]); yret = Buf(di("yret", [TPC, 2048], BF16)); yfox = Buf(di("yfox", [TPC, 2048], BF16))
        mod_row = di("mod_row", [1, 24576]); modT = di("modT", [128, 192]); n2T = di("n2T", [128, 32]); gfT = di("gfT", [128, 16]); fg_row = di("fg_row", [1, 4096])
        Wo = di("Wo", [4096, 4096]); Wq = di("Wq", [16, 128, 32, 128]); skT = di("skT", [16, 128, 128]); uT = di("uT", [128, 128, 32, 128]); v = di("v", [128, 128, 4096]); ident = di("ident", [128, 128])
        d_x2 = cx.dram("x2", [TPC, 4096], F32); d_x3 = cx.dram("x3", [TPC, 4096], F32)
        out = cx.dram("out", [TPC, 4096], F32, kind="ExternalOutput")
        emit_l2(cx, nc, TPC, x, yret, yfox, mod_row, modT, n2T, gfT, fg_row, Wo, Wq, skT, uT, v, ident, d_x2, d_x3, out)
        cx.barrier()
        cx.finish([out])
    return nc


def kernel(x, c, positions, w_ada, b_ada, norm1_g, w_in, b_forget, ret_gn_g, fox_norm_g, w_out, norm2_g, w_peer_q, peer_sub_keys, peer_u, peer_v, final_g):
    f32 = lambda a: np.ascontiguousarray(np.asarray(a, dtype=np.float32))
    x = f32(x); S = x.shape[1]; TPC = S // 8
    col = lambda v_: np.ascontiguousarray(np.asarray(v_, np.float32).reshape(-1, 128).T)
    cores = list(range(8))
    w_ada0 = np.asarray(w_ada, np.float32)[0]; b_ada0 = np.asarray(b_ada, np.float32)[0]
    nc0 = build_l0()
    maps = [{"cT": col(np.asarray(c, np.float32)[0]), "w": np.ascontiguousarray(w_ada0[:, i * 3072:(i + 1) * 3072]), "b": np.ascontiguousarray(b_ada0[None, i * 3072:(i + 1) * 3072])} for i in cores]
    r0 = run_bass_kernel_spmd(nc0, maps, core_ids=cores)
    mod = np.concatenate([np.asarray(r["mod"])[0] for r in r0.results])
    modT = col(mod)
    ident = np.eye(128, dtype=np.float32)
    w_in0 = np.asarray(w_in, np.float32)[0]
    pos = np.asarray(positions, np.int32)[0]
    invf2, offs = rot_consts(); triU, ones, sel = fox_consts()
    nc1 = build_l1(S)
    maps = []
    for i in cores:
        rc, maskT = ret_consts(i)
        maps.append({"x": x[0], "wc": np.ascontiguousarray(w_in0[:, core_cols(i)]), "modT": modT, "n1T": col(np.asarray(norm1_g)[0]), "ident": ident,
                     "posT": np.ascontiguousarray(pos.reshape(-1, 128).T), "rc": rc, "maskT": maskT, "invf2": invf2, "offs": offs,
                     "gn": np.ascontiguousarray(np.tile(np.asarray(ret_gn_g, np.float32)[0][None, i * 256:(i + 1) * 256], (128, 1))),
                     "bf": np.ascontiguousarray(np.tile(np.asarray(b_forget, np.float32)[0][None, 2 * i:2 * i + 2], (128, 1))), "triU": triU, "ones": ones, "sel": sel})
    r1 = run_bass_kernel_spmd(nc1, maps, core_ids=cores)
    yret = np.concatenate([np.asarray(r["yret"]) for r in r1.results], axis=1)
    yfox = np.concatenate([np.asarray(r["yfox"]) for r in r1.results], axis=1)
    u = np.asarray(peer_u, np.float32)[0]; v = np.asarray(peer_v, np.float32)[0]
    uT = np.ascontiguousarray(u.reshape(128, 128, 32, 128).transpose(1, 3, 2, 0))
    vr = np.ascontiguousarray(v.reshape(128, 128, 4096).transpose(1, 0, 2))
    skT = np.ascontiguousarray(np.asarray(peer_sub_keys, np.float32)[0].reshape(16, 128, 128).transpose(0, 2, 1))
    wm = {"mod_row": np.ascontiguousarray(mod[None, :]), "modT": modT, "n2T": col(np.asarray(norm2_g)[0]), "gfT": col(np.asarray(fox_norm_g)[0]),
          "fg_row": np.ascontiguousarray(np.asarray(final_g, np.float32)[None, :]), "Wo": f32(np.asarray(w_out)[0]), "Wq": np.ascontiguousarray(np.asarray(w_peer_q, np.float32)[0].reshape(32, 128, 16, 128).transpose(2, 1, 0, 3)),
          "skT": skT, "uT": uT, "v": vr, "ident": ident}
    nc2 = build_l2(TPC)
    maps = []
    for i in cores:
        sl = slice(i * TPC, (i + 1) * TPC)
        m = dict(wm); m.update({"x": np.ascontiguousarray(x[0, sl]), "yret": np.ascontiguousarray(yret[sl]), "yfox": np.ascontiguousarray(yfox[sl])})
        maps.append(m)
    r2 = run_bass_kernel_spmd(nc2, maps, core_ids=cores)
    out = np.concatenate([np.asarray(r["out"]) for r in r2.results], axis=0)
    return out.reshape(1, S, 4096).astype(np.float32)
```

```python
import numpy as np
from contextlib import ExitStack
import concourse.bass as bass
import concourse.mybir as mybir
from concourse.bass_utils import run_bass_kernel_spmd

F32 = mybir.dt.float32
BF16 = mybir.dt.bfloat16
I32 = mybir.dt.int32
U32 = mybir.dt.uint32
ALU = mybir.AluOpType
AF = mybir.ActivationFunctionType
AX = mybir.AxisListType


class Tok:
    __slots__ = ("sem", "val", "key")
    def __init__(self, sem, val, key):
        self.sem = sem; self.val = val; self.key = key


class Buf:
    def __init__(self, t, name=""):
        self.t = t
        self.name = name
        self.writers = {}
        self.readers = {}
    def __getitem__(self, idx):
        return self.t[idx]


class Slot:
    def __init__(self, sem, key):
        self.sem = sem; self.total = 0; self.key = key


class Eng:
    def __init__(self, h, sem, key, is_pe=False):
        self.h = h; self.sem = sem; self.count = 0; self.key = key
        self.seen = {}
        self.is_pe = is_pe
    def wait(self, tok, same_ok=False):
        if tok is None:
            return
        if tok.key == self.key and (self.is_pe or same_ok):
            return
        if self.seen.get(tok.key, 0) >= tok.val:
            return
        self.h.wait_ge(tok.sem, tok.val)
        self.seen[tok.key] = tok.val
    def _deps(self, reads, writes):
        for b in reads:
            for t in b.writers.values():
                self.wait(t)
        for b in writes:
            for t in b.writers.values():
                self.wait(t, same_ok=True)
            for t in b.readers.values():
                self.wait(t, same_ok=True)
    def _commit(self, tok, reads, writes):
        for b in reads:
            b.readers[tok.key] = tok
        for b in writes:
            b.writers[tok.key] = tok
            b.readers = {}
    def op(self, fn, reads=(), writes=()):
        self._deps(reads, writes)
        inst = fn()
        self.count += 1
        inst.then_inc(self.sem, 1)
        tok = Tok(self.sem, self.count, self.key)
        self._commit(tok, reads, writes)
        return tok
    def dma(self, out, in_, slot, reads=(), writes=(), cont=False, **kw):
        self._deps(reads, writes)
        if not cont:
            self.wait(Tok(slot.sem, slot.total, slot.key))
        inst = self.h.dma_start(out=out, in_=in_, **kw)
        slot.total += 16
        inst.then_inc(slot.sem, 16)
        tok = Tok(slot.sem, slot.total, slot.key)
        self._commit(tok, reads, writes)
        return tok


class Ctx:
    def __init__(self, nc, es):
        self.nc = nc; self.es = es
        self._n = 0
        def mk(h, name, is_pe=False):
            sem = es.enter_context(nc.semaphore("e_" + name))
            return Eng(h, sem, "e_" + name, is_pe)
        self.pe = mk(nc.tensor, "pe", True)
        self.act = mk(nc.scalar, "act")
        self.dve = mk(nc.vector, "dve")
        self.pool = mk(nc.gpsimd, "pool")
        self.sp = mk(nc.sync, "sp")
        self.engs = [self.pe, self.act, self.dve, self.pool, self.sp]
        self.slots = []
    def new_epoch(self):
        self._site_cnt = {}
    def slot(self, name=None):
        import sys as _sys
        ln = _sys._getframe(1).f_lineno
        if not hasattr(self, "_site_cnt"):
            self._site_cnt = {}; self._site_cache = {}
        k = self._site_cnt.get(ln, 0); self._site_cnt[ln] = k + 1
        if (ln, k) in self._site_cache:
            return self._site_cache[(ln, k)]
        sl = self._slot_new(name)
        self._site_cache[(ln, k)] = sl
        return sl
    def _slot_new(self, name=None):
        self._n += 1
        name = name or f"s{self._n}"
        sem = self.es.enter_context(self.nc.semaphore("sl_" + name + f"_{self._n}"))
        sl = Slot(sem, "sl_" + name + f"_{self._n}")
        self.slots.append(sl)
        return sl
    def sbuf(self, es, name, shape, dt):
        self._n += 1
        t = es.enter_context(self.nc.sbuf_tensor(f"{name}_{self._n}", list(shape), dt))
        return Buf(t, name)
    def psum(self, es, name, shape, dt=F32):
        self._n += 1
        t = es.enter_context(self.nc.psum_tensor(f"{name}_{self._n}", list(shape), dt))
        return Buf(t, name)
    def dram(self, name, shape, dt, kind="Internal"):
        t = self.nc.dram_tensor(name, list(shape), dt, kind=kind)
        return Buf(t.ap(), name)
    def barrier(self):
        toks = [Tok(e.sem, e.count, e.key) for e in self.engs]
        toks += [Tok(s.sem, s.total, s.key) for s in self.slots]
        for e in self.engs:
            for t in toks:
                if t.key != e.key and t.val > 0:
                    e.wait(t)
    def finish(self, bufs):
        for b in bufs:
            for t in b.writers.values():
                self.sp.wait(t)

import math

def build_l0(NCOL=3072):
    nc = bass.Bass("TRN2", target_bir_lowering=False)
    cT = nc.dram_tensor("cT", [128, 32], F32, kind="ExternalInput").ap()
    w = nc.dram_tensor("w", [4096, NCOL], F32, kind="ExternalInput").ap()
    b = nc.dram_tensor("b", [1, NCOL], F32, kind="ExternalInput").ap()
    o = nc.dram_tensor("mod", [1, NCOL], F32, kind="ExternalOutput").ap()
    NB = NCOL // 512
    with ExitStack() as es:
        cx = Ctx(nc, es)
        c_sb = cx.sbuf(es, "c", [128, 32], F32)
        ca = cx.sbuf(es, "ca", [128, 32], F32)
        bs = cx.sbuf(es, "b", [1, NCOL], F32)
        res = cx.sbuf(es, "res", [1, NCOL], F32)
        wb = [cx.sbuf(es, f"w{i}", [128, 32, 512], F32) for i in range(2)]
        ws = [cx.slot() for _ in range(2)]
        ps = [cx.psum(es, f"ps{i}", [1, 512]) for i in range(2)]
        s0 = cx.slot()
        cx.sp.dma(c_sb[:], cT, s0, writes=[c_sb])
        cx.sp.dma(bs[:], b, s0, writes=[bs], cont=True)
        cx.act.op(lambda: nc.scalar.activation(out=ca[:], in_=c_sb[:], func=AF.Silu), reads=[c_sb], writes=[ca])
        wv = w.rearrange("(k p) n -> p k n", p=128)
        for j in range(NB):
            wt = wb[j % 2]
            cx.sp.dma(wt[:], wv[:, :, j * 512:(j + 1) * 512], ws[j % 2], writes=[wt])
            p = ps[j % 2]
            for k in range(32):
                cx.pe.op(lambda: nc.tensor.matmul(p[:], lhsT=ca[:, k:k + 1], rhs=wt[:, k, :], start=(k == 0), stop=(k == 31)),
                         reads=[ca, wt], writes=[p])
            cx.dve.op(lambda: nc.vector.tensor_tensor(out=res[:, j * 512:(j + 1) * 512], in0=p[:], in1=bs[:, j * 512:(j + 1) * 512], op=ALU.add),
                      reads=[p, bs], writes=[res])
        so = cx.slot()
        ob = Buf(o, 'o')
        cx.sp.dma(o, res[:], so, reads=[res], writes=[ob])
        cx.finish([ob])
    return nc


NW = 1538
EPS = 1e-6

def core_cols(i):
    r = lambda a, n: list(range(a, a + n))
    fm = r(6144 + 256 * i, 256) + r(8192 + 256 * i, 256)
    tm = r(128 * i, 128) + r(1024 + 128 * i, 128) + r(2048 + 256 * i, 256) + r(4096 + 256 * i, 256) + r(10240 + 256 * i, 256) + r(12288 + 2 * i, 2)
    return np.array(fm + tm)


def emit_inproj(cx, nc, S, x, wc, modT_d, n1T_d, ident, d_fqk, d_rqkv, d_rgfv, d_ff, d_dbg=None):
    NT = S // 128
    NBLK = S // 512
    with ExitStack() as es:
        Wb = cx.sbuf(es, "Wb", [128, 32, NW], BF16)
        idt = cx.sbuf(es, "ident", [128, 128], F32)
        gp = cx.sbuf(es, "gp", [128, 32], F32)
        sh = cx.sbuf(es, "sh", [128, 32], F32)
        sW = cx.sbuf(es, "sW", [1, NW], F32)
        ones_r = cx.sbuf(es, "ones", [1, 128], F32)
        ss = cx.sbuf(es, "ss", [128, NT], F32)
        rs = cx.sbuf(es, "rs", [128, NT], F32)
        irs = cx.sbuf(es, "irs", [128, NT], F32)
        banks = [cx.psum(es, f"bank{i}", [128, 512]) for i in range(8)]
        s0 = cx.slot()
        cx.sp.dma(idt[:], ident, s0, writes=[idt])
        modT = cx.sbuf(es, "modT1", [128, 192], F32)
        n1T = cx.sbuf(es, "n1T", [128, 32], F32)
        cx.sp.dma(modT[:], modT_d, s0, writes=[modT], cont=True)
        cx.sp.dma(n1T[:], n1T_d, s0, writes=[n1T], cont=True)
        ft = Tok(s0.sem, s0.total, s0.key)
        for dst in (idt, modT, n1T):
            dst.writers[s0.key] = ft
        cx.dve.op(lambda: nc.vector.scalar_tensor_tensor(out=gp[:], in0=modT[:, 32:64], scalar=1.0, in1=n1T[:], op0=ALU.add, op1=ALU.mult), reads=[modT, n1T], writes=[gp])
        cx.dve.op(lambda: nc.vector.tensor_copy(out=sh[:], in_=modT[:, 0:32]), reads=[modT], writes=[sh])
        cx.dve.op(lambda: nc.vector.memset(ones_r[:], 1.0), writes=[ones_r])
        cx.dve.op(lambda: nc.vector.memset(ss[:], 0.0), writes=[ss])
        epsc = cx.sbuf(es, "epsc", [128, 1], F32)
        cx.dve.op(lambda: nc.vector.memset(epsc[:], EPS), writes=[epsc])
        with ExitStack() as es2:
            stg = [cx.sbuf(es2, f"wst{i}", [128, 4, NW], F32) for i in range(2)]
            sl = [cx.slot() for _ in range(2)]
            wv = wc.rearrange("(k p) n -> p k n", p=128)
            nsl = [(0, 512), (512, 512), (1024, 512), (1536, 2)]
            for g in range(8):
                st = stg[g % 2]
                cx.sp.dma(st[:], wv[:, 4 * g:4 * g + 4, :], sl[g % 2], writes=[st])
                for kk in range(4):
                    kc = 4 * g + kk
                    for bi, (a, n) in enumerate(nsl):
                        cx.pe.op(lambda: nc.tensor.matmul(banks[bi][0:1, 0:n], lhsT=sh[:, kc:kc + 1], rhs=st[:, kk, a:a + n],
                                                          start=(kc == 0), stop=(kc == 31)), reads=[sh, st], writes=[banks[bi]])
                    if kk % 2 == 0:
                        cx.dve.op(lambda: nc.vector.tensor_scalar(out=Wb[:, kc, :], in0=st[:, kk, :], scalar1=gp[:, kc:kc + 1], scalar2=None, op0=ALU.mult),
                                  reads=[st, gp], writes=[Wb])
                    else:
                        cx.act.op(lambda: nc.scalar.activation(out=Wb[:, kc, :], in_=st[:, kk, :], func=AF.Copy, scale=gp[:, kc:kc + 1]),
                                  reads=[st, gp], writes=[Wb])
            for bi, (a, n) in enumerate(nsl):
                cx.act.op(lambda: nc.scalar.copy(out=sW[0:1, a:a + n], in_=banks[bi][0:1, 0:n]), reads=[banks[bi]], writes=[sW])
        cx.barrier()
        if d_dbg is not None:
            cx.sp.dma(d_dbg[:, :], sW[:], cx.slot(), reads=[sW], writes=[d_dbg])
        with ExitStack() as es2:
            xt = [cx.sbuf(es2, f"xt{i}", [128, 4096], F32) for i in range(2)]
            xsl = [cx.slot() for _ in range(2)]
            junk = cx.sbuf(es2, "junk", [128, 4096], BF16)
            xT = [cx.sbuf(es2, f"xT{i}", [128, 32, 512], BF16) for i in range(1)]
            rrow = [cx.sbuf(es2, f"rrow{i}", [1, 512], F32) for i in range(2)]
            rbc = [cx.sbuf(es2, f"rbc{i}", [128, 512], F32) for i in range(2)]
            rrow2 = [cx.sbuf(es2, f"rrowb{i}", [1, 512], F32) for i in range(2)]
            ofm = [cx.sbuf(es2, f"ofm{i}", [128, 4, 512], BF16) for i in range(2)]
            otm = [cx.sbuf(es2, f"otm{i}", [128, 1026], F32) for i in range(2)]
            osl = [cx.slot() for _ in range(4)]
            pT = banks[0:2]; pFM = banks[2:4]; pTM = banks[4:7]; pM = banks[7]
            xv = x.rearrange("(n p) d -> n p d", p=128)
            for blk in range(NBLK):
                xTb = xT[0]
                for tt in range(4):
                    ti = blk * 4 + tt
                    xb = xt[ti % 2]
                    cx.sp.dma(xb[:], xv[ti], xsl[ti % 2], writes=[xb])
                    cx.act.op(lambda: nc.scalar.activation(out=junk[:], in_=xb[:], func=AF.Square, accum_out=ss[:, ti:ti + 1]),
                              reads=[xb], writes=[junk, ss])
                    cx.act.op(lambda: nc.scalar.activation(out=irs[:, ti:ti + 1], in_=ss[:, ti:ti + 1], func=AF.Sqrt, bias=epsc[:, 0:1], scale=1.0 / 4096),
                              reads=[ss, epsc], writes=[irs])
                    cx.dve.op(lambda: nc.vector.reciprocal(out=rs[:, ti:ti + 1], in_=irs[:, ti:ti + 1]), reads=[irs], writes=[rs])
                    for g in range(8):
                        pb = pT[g % 2]
                        for kk in range(4):
                            kc = 4 * g + kk
                            cx.pe.op(lambda: nc.tensor.transpose(out=pb[:, kk * 128:(kk + 1) * 128], in_=xb[:, kc * 128:(kc + 1) * 128], identity=idt[:]),
                                     reads=[xb, idt], writes=[pb])
                        cx.dve.op(lambda: nc.vector.tensor_copy(out=xTb[:, 4 * g:4 * g + 4, tt * 128:(tt + 1) * 128],
                                                                in_=pb[:].rearrange("p (a b) -> p a b", a=4)), reads=[pb], writes=[xTb])
                    for (col, row) in ((irs, rrow[blk % 2]), (rs, rrow2[blk % 2])):
                        cx.pe.op(lambda: nc.tensor.transpose(out=pM[0:1, 0:128], in_=col[:, ti:ti + 1], identity=idt[:]), reads=[col, idt], writes=[pM])
                        cx.act.op(lambda: nc.scalar.copy(out=row[0:1, tt * 128:(tt + 1) * 128], in_=pM[0:1, 0:128]), reads=[pM], writes=[row])
                cx.pe.op(lambda: nc.tensor.matmul(pM[:, :], lhsT=ones_r[0:1, :], rhs=rrow2[blk % 2][0:1, :], start=True, stop=True),
                         reads=[ones_r, rrow2[blk % 2]], writes=[pM])
                cx.act.op(lambda: nc.scalar.copy(out=rbc[blk % 2][:], in_=pM[:, :]), reads=[pM], writes=[rbc[blk % 2]])
                of = ofm[blk % 2]
                for j in range(4):
                    pf = pFM[j % 2]
                    for kc in range(32):
                        cx.pe.op(lambda: nc.tensor.matmul(pf[:, :], lhsT=Wb[:, kc, j * 128:(j + 1) * 128], rhs=xTb[:, kc, :], start=(kc == 0), stop=False),
                                 reads=[Wb, xTb], writes=[pf])
                    cx.pe.op(lambda: nc.tensor.matmul(pf[:, :], lhsT=sW[0:1, j * 128:(j + 1) * 128], rhs=rrow[blk % 2][0:1, :], start=False, stop=True),
                             reads=[sW, rrow[blk % 2]], writes=[pf])
                    cx.dve.op(lambda: nc.vector.tensor_tensor(out=of[:, j, :], in0=pf[:, :], in1=rbc[blk % 2][:], op=ALU.mult),
                              reads=[pf, rbc[blk % 2]], writes=[of])
                cx.sp.dma(d_fqk[:, :, blk * 512:(blk + 1) * 512].rearrange("j p t -> p j t"), of[:], osl[blk % 2], reads=[of], writes=[d_fqk])
                for tt in range(4):
                    ti = blk * 4 + tt
                    ot = otm[ti % 2]
                    for bi, (a, n) in enumerate(((512, 512), (1024, 512), (1536, 2))):
                        pt = pTM[bi]
                        for kc in range(32):
                            cx.pe.op(lambda: nc.tensor.matmul(pt[:, 0:n], lhsT=xTb[:, kc, tt * 128:(tt + 1) * 128], rhs=Wb[:, kc, a:a + n], start=(kc == 0), stop=False),
                                     reads=[Wb, xTb], writes=[pt])
                        cx.pe.op(lambda: nc.tensor.matmul(pt[:, 0:n], lhsT=rrow[blk % 2][0:1, tt * 128:(tt + 1) * 128], rhs=sW[0:1, a:a + n], start=False, stop=True),
                                 reads=[sW, rrow[blk % 2]], writes=[pt])
                        cx.act.op(lambda: nc.scalar.activation(out=ot[:, a - 512:a - 512 + n], in_=pt[:, 0:n], func=AF.Copy, scale=rs[:, ti:ti + 1]),
                                  reads=[pt, rs], writes=[ot])
                    sl_ = osl[2 + ti % 2]
                    cx.sp.dma(d_rqkv[ti * 128:(ti + 1) * 128, :], ot[:, 0:512], sl_, reads=[ot], writes=[d_rqkv])
                    cx.sp.dma(d_rgfv[ti * 128:(ti + 1) * 128, :], ot[:, 512:1024], sl_, reads=[ot], writes=[d_rgfv], cont=True)
                    cx.sp.dma(d_ff[ti * 128:(ti + 1) * 128, :], ot[:, 1024:1026], sl_, reads=[ot], writes=[d_ff], cont=True)
        cx.barrier()


def ret_consts(h):
    lg = math.log1p(-2.0 ** (-5.0 - h))
    c = np.arange(128, dtype=np.float64)
    t = np.zeros((128, 8), np.float32)
    t[:, 0] = np.exp((c + 1) * lg)
    t[:, 1] = -np.exp((c + 1) * lg)
    t[:, 2] = 128 ** -0.5
    t[:, 3] = -(128 ** -0.5)
    t[:, 4] = np.exp((127 - c) * lg) * 128 ** -0.5
    t[:, 5] = -np.exp((127 - c) * lg) * 128 ** -0.5
    t[:, 6] = np.exp(128 * lg)
    t[:, 7] = -math.pi
    s = c[:, None]; cc = c[None, :]
    maskT = np.where(cc >= s, np.exp((-s - 1) * lg) * np.ones_like(cc), 0.0).astype(np.float32)
    return t, maskT

def rot_consts():
    half = 64
    inv_freq = (10000.0 ** (-np.arange(half, dtype=np.float32) / half)).astype(np.float32)
    invf2 = np.tile(np.concatenate([inv_freq, inv_freq])[None, :], (128, 1)).astype(np.float32)
    offs = np.tile(np.concatenate([np.zeros(64), np.full(64, math.pi / 2)])[None, :], (128, 1)).astype(np.float32)
    return invf2, offs


def emit_ret(cx, nc, S, d_rqkv, d_rgfv, posT, rc, maskT_d, invf2_d, offs_d, gn_d, ident, d_yret, dbg=None):
    NT = S // 128
    TWO_PI = 2 * math.pi
    with ExitStack() as es:
        idt = cx.sbuf(es, "ident", [128, 128], F32)
        rcs = cx.sbuf(es, "rc", [128, 8], F32)
        mk = cx.sbuf(es, "maskT", [128, 128], F32)
        invf2 = cx.sbuf(es, "invf2", [128, 128], F32)
        offs = cx.sbuf(es, "offs", [128, 128], F32)
        gn = cx.sbuf(es, "gn", [128, 256], F32)
        posi = cx.sbuf(es, "posi", [128, NT], I32)
        posf = cx.sbuf(es, "posf", [128, NT], F32)
        epsc = cx.sbuf(es, "epsc", [128, 1], F32)
        state = cx.sbuf(es, "state", [128, 256], F32)
        state_bf = cx.sbuf(es, "state_bf", [128, 256], BF16)
        banks = [cx.psum(es, f"rbank{i}", [128, 512]) for i in range(8)]
        s0 = cx.slot()
        for i, (dst, src) in enumerate(((idt, ident), (rcs, rc), (mk, maskT_d), (invf2, invf2_d), (offs, offs_d), (gn, gn_d), (posi, posT))):
            cx.sp.dma(dst[:], src, s0, writes=[dst], cont=(i > 0))
        ft = Tok(s0.sem, s0.total, s0.key)
        for dst in (idt, rcs, mk, invf2, offs, gn, posi):
            dst.writers[s0.key] = ft
        cx.dve.op(lambda: nc.vector.tensor_copy(out=posf[:], in_=posi[:]), reads=[posi], writes=[posf])
        cx.dve.op(lambda: nc.vector.memset(epsc[:], 1e-6), writes=[epsc])
        NB = 2
        qkv = [cx.sbuf(es, f"qkv{i}", [128, 512], F32) for i in range(NB)]
        rg = [cx.sbuf(es, f"rg{i}", [128, 256], F32) for i in range(NB)]
        lsl = [cx.slot() for _ in range(NB)]
        ang = cx.sbuf(es, "ang", [128, 128], F32)
        sc = cx.sbuf(es, "sc", [128, 128], F32)
        tq = cx.sbuf(es, "tq", [128, 128], F32)
        ki = cx.sbuf(es, "ki", [128, 128], I32)
        tmp = [cx.sbuf(es, f"rt{i}", [128, 64], F32) for i in range(4)]
        qd = cx.sbuf(es, "qd", [128, 128], F32)
        kr = cx.sbuf(es, "kr", [128, 128], F32)
        kd = cx.sbuf(es, "kd", [128, 128], BF16)
        vb = cx.sbuf(es, "vb", [128, 256], BF16)
        qdT = cx.sbuf(es, "qdT", [128, 128], BF16)
        krT = cx.sbuf(es, "krT", [128, 128], BF16)
        scT = cx.sbuf(es, "scT", [128, 128], BF16)
        o = cx.sbuf(es, "o", [128, 256], F32)
        junk = cx.sbuf(es, "rjunk", [128, 256], F32)
        st = cx.sbuf(es, "st", [128, 8], F32)
        sg = cx.sbuf(es, "sg", [128, 256], F32)
        yo = [cx.sbuf(es, f"yo{i}", [128, 256], BF16) for i in range(2)]
        osl = [cx.slot() for _ in range(2)]
        pqT, pkT, pS, pKV, pO = banks[0], banks[1], banks[2], banks[3], banks[4]
        V = nc.vector
        for n in range(NT):
            t_ = qkv[n % NB]; g_ = rg[n % NB]
            cx.sp.dma(t_[:], d_rqkv[n * 128:(n + 1) * 128, :], lsl[n % NB], reads=[d_rqkv], writes=[t_])
            cx.sp.dma(g_[:], d_rgfv[n * 128:(n + 1) * 128, 0:256], lsl[n % NB], reads=[d_rgfv], writes=[g_], cont=True)
            ft = Tok(lsl[n % NB].sem, lsl[n % NB].total, lsl[n % NB].key)
            t_.writers[ft.key] = ft
            cx.dve.op(lambda: V.scalar_tensor_tensor(out=ang[:], in0=invf2[:], scalar=posf[:, n:n + 1], in1=offs[:], op0=ALU.mult, op1=ALU.add),
                      reads=[invf2, posf, offs], writes=[ang])
            cx.dve.op(lambda: V.tensor_scalar(out=tq[:], in0=ang[:], scalar1=1.0 / TWO_PI, scalar2=None, op0=ALU.mult), reads=[ang], writes=[tq])
            cx.dve.op(lambda: V.tensor_copy(out=ki[:], in_=tq[:]), reads=[tq], writes=[ki])
            cx.dve.op(lambda: V.tensor_copy(out=tq[:], in_=ki[:]), reads=[ki], writes=[tq])
            cx.dve.op(lambda: V.scalar_tensor_tensor(out=ang[:], in0=tq[:], scalar=-6.28125, in1=ang[:], op0=ALU.mult, op1=ALU.add), reads=[tq, ang], writes=[ang])
            cx.dve.op(lambda: V.scalar_tensor_tensor(out=ang[:], in0=tq[:], scalar=-0.0019353071795864769, in1=ang[:], op0=ALU.mult, op1=ALU.add), reads=[tq, ang], writes=[ang])
            cx.dve.op(lambda: V.tensor_scalar(out=ang[:], in0=ang[:], scalar1=3.1415925, scalar2=-3.1415925, op0=ALU.min, op1=ALU.max), reads=[ang], writes=[ang])
            cx.act.op(lambda: nc.scalar.activation(out=sc[:], in_=ang[:], func=AF.Sin), reads=[ang], writes=[sc])
            sinp = sc[:, 0:64]; cosp = sc[:, 64:128]
            for (base, outs) in ((0, ((qd, 0, 1),)), (128, ((kr, 2, 3), (kd, 4, 5)))):
                x1 = t_[:, base:base + 64]; x2 = t_[:, base + 64:base + 128]
                cx.dve.op(lambda: V.tensor_tensor(out=tmp[0][:], in0=x1, in1=cosp, op=ALU.mult), reads=[t_, sc], writes=[tmp[0]])
                cx.dve.op(lambda: V.tensor_tensor(out=tmp[1][:], in0=x2, in1=sinp, op=ALU.mult), reads=[t_, sc], writes=[tmp[1]])
                cx.dve.op(lambda: V.tensor_tensor(out=tmp[1][:], in0=tmp[0][:], in1=tmp[1][:], op=ALU.subtract), reads=[tmp[0], tmp[1]], writes=[tmp[1]])
                cx.dve.op(lambda: V.tensor_tensor(out=tmp[2][:], in0=x1, in1=sinp, op=ALU.mult), reads=[t_, sc], writes=[tmp[2]])
                cx.dve.op(lambda: V.tensor_tensor(out=tmp[3][:], in0=x2, in1=cosp, op=ALU.mult), reads=[t_, sc], writes=[tmp[3]])
                cx.dve.op(lambda: V.tensor_tensor(out=tmp[3][:], in0=tmp[3][:], in1=tmp[2][:], op=ALU.add), reads=[tmp[2], tmp[3]], writes=[tmp[3]])
                for (dst, cp, cn) in outs:
                    cx.dve.op(lambda: V.tensor_scalar(out=dst[:, 0:64], in0=tmp[1][:], scalar1=rcs[:, cp:cp + 1], scalar2=None, op0=ALU.mult), reads=[tmp[1], rcs], writes=[dst])
                    cx.dve.op(lambda: V.tensor_scalar(out=dst[:, 64:128], in0=tmp[3][:], scalar1=rcs[:, cp:cp + 1], scalar2=None, op0=ALU.mult), reads=[tmp[3], rcs], writes=[dst])
            cx.act.op(lambda: nc.scalar.copy(out=vb[:], in_=t_[:, 256:512]), reads=[t_], writes=[vb])
            cx.pe.op(lambda: nc.tensor.transpose(out=pqT[:, 0:128], in_=qd[:], identity=idt[:]), reads=[qd, idt], writes=[pqT])
            cx.act.op(lambda: nc.scalar.copy(out=qdT[:], in_=pqT[:, 0:128]), reads=[pqT], writes=[qdT])
            cx.pe.op(lambda: nc.tensor.transpose(out=pkT[:, 0:128], in_=kr[:], identity=idt[:]), reads=[kr, idt], writes=[pkT])
            cx.act.op(lambda: nc.scalar.copy(out=krT[:], in_=pkT[:, 0:128]), reads=[pkT], writes=[krT])
            cx.pe.op(lambda: nc.tensor.matmul(pS[:, 0:128], lhsT=krT[:], rhs=qdT[:], start=True, stop=True), reads=[krT, qdT], writes=[pS])
            cx.dve.op(lambda: V.tensor_tensor(out=scT[:], in0=pS[:, 0:128], in1=mk[:], op=ALU.mult), reads=[pS, mk], writes=[scT])
            cx.pe.op(lambda: nc.tensor.matmul(pKV[:, 0:256], lhsT=kd[:], rhs=vb[:], start=True, stop=True), reads=[kd, vb], writes=[pKV])
            if n > 0:
                cx.pe.op(lambda: nc.tensor.matmul(pO[:, 0:256], lhsT=qdT[:], rhs=state_bf[:], start=True, stop=False), reads=[qdT, state_bf], writes=[pO])
            cx.pe.op(lambda: nc.tensor.matmul(pO[:, 0:256], lhsT=scT[:], rhs=vb[:], start=(n == 0), stop=True), reads=[scT, vb], writes=[pO])
            if n == 0:
                cx.dve.op(lambda: V.tensor_copy(out=state[:], in_=pKV[:, 0:256]), reads=[pKV], writes=[state])
            else:
                cx.dve.op(lambda: V.scalar_tensor_tensor(out=state[:], in0=state[:], scalar=rcs[:, 6:7], in1=pKV[:, 0:256], op0=ALU.mult, op1=ALU.add),
                          reads=[state, rcs, pKV], writes=[state])
            cx.act.op(lambda: nc.scalar.copy(out=state_bf[:], in_=state[:]), reads=[state], writes=[state_bf])
            cx.dve.op(lambda: V.memset(st[:], 0.0), writes=[st])
            cx.act.op(lambda: nc.scalar.activation(out=o[:], in_=pO[:, 0:256], func=AF.Identity, accum_out=st[:, 0:1]), reads=[pO, st], writes=[o, st])
            cx.act.op(lambda: nc.scalar.activation(out=junk[:], in_=pO[:, 0:256], func=AF.Square, accum_out=st[:, 1:2]), reads=[pO, st], writes=[junk, st])
            cx.dve.op(lambda: V.tensor_scalar(out=st[:, 2:3], in0=st[:, 0:1], scalar1=1.0 / 256, scalar2=None, op0=ALU.mult), reads=[st], writes=[st])
            cx.dve.op(lambda: V.tensor_tensor(out=st[:, 3:4], in0=st[:, 2:3], in1=st[:, 2:3], op=ALU.mult), reads=[st], writes=[st])
            cx.dve.op(lambda: V.scalar_tensor_tensor(out=st[:, 4:5], in0=st[:, 1:2], scalar=1.0 / 256, in1=st[:, 3:4], op0=ALU.mult, op1=ALU.subtract),
                      reads=[st], writes=[st])
            cx.act.op(lambda: nc.scalar.activation(out=st[:, 5:6], in_=st[:, 4:5], func=AF.Sqrt, bias=epsc[:, 0:1], scale=1.0), reads=[st, epsc], writes=[st])
            cx.dve.op(lambda: V.reciprocal(out=st[:, 6:7], in_=st[:, 5:6]), reads=[st], writes=[st])
            cx.dve.op(lambda: V.tensor_scalar(out=o[:], in0=o[:], scalar1=st[:, 2:3], scalar2=st[:, 6:7], op0=ALU.subtract, op1=ALU.mult), reads=[o, st], writes=[o])
            cx.act.op(lambda: nc.scalar.activation(out=sg[:], in_=g_[:], func=AF.Silu), reads=[g_], writes=[sg])
            cx.dve.op(lambda: V.tensor_tensor(out=o[:], in0=o[:], in1=gn[:], op=ALU.mult), reads=[o, gn], writes=[o])
            y_ = yo[n % 2]
            cx.dve.op(lambda: V.tensor_tensor(out=y_[:], in0=o[:], in1=sg[:], op=ALU.mult), reads=[o, sg], writes=[y_])
            cx.sp.dma(d_yret[n * 128:(n + 1) * 128, :], y_[:], osl[n % 2], reads=[y_], writes=[d_yret])
        cx.barrier()

def fox_consts():
    s = np.arange(128)[:, None]; c = np.arange(128)[None, :]
    triU = (s <= c).astype(np.float32)
    ones = np.ones((128, 128), np.float32)
    sel = np.zeros((128, 128), np.float32); sel[64, :] = 1.0
    return triU, ones, sel


def emit_fox(cx, nc, S, d_fqk, d_rgfv, d_ff, bf_d, triU_d, ones_d, sel_d, d_yfox):
    NT = S // 128
    SCALE = 128 ** -0.5
    V = nc.vector
    with ExitStack() as es:
        triU = cx.sbuf(es, "triU", [128, 128], F32)
        triUb = cx.sbuf(es, "triUb", [128, 128], BF16)
        ones = cx.sbuf(es, "ones", [128, 128], F32)
        sel = cx.sbuf(es, "sel", [128, 128], F32)
        bfc = cx.sbuf(es, "bfc", [128, 2], F32)
        nbf = cx.sbuf(es, "nbf", [128, 2], F32)
        one1 = cx.sbuf(es, "one1", [128, 1], F32)
        ff = cx.sbuf(es, "ff", [128, NT, 2], F32)
        lf = cx.sbuf(es, "lf", [128, NT, 2], F32)
        cum = cx.sbuf(es, "cum", [128, NT, 2], F32)
        negc = cx.sbuf(es, "negc", [128, 2, NT], F32)
        tot = cx.sbuf(es, "tot", [128, NT, 2], F32)
        off = cx.sbuf(es, "off", [128, NT, 2], F32)
        rmid = cx.sbuf(es, "rmid", [128, 2, NT], F32)
        banks = [cx.psum(es, f"fbank{i}", [128, 512]) for i in range(8)]
        s0 = cx.slot()
        for i, (dst, src) in enumerate(((triU, triU_d), (ones, ones_d), (sel, sel_d), (bfc, bf_d), (ff, d_ff.t.rearrange("(n p) h -> p n h", p=128)))):
            cx.sp.dma(dst[:], src, s0, reads=([d_ff] if dst is ff else []), writes=[dst], cont=(i > 0))
        ft = Tok(s0.sem, s0.total, s0.key)
        for dst in (triU, ones, sel, bfc, ff):
            dst.writers[s0.key] = ft
        cx.dve.op(lambda: V.memset(one1[:], 1.0), writes=[one1])
        cx.dve.op(lambda: V.tensor_copy(out=triUb[:], in_=triU[:]), reads=[triU], writes=[triUb])
        cx.dve.op(lambda: V.tensor_scalar(out=nbf[:], in0=bfc[:], scalar1=-1.0, scalar2=None, op0=ALU.mult), reads=[bfc], writes=[nbf])
        for h in range(2):
            cx.act.op(lambda: nc.scalar.activation(out=lf[:, :, h], in_=ff[:, :, h], func=AF.Exp, bias=nbf[:, h:h + 1], scale=-1.0), reads=[ff, nbf], writes=[lf])
        cx.act.op(lambda: nc.scalar.activation(out=lf[:], in_=lf[:], func=AF.Ln, bias=one1[:, 0:1], scale=1.0), reads=[lf, one1], writes=[lf])
        cx.dve.op(lambda: V.tensor_scalar(out=lf[:], in0=lf[:], scalar1=-1.0, scalar2=None, op0=ALU.mult), reads=[lf], writes=[lf])
        lf2 = lf[:].rearrange("p n h -> p (n h)"); cum2 = cum[:].rearrange("p n h -> p (n h)"); tot2 = tot[:].rearrange("p n h -> p (n h)")
        W2 = NT * 2
        for a in range(0, W2, 512):
            n_ = min(512, W2 - a)
            cx.pe.op(lambda: nc.tensor.matmul(banks[0][:, 0:n_], lhsT=triU[:], rhs=lf2[:, a:a + n_], start=True, stop=True), reads=[triU, lf], writes=[banks[0]])
            cx.dve.op(lambda: V.tensor_copy(out=cum2[:, a:a + n_], in_=banks[0][:, 0:n_]), reads=[banks[0]], writes=[cum])
            cx.pe.op(lambda: nc.tensor.matmul(banks[1][:, 0:n_], lhsT=ones[:], rhs=lf2[:, a:a + n_], start=True, stop=True), reads=[ones, lf], writes=[banks[1]])
            cx.dve.op(lambda: V.tensor_copy(out=tot2[:, a:a + n_], in_=banks[1][:, 0:n_]), reads=[banks[1]], writes=[tot])
        cx.dve.op(lambda: V.memset(off[:, 0, :], 0.0), writes=[off])
        for n in range(1, NT):
            cx.dve.op(lambda: V.tensor_tensor(out=off[:, n, :], in0=off[:, n - 1, :], in1=tot[:, n - 1, :], op=ALU.add), reads=[off, tot], writes=[off])
        cx.dve.op(lambda: V.tensor_tensor(out=cum[:], in0=cum[:], in1=off[:], op=ALU.add), reads=[cum, off], writes=[cum])
        for h in range(2):
            cx.dve.op(lambda: V.tensor_scalar(out=negc[:, h, :], in0=cum[:, :, h], scalar1=-1.0, scalar2=None, op0=ALU.mult), reads=[cum], writes=[negc])
        for a in range(0, W2, 512):
            n_ = min(512, W2 - a)
            cx.pe.op(lambda: nc.tensor.matmul(banks[0][:, 0:n_], lhsT=sel[:], rhs=cum2[:, a:a + n_], start=True, stop=True), reads=[sel, cum], writes=[banks[0]])
            cx.dve.op(lambda: V.tensor_copy(out=tot2[:, a:a + n_], in_=banks[0][:, 0:n_]), reads=[banks[0]], writes=[tot])
        for h in range(2):
            cx.dve.op(lambda: V.tensor_copy(out=rmid[:, h, :], in_=tot[:, :, h]), reads=[tot], writes=[rmid])
        KT = cx.sbuf(es, "KT", [128, S], BF16)
        QT = cx.sbuf(es, "QT", [128, S], BF16)
        Va = cx.sbuf(es, "Va", [128, NT, 130], BF16)
        vst = [cx.sbuf(es, f"vst{i}", [128, 8, 128], F32) for i in range(2)]
        vsl = [cx.slot() for _ in range(2)]
        BQ = [cx.sbuf(es, f"BQ{i}", [128, NT], F32) for i in range(2)]
        PTw = [cx.sbuf(es, f"PTw{i}", [128, 512], BF16) for i in range(3)]
        boff = [cx.sbuf(es, f"boff{i}", [128, NT], F32) for i in range(2)]
        Osb = [cx.sbuf(es, f"Osb{i}", [128, 129], F32) for i in range(4)]
        fd = cx.sbuf(es, "fd", [128, 4], F32)
        fcol = cx.sbuf(es, "fcol", [128, 4], F32)
        rl = cx.sbuf(es, "rl", [128, 2], F32)
        yo = [cx.sbuf(es, f"fyo{i}", [128, 128], BF16) for i in range(2)]
        ysl = [cx.slot() for _ in range(2)]
        ksl = cx.slot()
        pS = banks[0:3]; pO = banks[4:8]
        cx.dve.op(lambda: V.memset(Va[:, :, 128:130], 1.0), writes=[Va])
        fv = d_rgfv.t.rearrange("(n p) c -> p n c", p=128)
        grp = 0
        for h in range(2):
            cx.sp.dma(QT[:], d_fqk[h], ksl, reads=[d_fqk], writes=[QT])
            cx.sp.dma(KT[:], d_fqk[2 + h], ksl, reads=[d_fqk], writes=[KT], cont=True)
            ft = Tok(ksl.sem, ksl.total, ksl.key); QT.writers[ksl.key] = ft
            for g in range(0, NT, 8):
                ng = min(8, NT - g); i_ = (g // 8) % 2
                cx.sp.dma(vst[i_][:, 0:ng, :], fv[:, g:g + ng, 256 + h * 128:256 + (h + 1) * 128], vsl[i_], reads=[d_rgfv], writes=[vst[i_]])
                cx.dve.op(lambda: V.tensor_copy(out=Va[:, g:g + ng, 0:128], in_=vst[i_][:, 0:ng, :]), reads=[vst[i_]], writes=[Va])
            for B in range(NT // 4):
                q0 = 4 * B
                nko = 4 * B
                if nko > 0:
                    bo = boff[B % 2]
                    cx.dve.op(lambda: V.tensor_scalar(out=bo[:, 0:nko], in0=negc[:, h, 0:nko], scalar1=rmid[:, h, q0:q0 + 1], scalar2=None, op0=ALU.add),
                              reads=[negc, rmid], writes=[bo])
                    cx.dve.op(lambda: V.tensor_scalar(out=fd[:, 0:4], in0=rmid[:, h, q0:q0 + 4], scalar1=rmid[:, h, q0:q0 + 1], scalar2=None, op0=ALU.subtract),
                              reads=[rmid], writes=[fd])
                    cx.act.op(lambda: nc.scalar.activation(out=fcol[:, 0:4], in_=fd[:, 0:4], func=AF.Exp), reads=[fd], writes=[fcol])
                    for kap in range(nko):
                        ps = pS[grp % 3]; pt = PTw[grp % 3]; grp += 1
                        cx.pe.op(lambda: nc.tensor.matmul(ps[:, :], lhsT=KT[:, kap * 128:(kap + 1) * 128], rhs=QT[:, q0 * 128:(q0 + 4) * 128], start=True, stop=True),
                                 reads=[KT, QT], writes=[ps])
                        cx.act.op(lambda: nc.scalar.activation(out=pt[:, :], in_=ps[:, :], func=AF.Exp, bias=bo[:, kap:kap + 1], scale=SCALE), reads=[ps, bo], writes=[pt])
                        for j in range(4):
                            cx.pe.op(lambda: nc.tensor.matmul(pO[j][:, 0:129], lhsT=pt[:, j * 128:(j + 1) * 128], rhs=Va[:, kap, 0:129], start=(kap == 0), stop=(kap == nko - 1)),
                                     reads=[pt, Va], writes=[pO[j]])
                    for j in range(4):
                        cx.act.op(lambda: nc.scalar.activation(out=Osb[j][:], in_=pO[j][:, 0:129], func=AF.Copy, scale=fcol[:, j:j + 1]), reads=[pO[j], fcol], writes=[Osb[j]])
                for j in range(4):
                    Q = q0 + j
                    bq = BQ[Q % 2]
                    cx.dve.op(lambda: V.tensor_scalar(out=bq[:, q0:Q + 1], in0=negc[:, h, q0:Q + 1], scalar1=rmid[:, h, Q:Q + 1], scalar2=None, op0=ALU.add),
                              reads=[negc, rmid], writes=[bq])
                    po = pO[j]
                    nk = j + 1
                    ps = pS[grp % 3]; pt = PTw[grp % 3]; grp += 1
                    for jj in range(nk):
                        kap = q0 + jj
                        cx.pe.op(lambda: nc.tensor.matmul(ps[:, jj * 128:(jj + 1) * 128], lhsT=KT[:, kap * 128:(kap + 1) * 128], rhs=QT[:, Q * 128:(Q + 1) * 128],
                                                          start=True, stop=True), reads=[KT, QT], writes=[ps])
                    for jj in range(nk):
                        kap = q0 + jj
                        cx.act.op(lambda: nc.scalar.activation(out=pt[:, jj * 128:(jj + 1) * 128], in_=ps[:, jj * 128:(jj + 1) * 128], func=AF.Exp, bias=bq[:, kap:kap + 1], scale=SCALE),
                                  reads=[ps, bq], writes=[pt])
                        if kap == Q:
                            cx.dve.op(lambda: V.tensor_tensor(out=pt[:, jj * 128:(jj + 1) * 128], in0=pt[:, jj * 128:(jj + 1) * 128], in1=triUb[:], op=ALU.mult), reads=[pt, triUb], writes=[pt])
                    for jj in range(nk):
                        kap = q0 + jj
                        cx.pe.op(lambda: nc.tensor.matmul(po[:, 0:129], lhsT=pt[:, jj * 128:(jj + 1) * 128], rhs=Va[:, kap, 0:129], start=(jj == 0), stop=(jj == nk - 1)),
                                 reads=[pt, Va], writes=[po])
                    if nko > 0:
                        cx.dve.op(lambda: V.tensor_tensor(out=Osb[j][:], in0=Osb[j][:], in1=po[:, 0:129], op=ALU.add), reads=[Osb[j], po], writes=[Osb[j]])
                    else:
                        cx.dve.op(lambda: V.tensor_copy(out=Osb[j][:], in_=po[:, 0:129]), reads=[po], writes=[Osb[j]])
                    cx.dve.op(lambda: V.reciprocal(out=rl[:, Q % 2:Q % 2 + 1], in_=Osb[j][:, 128:129]), reads=[Osb[j]], writes=[rl])
                    y_ = yo[Q % 2]
                    cx.dve.op(lambda: V.tensor_scalar(out=y_[:], in0=Osb[j][:, 0:128], scalar1=rl[:, Q % 2:Q % 2 + 1], scalar2=None, op0=ALU.mult), reads=[Osb[j], rl], writes=[y_])
                    cx.sp.dma(d_yfox[Q * 128:(Q + 1) * 128, h * 128:(h + 1) * 128], y_[:], ysl[Q % 2], reads=[y_], writes=[d_yfox])
        cx.barrier()

class TopkScratch:
    def __init__(self, cx, es, nc):
        self.s2 = cx.sbuf(es, "tk_s2", [128, 128], F32)
        self.t1 = None
        self.i1u = None
        self.i1 = cx.sbuf(es, "tk_i1", [128, 16, 16], F32)
        self.cand = cx.sbuf(es, "tk_cand", [128, 256], F32)
        self.cand2 = cx.sbuf(es, "tk_cand2", [128, 256], F32)
        self.cta = cx.sbuf(es, "tk_cta", [128, 8, 16], F32)
        self.pua = cx.sbuf(es, "tk_pua", [128, 8, 16], U32)
        self.aua = cx.sbuf(es, "tk_aua", [128, 8, 16], U32)
        self.bua = self.aua
        self.afa = cx.sbuf(es, "tk_afa", [128, 8, 16], F32)
        self.bfa = cx.sbuf(es, "tk_bfa", [128, 8, 16], F32)
        self.eq4 = cx.sbuf(es, "tk_eq4", [128, 2, 16, 16], BF16)
        self.io16 = cx.sbuf(es, "tk_io16", [128, 16], F32)
        self.exa = cx.sbuf(es, "tk_exa", [128, 8, 16], F32)
        self.za = cx.sbuf(es, "tk_za", [128, 8], F32)
        cx.pool.op(lambda: nc.gpsimd.iota(self.io16[:], pattern=[[1, 16]], base=0, channel_multiplier=0, allow_small_or_imprecise_dtypes=True), writes=[self.io16])


def emit_level1(cx, nc, sc, s_sb, chunk):
    V = nc.vector
    cx.dve.op(lambda: V.max(out=sc.t1[:, chunk, 0:8], in_=s_sb[:]), reads=[s_sb], writes=[sc.t1])
    cx.dve.op(lambda: V.max_index(out=sc.i1u[:, chunk, 0:8], in_max=sc.t1[:, chunk, 0:8], in_values=s_sb[:]), reads=[s_sb, sc.t1], writes=[sc.i1u])
    cx.dve.op(lambda: V.match_replace(out=sc.s2[:], in_to_replace=sc.t1[:, chunk, 0:8], in_values=s_sb[:], imm_value=-1e30), reads=[s_sb, sc.t1], writes=[sc.s2])
    cx.dve.op(lambda: V.max(out=sc.t1[:, chunk, 8:16], in_=sc.s2[:]), reads=[sc.s2], writes=[sc.t1])
    cx.dve.op(lambda: V.max_index(out=sc.i1u[:, chunk, 8:16], in_max=sc.t1[:, chunk, 8:16], in_values=sc.s2[:]), reads=[sc.s2, sc.t1], writes=[sc.i1u])


def emit_level2(cx, nc, sc, TI1, TI2, TG):
    V = nc.vector
    cx.dve.op(lambda: V.tensor_copy(out=sc.i1[:], in_=sc.i1u[:]), reads=[sc.i1u], writes=[sc.i1])
    for h in range(8):
        ta = sc.t1[:, 2 * h, :]; tb = sc.t1[:, 2 * h + 1, :]
        cand3 = sc.cand[:].rearrange("p (a b) -> p a b", a=16)
        cx.dve.op(lambda: V.tensor_tensor(out=cand3, in0=ta.unsqueeze(2).to_broadcast([128, 16, 16]), in1=tb.unsqueeze(1).to_broadcast([128, 16, 16]), op=ALU.add),
                  reads=[sc.t1], writes=[sc.cand])
        cx.dve.op(lambda: V.max(out=sc.cta[:, h, 0:8], in_=sc.cand[:]), reads=[sc.cand], writes=[sc.cta])
        cx.dve.op(lambda: V.max_index(out=sc.pua[:, h, 0:8], in_max=sc.cta[:, h, 0:8], in_values=sc.cand[:]), reads=[sc.cand, sc.cta], writes=[sc.pua])
        cx.dve.op(lambda: V.match_replace(out=sc.cand2[:], in_to_replace=sc.cta[:, h, 0:8], in_values=sc.cand[:], imm_value=-1e30), reads=[sc.cand, sc.cta], writes=[sc.cand2])
        cx.dve.op(lambda: V.max(out=sc.cta[:, h, 8:16], in_=sc.cand2[:]), reads=[sc.cand2], writes=[sc.cta])
        cx.dve.op(lambda: V.max_index(out=sc.pua[:, h, 8:16], in_max=sc.cta[:, h, 8:16], in_values=sc.cand2[:]), reads=[sc.cand2, sc.cta], writes=[sc.pua])
    cx.dve.op(lambda: V.tensor_single_scalar(out=sc.aua[:], in_=sc.pua[:], scalar=4, op=ALU.logical_shift_right), reads=[sc.pua], writes=[sc.aua])
    cx.dve.op(lambda: V.tensor_copy(out=sc.afa[:], in_=sc.aua[:]), reads=[sc.aua], writes=[sc.afa])
    cx.dve.op(lambda: V.tensor_single_scalar(out=sc.bua[:], in_=sc.pua[:], scalar=15, op=ALU.bitwise_and), reads=[sc.pua], writes=[sc.bua])
    cx.dve.op(lambda: V.tensor_copy(out=sc.bfa[:], in_=sc.bua[:]), reads=[sc.bua], writes=[sc.bfa])
    i1v = sc.i1[:].rearrange("p (h two) a -> p h two a", two=2)
    for (sel, half, TI) in ((sc.afa, 0, TI1), (sc.bfa, 1, TI2)):
        for hb in range(4):
            hs = slice(2 * hb, 2 * hb + 2)
            cx.dve.op(lambda: V.tensor_tensor(out=sc.eq4[:], in0=sel[:, hs, :].unsqueeze(3).to_broadcast([128, 2, 16, 16]),
                                              in1=sc.io16[:].unsqueeze(1).unsqueeze(1).to_broadcast([128, 2, 16, 16]), op=ALU.is_equal),
                      reads=[sel, sc.io16], writes=[sc.eq4])
            cx.dve.op(lambda: V.tensor_tensor(out=sc.eq4[:], in0=sc.eq4[:], in1=i1v[:, hs, half, :].unsqueeze(2).to_broadcast([128, 2, 16, 16]), op=ALU.mult),
                      reads=[sc.eq4, sc.i1], writes=[sc.eq4])
            cx.dve.op(lambda: V.tensor_reduce(out=TI[:, 32 * hb:32 * hb + 32].rearrange("p (h j) -> p h j", h=2), in_=sc.eq4[:], axis=AX.X, op=ALU.add), reads=[sc.eq4], writes=[TI])
    cx.dve.op(lambda: V.tensor_tensor(out=sc.exa[:], in0=sc.cta[:], in1=sc.cta[:, :, 0:1].to_broadcast([128, 8, 16]), op=ALU.subtract), reads=[sc.cta], writes=[sc.exa])
    cx.act.op(lambda: nc.scalar.activation(out=sc.exa[:], in_=sc.exa[:], func=AF.Exp), reads=[sc.exa], writes=[sc.exa])
    cx.dve.op(lambda: V.tensor_reduce(out=sc.za[:], in_=sc.exa[:], axis=AX.X, op=ALU.add), reads=[sc.exa], writes=[sc.za])
    cx.dve.op(lambda: V.reciprocal(out=sc.za[:], in_=sc.za[:]), reads=[sc.za], writes=[sc.za])
    cx.dve.op(lambda: V.tensor_tensor(out=TG[:].rearrange("p (h j) -> p h j", h=8), in0=sc.exa[:], in1=sc.za[:].unsqueeze(2).to_broadcast([128, 8, 16]), op=ALU.mult),
              reads=[sc.exa, sc.za], writes=[TG])


EPS = 1e-6

def emit_l2(cx, nc, TPC, x_d, yret_d, yfox_d, mod_row, modT_d, n2T_d, gfT_d, fg_row, Wo_d, Wq_d, skT_d, uT_d, v_d, ident_d, d_x2, d_x3, out_d, stage=9, NCH=128):
    V = nc.vector
    NTT = TPC // 128
    TGA = min(TPC, 1024)
    T = min(TPC, 512)
    NTG = T // 128
    with ExitStack() as es:
        banks = [cx.psum(es, f"l2bank{i}", [128, 512]) for i in range(8)]
        idt = cx.sbuf(es, "ident", [128, 128], F32)
        idtb = cx.sbuf(es, "identb", [128, 128], BF16)
        modT = cx.sbuf(es, "modT", [128, 192], F32)
        n2T = cx.sbuf(es, "n2T", [128, 32], F32)
        gfT = cx.sbuf(es, "gfT", [128, 16], F32)
        g2p = cx.sbuf(es, "g2p", [128, 32], F32)
        epsc = cx.sbuf(es, "epsc", [128, 1], F32)
        ssf = cx.sbuf(es, "ssf", [128, NTT], F32)
        rsf = cx.sbuf(es, "rsf", [128, NTT], F32)
        ss2 = cx.sbuf(es, "ss2", [128, NTT, 8], F32)
        rs2 = cx.sbuf(es, "rs2", [128, NTT], F32)
        ss3 = cx.sbuf(es, "ss3", [128, NTT, 8], F32)
        rs3 = cx.sbuf(es, "rs3", [128, NTT], F32)
        iota = cx.sbuf(es, "iota", [128, 128], F32)
        s0 = cx.slot()
        for i, (dst, src) in enumerate(((idt, ident_d), (modT, modT_d), (n2T, n2T_d), (gfT, gfT_d))):
            cx.sp.dma(dst[:], src, s0, writes=[dst], cont=(i > 0))
        ft = Tok(s0.sem, s0.total, s0.key)
        for dst in (idt, modT, n2T, gfT):
            dst.writers[s0.key] = ft
        cx.dve.op(lambda: V.tensor_copy(out=idtb[:], in_=idt[:]), reads=[idt], writes=[idtb])
        cx.dve.op(lambda: V.memset(epsc[:], EPS), writes=[epsc])
        for b_ in (ssf, ss2, ss3):
            cx.dve.op(lambda: V.memset(b_[:], 0.0), writes=[b_])
        cx.pool.op(lambda: nc.gpsimd.iota(iota[:], pattern=[[1, 128]], base=0, channel_multiplier=0, allow_small_or_imprecise_dtypes=True), writes=[iota])
        cx.dve.op(lambda: V.scalar_tensor_tensor(out=g2p[:], in0=modT[:, 128:160], scalar=1.0, in1=n2T[:], op0=ALU.add, op1=ALU.mult), reads=[modT, n2T], writes=[g2p])
        sh2 = modT[:, 96:128]

        with ExitStack() as esA:
            gate1 = cx.sbuf(esA, "gate1", [128, 4096], F32)
            cx.sp.dma(gate1[:], mod_row[0:1, 8192:12288].partition_broadcast(128), cx.slot(), writes=[gate1])
            yT = cx.sbuf(esA, "yT", [128, 32, TGA], BF16)
            yt = [cx.sbuf(esA, f"yt{i}", [128, 4096], BF16) for i in range(2)]
            ysl = [cx.slot() for _ in range(2)]
            junkb = cx.sbuf(esA, "junkb", [128, 2048], BF16)
            wst = [cx.sbuf(esA, f"wost{i}", [128, 8, 512], F32) for i in range(2)]
            wsl = [cx.slot() for _ in range(2)]
            wb = [cx.sbuf(esA, f"wob{i}", [128, 32, 512], BF16) for i in range(2)]
            xp = [cx.sbuf(esA, f"xp{i}", [128, 512], F32) for i in range(3)]
            xsl = [cx.slot() for _ in range(3)]
            osl = [cx.slot() for _ in range(3)]
            junk = cx.sbuf(esA, "junkA", [128, 512], F32)
            pTb = [banks[i][:].bitcast(BF16) for i in (0, 1)]
            wov = Wo_d.rearrange("(k p) n -> p k n", p=128)
            pc = 0
            for ga in range(TPC // TGA):
                for tt in range(TGA // 128):
                    ti = ga * (TGA // 128) + tt
                    yb = yt[ti % 2]
                    cx.sp.dma(yb[:, 0:2048], yret_d[ti * 128:(ti + 1) * 128, :], ysl[ti % 2], reads=[yret_d], writes=[yb])
                    cx.sp.dma(yb[:, 2048:4096], yfox_d[ti * 128:(ti + 1) * 128, :], ysl[ti % 2], reads=[yfox_d], writes=[yb], cont=True)
                    ft = Tok(ysl[ti % 2].sem, ysl[ti % 2].total, ysl[ti % 2].key); yb.writers[ft.key] = ft
                    cx.act.op(lambda: nc.scalar.activation(out=junkb[:], in_=yb[:, 2048:4096], func=AF.Square, accum_out=ssf[:, ti:ti + 1]), reads=[yb, ssf], writes=[junkb, ssf])
                    cx.act.op(lambda: nc.scalar.activation(out=rsf[:, ti:ti + 1], in_=ssf[:, ti:ti + 1], func=AF.Sqrt, bias=epsc[:, 0:1], scale=1.0 / 2048), reads=[ssf, epsc], writes=[rsf])
                    cx.dve.op(lambda: V.reciprocal(out=rsf[:, ti:ti + 1], in_=rsf[:, ti:ti + 1]), reads=[rsf], writes=[rsf])
                    for g in range(4):
                        pb = banks[g % 2]; pv = pTb[g % 2]
                        for kk in range(8):
                            kc = 8 * g + kk
                            cx.pe.op(lambda: nc.tensor.transpose(out=pv[:, kk * 128:(kk + 1) * 128], in_=yb[:, kc * 128:(kc + 1) * 128], identity=idtb[:]), reads=[yb, idtb], writes=[pb])
                        cx.dve.op(lambda: V.tensor_copy(out=yT[:, 8 * g:8 * g + 8, tt * 128:(tt + 1) * 128], in_=pv[:, :].rearrange("p (a b) -> p a b", a=8)), reads=[pb], writes=[yT])
                for nb in range(8):
                    wbb = wb[nb % 2]
                    for q4 in range(4):
                        st = wst[(nb * 4 + q4) % 2]
                        cx.sp.dma(st[:], wov[:, 8 * q4:8 * q4 + 8, nb * 512:(nb + 1) * 512], wsl[(nb * 4 + q4) % 2], writes=[st])
                        if q4 < 2:
                            cx.dve.op(lambda: V.tensor_tensor(out=wbb[:, 8 * q4:8 * q4 + 8, :], in0=st[:], in1=gate1[:, nb * 512:(nb + 1) * 512].unsqueeze(1).to_broadcast([128, 8, 512]), op=ALU.mult),
                                      reads=[st, gate1], writes=[wbb])
                        else:
                            for kk in range(8):
                                kc = 8 * q4 + kk
                                cx.dve.op(lambda: V.scalar_tensor_tensor(out=wbb[:, kc, :], in0=st[:, kk, :], scalar=gfT[:, kc - 16:kc - 15], in1=gate1[:, nb * 512:(nb + 1) * 512], op0=ALU.mult, op1=ALU.mult),
                                          reads=[st, gfT, gate1], writes=[wbb])
                    for tt in range(TGA // 128):
                        ti = ga * (TGA // 128) + tt
                        xb = xp[pc % 3]; sl_x = xsl[pc % 3]; sl_o = osl[pc % 3]; pc += 1
                        cx.sp.dma(xb[:], x_d[ti * 128:(ti + 1) * 128, nb * 512:(nb + 1) * 512], sl_x, writes=[xb])
                        pr = banks[2 + (tt % 2) * 2]; pf = banks[3 + (tt % 2) * 2]
                        for kc in range(16):
                            cx.pe.op(lambda: nc.tensor.matmul(pr[:, :], lhsT=yT[:, kc, tt * 128:(tt + 1) * 128], rhs=wbb[:, kc, :], start=(kc == 0), stop=(kc == 15)), reads=[yT, wbb], writes=[pr])
                        for kc in range(16, 32):
                            cx.pe.op(lambda: nc.tensor.matmul(pf[:, :], lhsT=yT[:, kc, tt * 128:(tt + 1) * 128], rhs=wbb[:, kc, :], start=(kc == 16), stop=(kc == 31)), reads=[yT, wbb], writes=[pf])
                        cx.dve.op(lambda: V.scalar_tensor_tensor(out=xb[:], in0=pf[:, :], scalar=rsf[:, ti:ti + 1], in1=xb[:], op0=ALU.mult, op1=ALU.add), reads=[pf, rsf, xb], writes=[xb])
                        cx.dve.op(lambda: V.tensor_tensor(out=xb[:], in0=xb[:], in1=pr[:, :], op=ALU.add), reads=[xb, pr], writes=[xb])
                        cx.act.op(lambda: nc.scalar.activation(out=junk[:], in_=xb[:], func=AF.Square, accum_out=ss2[:, ti, nb:nb + 1]), reads=[xb, ss2], writes=[junk, ss2])
                        cx.sp.dma(d_x2[ti * 128:(ti + 1) * 128, nb * 512:(nb + 1) * 512], xb[:], sl_o, reads=[xb], writes=[d_x2])
            cx.barrier()
        if stage < 1:
            return
        cx.dve.op(lambda: V.tensor_reduce(out=rs2[:], in_=ss2[:], axis=AX.X, op=ALU.add), reads=[ss2], writes=[rs2])
        cx.act.op(lambda: nc.scalar.activation(out=rs2[:], in_=rs2[:], func=AF.Sqrt, bias=epsc[:, 0:1], scale=1.0 / 4096), reads=[rs2, epsc], writes=[rs2])
        cx.dve.op(lambda: V.reciprocal(out=rs2[:], in_=rs2[:]), reads=[rs2], writes=[rs2])

        NGRP = TPC // T
        d_ub = cx.dram("ub_scr", [NCH, 128, 32, 128], BF16)
        d_vb = cx.dram("vb_scr", [NCH, 128, 4096], BF16)
        G = cx.sbuf(es, "G", [128, 128, T], BF16)
        kI1 = cx.sbuf(es, "kI1", [128, T], F32)
        kI2 = cx.sbuf(es, "kI2", [128, T], F32)
        kG = cx.sbuf(es, "kG", [128, T], F32)
        for gp_ in range(TPC // T):
            t0 = gp_ * T
            cx.new_epoch()
            with ExitStack() as esB:
                h2T = cx.sbuf(esB, "h2T", [128, 32, T], BF16)
                with ExitStack() as esa:
                    x2ts = [cx.sbuf(esa, f"x2t{i}", [128, 4096], F32) for i in range(2)]
                    xss_ = [cx.slot() for _ in range(2)]
                    for tt in range(NTG):
                        ti = gp_ * NTG + tt
                        x2t = x2ts[tt % 2]; xs_ = xss_[tt % 2]
                        cx.sp.dma(x2t[:], d_x2[ti * 128:(ti + 1) * 128, :], xs_, reads=[d_x2], writes=[x2t])
                        cx.act.op(lambda: nc.scalar.activation(out=x2t[:], in_=x2t[:], func=AF.Copy, scale=rs2[:, ti:ti + 1]), reads=[x2t, rs2], writes=[x2t])
                        for g in range(8):
                            pb = banks[g % 2]
                            for kk in range(4):
                                kc = 4 * g + kk
                                cx.pe.op(lambda: nc.tensor.transpose(out=pb[:, kk * 128:(kk + 1) * 128], in_=x2t[:, kc * 128:(kc + 1) * 128], identity=idt[:]), reads=[x2t, idt], writes=[pb])
                            for kk in range(4):
                                kc = 4 * g + kk
                                e_ = cx.act if kk % 2 == 0 else cx.dve
                                if kk % 2 == 0:
                                    cx.act.op(lambda: nc.scalar.activation(out=h2T[:, kc, tt * 128:(tt + 1) * 128], in_=pb[:, kk * 128:(kk + 1) * 128], func=AF.Identity, scale=g2p[:, kc:kc + 1], bias=sh2[:, kc:kc + 1]),
                                              reads=[pb, g2p, modT], writes=[h2T])
                                else:
                                    cx.dve.op(lambda: V.tensor_scalar(out=h2T[:, kc, tt * 128:(tt + 1) * 128], in0=pb[:, kk * 128:(kk + 1) * 128], scalar1=g2p[:, kc:kc + 1], scalar2=sh2[:, kc:kc + 1], op0=ALU.mult, op1=ALU.add),
                                              reads=[pb, g2p, modT], writes=[h2T])
                    cx.barrier()
                if stage < 2:
                    return
                with ExitStack() as esb:
                    sc = TopkScratch(cx, esb, nc)
                    scs = [TopkScratch.__new__(TopkScratch) for _ in range(NTG)]
                    t1s = [cx.sbuf(esb, f"t1s{i}", [128, 16, 16], F32) for i in range(NTG)]
                    i1s = [cx.sbuf(esb, f"i1s{i}", [128, 16, 16], U32) for i in range(NTG)]
                    wqst = [cx.sbuf(esb, f"wqst{i}", [128, 8, 128], F32) for i in range(2)]
                    wqsl = [cx.slot() for _ in range(2)]
                    wqb = [cx.sbuf(esb, f"wqb{i}", [128, 32, 128], BF16) for i in range(1)]
                    sksl = [cx.slot() for _ in range(2)]
                    skst = [cx.sbuf(esb, f"skst{i}", [128, 128], F32) for i in range(1)] * 2
                    skb = [cx.sbuf(esb, f"skb{i}", [128, 128], BF16) for i in range(2)]
                    qTc = [cx.sbuf(esb, f"qTc{i}", [128, T], BF16) for i in range(2)]
                    s_sb = [cx.sbuf(esb, f"s_sb{i}", [128, 128], F32) for i in range(2)]
                    TI = [cx.sbuf(esb, n, [128, 128], F32) for n in ("TI1", "TI2", "TG")]
                    wqv = Wq_d
                    cnt = 0
                    for ch in range(16):
                        wq_ = wqb[0]; sks = skst[ch % 2]; sk_ = skb[ch % 2]; qc = qTc[ch % 2]
                        for hf in range(4):
                            st = wqst[hf % 2]
                            cx.sp.dma(st[:], wqv[ch, :, 8 * hf:8 * hf + 8, :], wqsl[hf % 2], writes=[st])
                            if hf % 2 == 0:
                                cx.pool.op(lambda: nc.gpsimd.tensor_copy(out=wq_[:, 8 * hf:8 * hf + 8, :], in_=st[:]), reads=[st], writes=[wq_])
                            else:
                                cx.dve.op(lambda: V.tensor_copy(out=wq_[:, 8 * hf:8 * hf + 8, :], in_=st[:]), reads=[st], writes=[wq_])
                        cx.sp.dma(sks[:], skT_d[ch], sksl[ch % 2], writes=[sks])
                        cx.dve.op(lambda: V.tensor_copy(out=sk_[:], in_=sks[:]), reads=[sks], writes=[sk_])
                        pq = banks[2 + ch % 2]
                        for kc in range(32):
                            cx.pe.op(lambda: nc.tensor.matmul(pq[:, 0:T], lhsT=wq_[:, kc, :], rhs=h2T[:, kc, :], start=(kc == 0), stop=(kc == 31)), reads=[wq_, h2T], writes=[pq])
                        cx.act.op(lambda: nc.scalar.copy(out=qc[:], in_=pq[:, 0:T]), reads=[pq], writes=[qc])
                        for tt in range(NTG):
                            ps = banks[4 + cnt % 2]; ss_ = s_sb[cnt % 2]; cnt += 1
                            cx.pe.op(lambda: nc.tensor.matmul(ps[:, 0:128], lhsT=qc[:, tt * 128:(tt + 1) * 128], rhs=sk_[:], start=True, stop=True), reads=[qc, sk_], writes=[ps])
                            cx.act.op(lambda: nc.scalar.copy(out=ss_[:], in_=ps[:, 0:128]), reads=[ps], writes=[ss_])
                            sc.t1, sc.i1u = t1s[tt], i1s[tt]
                            emit_level1(cx, nc, sc, ss_, ch)
                    for tt in range(NTG):
                        sc.t1, sc.i1u = t1s[tt], i1s[tt]
                        emit_level2(cx, nc, sc, *TI)
                        for (src, dst) in zip(TI, (kI1, kI2, kG)):
                            pt = banks[6]
                            cx.pe.op(lambda: nc.tensor.transpose(out=pt[:, 0:128], in_=src[:], identity=idt[:]), reads=[src, idt], writes=[pt])
                            cx.act.op(lambda: nc.scalar.copy(out=dst[:, tt * 128:(tt + 1) * 128], in_=pt[:, 0:128]), reads=[pt], writes=[dst])
                    cx.barrier()
                if stage < 3:
                    return
                with ExitStack() as ese:
                    At = [cx.sbuf(ese, f"At{i}", [128, 128], BF16) for i in range(4)]
                    Bt = [cx.sbuf(ese, f"Bt{i}", [128, 128], BF16) for i in range(4)]
                    for t4 in range(T // 4):
                        pg = banks[t4 % 2]
                        for j in range(4):
                            t_ = t4 * 4 + j
                            a_ = At[j]; b_ = Bt[j]
                            cx.dve.op(lambda: V.tensor_scalar(out=a_[:], in0=iota[:], scalar1=kI1[:, t_:t_ + 1], scalar2=kG[:, t_:t_ + 1], op0=ALU.is_equal, op1=ALU.mult), reads=[iota, kI1, kG], writes=[a_])
                            cx.dve.op(lambda: V.tensor_scalar(out=b_[:], in0=iota[:], scalar1=kI2[:, t_:t_ + 1], scalar2=None, op0=ALU.is_equal), reads=[iota, kI2], writes=[b_])
                            cx.pe.op(lambda: nc.tensor.matmul(pg[:, j * 128:(j + 1) * 128], lhsT=a_[:], rhs=b_[:], start=True, stop=True), reads=[a_, b_], writes=[pg])
                        cx.act.op(lambda: nc.scalar.copy(out=G[:, :, t4 * 4:t4 * 4 + 4].rearrange("p c t -> p t c"), in_=pg[:, :].rearrange("p (t c) -> p t c", t=4)), reads=[pg], writes=[G])
                    cx.barrier()
                if stage < 4:
                    return
                with ExitStack() as esf:
                    ust = [cx.sbuf(esf, f"ust{i}", [128, 16, 128], F32) for i in range(2)]
                    usl = [cx.slot() for _ in range(2)]
                    ub = [cx.sbuf(esf, f"ub{i}", [128, 32, 128], BF16) for i in range(2)]
                    ga = [cx.sbuf(esf, f"ga{i}", [128, T], BF16) for i in range(2)]
                    ubs = [cx.slot() for _ in range(2)]
                    uv = uT_d
                    hc = 0
                    for c in range(NCH):
                        ub_ = ub[c % 2]
                        if gp_ == 0:
                            for hf in range(2):
                                st = ust[hc % 2]; sl_ = usl[hc % 2]; hc += 1
                                cx.sp.dma(st[:], uv[c, :, 16 * hf:16 * hf + 16, :], sl_, writes=[st])
                                if hf == 0:
                                    cx.pool.op(lambda: nc.gpsimd.tensor_copy(out=ub_[:, 0:16, :], in_=st[:]), reads=[st], writes=[ub_])
                                else:
                                    cx.dve.op(lambda: V.tensor_copy(out=ub_[:, 16:32, :], in_=st[:]), reads=[st], writes=[ub_])
                            if NGRP > 1:
                                cx.sp.dma(d_ub[c], ub_[:], ubs[c % 2], reads=[ub_], writes=[d_ub])
                        else:
                            cx.sp.dma(ub_[:], d_ub[c], ubs[c % 2], reads=[d_ub], writes=[ub_])
                        pa = banks[2 + c % 2]
                        for kc in range(32):
                            cx.pe.op(lambda: nc.tensor.matmul(pa[:, 0:T], lhsT=ub_[:, kc, :], rhs=h2T[:, kc, :], start=(kc == 0), stop=(kc == 31)), reads=[ub_, h2T], writes=[pa])
                        g_ = ga[c % 2]
                        cx.act.op(lambda: nc.scalar.activation(out=g_[:], in_=pa[:, 0:T], func=AF.Gelu_apprx_tanh), reads=[pa], writes=[g_])
                        cx.dve.op(lambda: V.tensor_tensor(out=G[:, c, :], in0=G[:, c, :], in1=g_[:], op=ALU.mult), reads=[G, g_], writes=[G])
                    cx.barrier()
            if stage < 5:
                return
            with ExitStack() as esg:
                gate2 = cx.sbuf(esg, "gate2", [128, 4096], F32)
                cx.sp.dma(gate2[:], mod_row[0:1, 20480:24576].partition_broadcast(128), cx.slot(), writes=[gate2])
                vst = [cx.sbuf(esg, f"vst{i}", [128, 1024], F32) for i in range(3)]
                vsl = [cx.slot() for _ in range(3)]
                vbs = [cx.slot() for _ in range(3)]
                vb = [cx.sbuf(esg, f"vb{i}", [128, 2, 1024], BF16) for i in range(3)]
                xq = [cx.sbuf(esg, f"xq{i}", [128, 512], F32) for i in range(3)]
                xqs = [cx.slot() for _ in range(3)]
                xos = [cx.slot() for _ in range(3)]
                junk = cx.sbuf(esg, "junkg", [128, 512], F32)
                vc = 0; pc = 0
                for p in range(4):
                    c = 0
                    while c < NCH:
                        step = 1 if (gp_ == 0 or NCH - c < 2) else 2
                        st = vst[vc % 3]; vb_ = vb[vc % 3]; sl_ = vsl[vc % 3]; sl2_ = vbs[vc % 3]; vc += 1
                        if gp_ == 0:
                            cx.sp.dma(st[:], v_d[c, :, p * 1024:(p + 1) * 1024], sl_, writes=[st])
                            if c % 2 == 0:
                                cx.pool.op(lambda: nc.gpsimd.tensor_tensor(out=vb_[:, 0, :], in0=st[:], in1=gate2[:, p * 1024:(p + 1) * 1024], op=ALU.mult), reads=[st, gate2], writes=[vb_])
                            else:
                                cx.dve.op(lambda: V.tensor_tensor(out=vb_[:, 0, :], in0=st[:], in1=gate2[:, p * 1024:(p + 1) * 1024], op=ALU.mult), reads=[st, gate2], writes=[vb_])
                            if NGRP > 1:
                                cx.sp.dma(d_vb[c, :, p * 1024:(p + 1) * 1024], vb_[:, 0, :], sl2_, reads=[vb_], writes=[d_vb])
                        else:
                            cx.sp.dma(vb_[:, 0:step, :], d_vb[c:c + step, :, p * 1024:(p + 1) * 1024].rearrange("c i d -> i c d"), sl2_, reads=[d_vb], writes=[vb_])
                        for cc in range(step):
                            ce = c + cc
                            for ts in range(NTG):
                                for j in range(2):
                                    po = banks[ts * 2 + j]
                                    cx.pe.op(lambda: nc.tensor.matmul(po[:, :], lhsT=G[:, ce, ts * 128:(ts + 1) * 128], rhs=vb_[:, cc, j * 512:(j + 1) * 512], start=(ce == 0), stop=(ce == NCH - 1)), reads=[G, vb_], writes=[po])
                        c += step
                    for ts in range(NTG):
                        ti = gp_ * NTG + ts
                        for j in range(2):
                            nb = p * 2 + j
                            po = banks[ts * 2 + j]
                            xb = xq[pc % 3]; s1_ = xqs[pc % 3]; s2_ = xos[pc % 3]; pc += 1
                            cx.sp.dma(xb[:], d_x2[ti * 128:(ti + 1) * 128, nb * 512:(nb + 1) * 512], s1_, reads=[d_x2], writes=[xb])
                            cx.dve.op(lambda: V.tensor_tensor(out=xb[:], in0=xb[:], in1=po[:, :], op=ALU.add), reads=[xb, po], writes=[xb])
                            cx.act.op(lambda: nc.scalar.activation(out=junk[:], in_=xb[:], func=AF.Square, accum_out=ss3[:, ti, nb:nb + 1]), reads=[xb, ss3], writes=[junk, ss3])
                            cx.sp.dma(d_x3[ti * 128:(ti + 1) * 128, nb * 512:(nb + 1) * 512], xb[:], s2_, reads=[xb], writes=[d_x3])
                cx.barrier()
        if stage < 6:
            return
        cx.dve.op(lambda: V.tensor_reduce(out=rs3[:], in_=ss3[:], axis=AX.X, op=ALU.add), reads=[ss3], writes=[rs3])
        cx.act.op(lambda: nc.scalar.activation(out=rs3[:], in_=rs3[:], func=AF.Sqrt, bias=epsc[:, 0:1], scale=1.0 / 4096), reads=[rs3, epsc], writes=[rs3])
        cx.dve.op(lambda: V.reciprocal(out=rs3[:], in_=rs3[:]), reads=[rs3], writes=[rs3])
        with ExitStack() as esh:
            fg = cx.sbuf(esh, "fg", [128, 4096], F32)
            cx.sp.dma(fg[:], fg_row[0:1, :].partition_broadcast(128), cx.slot(), writes=[fg])
            xt = [cx.sbuf(esh, f"x3t{i}", [128, 4096], F32) for i in range(2)]
            sl1 = [cx.slot() for _ in range(2)]; sl2 = [cx.slot() for _ in range(2)]
            for ti in range(NTT):
                xb = xt[ti % 2]
                cx.sp.dma(xb[:], d_x3[ti * 128:(ti + 1) * 128, :], sl1[ti % 2], reads=[d_x3], writes=[xb])
                cx.dve.op(lambda: V.scalar_tensor_tensor(out=xb[:], in0=xb[:], scalar=rs3[:, ti:ti + 1], in1=fg[:], op0=ALU.mult, op1=ALU.mult), reads=[xb, rs3, fg], writes=[xb])
                cx.sp.dma(out_d[ti * 128:(ti + 1) * 128, :], xb[:], sl2[ti % 2], reads=[xb], writes=[out_d])
            cx.barrier()


S_FULL = 16384

def build_l1(S):
    nc = bass.Bass("TRN2", target_bir_lowering=False)
    di = lambda n, s, d=F32: nc.dram_tensor(n, s, d, kind="ExternalInput").ap()
    x = di("x", [S, 4096]); wc = di("wc", [4096, NW]); modT = di("modT", [128, 192]); n1T = di("n1T", [128, 32]); ident = di("ident", [128, 128])
    posT = di("posT", [128, S // 128], I32); rc = di("rc", [128, 8]); maskT = di("maskT", [128, 128]); invf2 = di("invf2", [128, 128]); offs = di("offs", [128, 128])
    gn = di("gn", [128, 256]); bf = di("bf", [128, 2]); triU = di("triU", [128, 128]); ones = di("ones", [128, 128]); sel = di("sel", [128, 128])
    with ExitStack() as es:
        cx = Ctx(nc, es)
        d_fqk = cx.dram("fqk", [4, 128, S], BF16); d_rqkv = cx.dram("rqkv", [S, 512], F32); d_rgfv = cx.dram("rgfv", [S, 512], F32); d_ff = cx.dram("ffl", [S, 2], F32)
        d_yret = cx.dram("yret", [S, 256], BF16, kind="ExternalOutput"); d_yfox = cx.dram("yfox", [S, 256], BF16, kind="ExternalOutput")
        emit_inproj(cx, nc, S, x, wc, modT, n1T, ident, d_fqk, d_rqkv, d_rgfv, d_ff)
        emit_ret(cx, nc, S, d_rqkv, d_rgfv, posT, rc, maskT, invf2, offs, gn, ident, d_yret)
        emit_fox(cx, nc, S, d_fqk, d_rgfv, d_ff, bf, triU, ones, sel, d_yfox)
        cx.barrier()
        cx.finish([d_yret, d_yfox])
    return nc


def build_l2(TPC):
    nc = bass.Bass("TRN2", target_bir_lowering=False)
    di = lambda n, s, d=F32: nc.dram_tensor(n, s, d, kind="ExternalInput").ap()
    with ExitStack() as es:
        cx = Ctx(nc, es)
        x = di("x", [TPC, 4096]); yret = Buf(di("yret", [TPC, 2048], BF16)); yfox = Buf(di("yfox", [TPC, 2048], BF16))
        mod_row = di("mod_row", [1, 24576]); modT = di("modT", [128, 192]); n2T = di("n2T", [128, 32]); gfT = di("gfT", [128, 16]); fg_row = di("fg_row", [1, 4096])
        Wo = di("Wo", [4096, 4096]); Wq = di("Wq", [16, 128, 32, 128]); skT = di("skT", [16, 128, 128]); uT = di("uT", [128, 128, 32, 128]); v = di("v", [128, 128, 4096]); ident = di("ident", [128, 128])
        d_x2 = cx.dram("x2", [TPC, 4096], F32); d_x3 = cx.dram("x3", [TPC, 4096], F32)
        out = cx.dram("out", [TPC, 4096], F32, kind="ExternalOutput")
        emit_l2(cx, nc, TPC, x, yret, yfox, mod_row, modT, n2T, gfT, fg_row, Wo, Wq, skT, uT, v, ident, d_x2, d_x3, out)
        cx.barrier()
        cx.finish([out])
    return nc


def kernel(x, c, positions, w_ada, b_ada, norm1_g, w_in, b_forget, ret_gn_g, fox_norm_g, w_out, norm2_g, w_peer_q, peer_sub_keys, peer_u, peer_v, final_g):
    f32 = lambda a: np.ascontiguousarray(np.asarray(a, dtype=np.float32))
    x = f32(x); S = x.shape[1]; TPC = S // 8
    col = lambda v_: np.ascontiguousarray(np.asarray(v_, np.float32).reshape(-1, 128).T)
    cores = list(range(8))
    w_ada0 = np.asarray(w_ada, np.float32)[0]; b_ada0 = np.asarray(b_ada, np.float32)[0]
    nc0 = build_l0()
    maps = [{"cT": col(np.asarray(c, np.float32)[0]), "w": np.ascontiguousarray(w_ada0[:, i * 3072:(i + 1) * 3072]), "b": np.ascontiguousarray(b_ada0[None, i * 3072:(i + 1) * 3072])} for i in cores]
    r0 = run_bass_kernel_spmd(nc0, maps, core_ids=cores)
    mod = np.concatenate([np.asarray(r["mod"])[0] for r in r0.results])
    modT = col(mod)
    ident = np.eye(128, dtype=np.float32)
    w_in0 = np.asarray(w_in, np.float32)[0]
    pos = np.asarray(positions, np.int32)[0]
    invf2, offs = rot_consts(); triU, ones, sel = fox_consts()
    nc1 = build_l1(S)
    maps = []
    for i in cores:
        rc, maskT = ret_consts(i)
        maps.append({"x": x[0], "wc": np.ascontiguousarray(w_in0[:, core_cols(i)]), "modT": modT, "n1T": col(np.asarray(norm1_g)[0]), "ident": ident,
                     "posT": np.ascontiguousarray(pos.reshape(-1, 128).T), "rc": rc, "maskT": maskT, "invf2": invf2, "offs": offs,
                     "gn": np.ascontiguousarray(np.tile(np.asarray(ret_gn_g, np.float32)[0][None, i * 256:(i + 1) * 256], (128, 1))),
                     "bf": np.ascontiguousarray(np.tile(np.asarray(b_forget, np.float32)[0][None, 2 * i:2 * i + 2], (128, 1))), "triU": triU, "ones": ones, "sel": sel})
    r1 = run_bass_kernel_spmd(nc1, maps, core_ids=cores)
    yret = np.concatenate([np.asarray(r["yret"]) for r in r1.results], axis=1)
    yfox = np.concatenate([np.asarray(r["yfox"]) for r in r1.results], axis=1)
    u = np.asarray(peer_u, np.float32)[0]; v = np.asarray(peer_v, np.float32)[0]
    uT = np.ascontiguousarray(u.reshape(128, 128, 32, 128).transpose(1, 3, 2, 0))
    vr = np.ascontiguousarray(v.reshape(128, 128, 4096).transpose(1, 0, 2))
    skT = np.ascontiguousarray(np.asarray(peer_sub_keys, np.float32)[0].reshape(16, 128, 128).transpose(0, 2, 1))
    wm = {"mod_row": np.ascontiguousarray(mod[None, :]), "modT": modT, "n2T": col(np.asarray(norm2_g)[0]), "gfT": col(np.asarray(fox_norm_g)[0]),
          "fg_row": np.ascontiguousarray(np.asarray(final_g, np.float32)[None, :]), "Wo": f32(np.asarray(w_out)[0]), "Wq": np.ascontiguousarray(np.asarray(w_peer_q, np.float32)[0].reshape(32, 128, 16, 128).transpose(2, 1, 0, 3)),
          "skT": skT, "uT": uT, "v": vr, "ident": ident}
    nc2 = build_l2(TPC)
    maps = []
    for i in cores:
        sl = slice(i * TPC, (i + 1) * TPC)
        m = dict(wm); m.update({"x": np.ascontiguousarray(x[0, sl]), "yret": np.ascontiguousarray(yret[sl]), "yfox": np.ascontiguousarray(yfox[sl])})
        maps.append(m)
    r2 = run_bass_kernel_spmd(nc2, maps, core_ids=cores)
    out = np.concatenate([np.asarray(r["out"]) for r in r2.results], axis=0)
    return out.reshape(1, S, 4096).astype(np.float32)
```
